# Optimizing a Trainium2 kernel written in Bass

```python
import math
import functools
import jax
import jax.numpy as jnp
from jax import lax
import numpy as np

D_MODEL = 1024
BATCH = 2
SEQ = 16384
DEPTH = 4

GRID_W = 64
CTX_LEN = 256

POOL_WIDTH = D_MODEL // 4
POOL_WINDOWS = (2, 4, 8, 16)
POOL_GROUPS = len(POOL_WINDOWS)
POOL_GROUP_DIM = POOL_WIDTH // POOL_GROUPS

HYENA_WIDTH = D_MODEL // 4
HYENA_SHORT = 3
HYENA_BANDS = 16
HYENA_EMB = 1 + 2 * HYENA_BANDS
HYENA_FILTER_HIDDEN = 64
HYENA_TARGET = 1e-2
HYENA_FAST_DECAY_PCT = 0.3
HYENA_SLOW_DECAY_PCT = 1.5

DIFF_HEADS = 4
DIFF_WIDTH = D_MODEL // 2
DIFF_V_DIM = DIFF_WIDTH // DIFF_HEADS
DIFF_HEAD_DIM = DIFF_V_DIM // 2
DIFF_QK_WIDTH = DIFF_HEADS * 2 * DIFF_HEAD_DIM
ROPE_FREQS = DIFF_HEAD_DIM // 4
ROPE_THETA = 10000.0
Q_BLOCK = 128

MIX_WIDTH = POOL_WIDTH + HYENA_WIDTH + DIFF_WIDTH
HYENA_OFF = POOL_WIDTH
ATT_OFF = POOL_WIDTH + 3 * HYENA_WIDTH
IN_WIDTH = ATT_OFF + 2 * DIFF_QK_WIDTH + DIFF_WIDTH

D_FF = ((8 * D_MODEL // 3 + 127) // 128) * 128
N_EXPERTS = 8
TOP_K = 2
D_FF_EXPERT = 7 * D_MODEL // 2
N_DENSE = (DEPTH + 1) // 2
N_MOE = DEPTH // 2
EPS = 1e-6

kernel_name = "hybrid_pool_hyena_diffattn_moe_dit"


def rmsnorm(x, g):
    xf = x.astype(jnp.float32)
    y = xf * lax.rsqrt(jnp.mean(xf * xf, axis=-1, keepdims=True) + EPS)
    return (y * g.astype(jnp.float32)).astype(x.dtype)


def modulate(x, shift, scale):
    return x * (1.0 + scale) + shift


def axial_rope_tables(L):
    rows = L // GRID_W
    row = jnp.repeat(jnp.arange(rows, dtype=jnp.float32), GRID_W)
    col = jnp.broadcast_to(jnp.arange(GRID_W, dtype=jnp.float32), (rows, GRID_W)).reshape(-1)
    inv_freq = jnp.power(ROPE_THETA, -jnp.arange(ROPE_FREQS, dtype=jnp.float32) / ROPE_FREQS)
    ang = jnp.stack([row, col], axis=-1)[:, :, None] * inv_freq
    return jnp.cos(ang), jnp.sin(ang)


def apply_axial_rope(x, cos, sin):
    shp = x.shape
    xr = x.astype(jnp.float32).reshape(shp[:-1] + (2, 2, ROPE_FREQS))
    a, b = xr[..., 0, :], xr[..., 1, :]
    c, s = cos[:, None, None], sin[:, None, None]
    out = jnp.stack([a * c - b * s, b * c + a * s], axis=-2)
    return out.reshape(shp).astype(x.dtype)


def attn_qkv(z_att, qk_g, rope):
    B, L, _ = z_att.shape
    q = z_att[..., :DIFF_QK_WIDTH].reshape(B, L, DIFF_HEADS, 2, DIFF_HEAD_DIM)
    k = z_att[..., DIFF_QK_WIDTH:2 * DIFF_QK_WIDTH].reshape(B, L, DIFF_HEADS, 2, DIFF_HEAD_DIM)
    v = z_att[..., 2 * DIFF_QK_WIDTH:].reshape(B, L, DIFF_HEADS, DIFF_V_DIM)
    q = rmsnorm(q, qk_g[0])
    k = rmsnorm(k, qk_g[1])
    if rope is not None:
        q = apply_axial_rope(q, rope[0], rope[1])
        k = apply_axial_rope(k, rope[0], rope[1])
    return q, k.transpose(0, 2, 3, 1, 4), v.transpose(0, 2, 1, 3)


def diff_lambda_value(lv, lam_init):
    lv = lv.astype(jnp.float32)
    return jnp.exp(jnp.sum(lv[0] * lv[1])) - jnp.exp(jnp.sum(lv[2] * lv[3])) + lam_init


def diff_attend(q, k, v, lam):
    s = jnp.einsum("bqhmd,bhmkd->bhmqk", q, k, preferred_element_type=jnp.float32) * (DIFF_HEAD_DIM ** -0.5)
    p = jax.nn.softmax(s, axis=-1)
    w = p[:, :, 0] - lam * p[:, :, 1]
    return jnp.einsum("bhqk,bhkd->bqhd", w.astype(v.dtype), v)


def diff_attend_blocks(q, k, v, lam):
    B, L = q.shape[:2]
    nb = L // Q_BLOCK
    qb = jnp.moveaxis(q.reshape((B, nb, Q_BLOCK) + q.shape[2:]), 1, 0)
    o = lax.map(lambda qblk: diff_attend(qblk, k, v, lam), qb)
    return jnp.moveaxis(o, 0, 1).reshape(B, L, DIFF_HEADS, DIFF_V_DIM)


def pool_mix(z, lin, scale):
    B, L, _ = z.shape
    zg = z.reshape(B, L, POOL_GROUPS, POOL_GROUP_DIM).astype(jnp.float32)
    cs = jnp.concatenate([jnp.zeros_like(zg[:, :1]), jnp.cumsum(zg, axis=1)], axis=1)
    t = jnp.arange(L)
    pooled = []
    for g, win in enumerate(POOL_WINDOWS):
        lo = jnp.clip(t - win // 2, 0, L)
        hi = jnp.clip(t + win // 2, 0, L)
        cnt = (hi - lo).astype(jnp.float32)[None, :, None]
        pooled.append((cs[:, hi, g] - cs[:, lo, g]) / cnt)
    d = (jnp.stack(pooled, axis=2) - zg).astype(z.dtype)
    y = jnp.einsum("blgc,gce->blge", d, lin).reshape(B, L, POOL_WIDTH)
    return y * scale


def short_conv(z, w, b):
    L = z.shape[1]
    pad = HYENA_SHORT // 2
    zp = jnp.pad(z, ((0, 0), (pad, HYENA_SHORT - 1 - pad), (0, 0)))
    y = b
    for j in range(HYENA_SHORT):
        y = y + zp[:, j:j + L] * w[j]
    return y


def hyena_filter(L, w1, b1, f1, w2, b2, f2, w3):
    f32 = jnp.float32
    t = jnp.linspace(0.0, 1.0, L, dtype=f32)[:, None]
    w = (2.0 * math.pi / L) * jnp.arange(L, dtype=f32)[:, None]
    bands = jnp.linspace(1e-4, HYENA_BANDS - 1, HYENA_BANDS, dtype=f32)[None, :]
    feat = jnp.concatenate([t, jnp.cos(bands * w), -jnp.sin(bands * w)], axis=-1)
    h = jnp.sin(f1.astype(f32) * (feat @ w1.astype(f32) + b1.astype(f32)))
    h = jnp.sin(f2.astype(f32) * (h @ w2.astype(f32) + b2.astype(f32)))
    h = (h @ w3.astype(f32)).reshape(L, 2, HYENA_WIDTH)
    max_decay = math.log(1.0 / HYENA_TARGET) / HYENA_FAST_DECAY_PCT
    min_decay = math.log(1.0 / HYENA_TARGET) / HYENA_SLOW_DECAY_PCT
    deltas = jnp.linspace(min_decay, max_decay, HYENA_WIDTH, dtype=f32)
    h = h * jnp.exp(-t * deltas)[:, None, :]
    k = jnp.concatenate([h[:, 0], jnp.zeros((1, HYENA_WIDTH), f32), jnp.flip(h[1:, 1], axis=0)], axis=0)
    return k / jnp.sum(jnp.abs(k), axis=0, keepdims=True)


def long_conv(u, k):
    L = u.shape[1]
    n = 2 * L
    uf = jnp.fft.rfft(u.astype(jnp.float32), n=n, axis=1)
    kf = jnp.fft.rfft(k, n=n, axis=0)
    return jnp.fft.irfft(uf * kf[None], n=n, axis=1)[:, :L].astype(u.dtype)


def hyena_mix(z, sw, sb, filt, bias):
    L = z.shape[1]
    x0, x1, v = jnp.split(short_conv(z, sw, sb), 3, axis=-1)
    v = v * x1
    y = long_conv(v, hyena_filter(L, *filt)) + v * bias
    return y * x0


def mix_merge(z, att, lam_init, pool_lin, pool_scale, sw, sb, filt, hy_bias, subln_g, w_out):
    B, L, _ = z.shape
    y_pool = pool_mix(z[..., :POOL_WIDTH], pool_lin, pool_scale)
    y_hy = hyena_mix(z[..., HYENA_OFF:ATT_OFF], sw, sb, filt, hy_bias)
    y_att = (rmsnorm(att, subln_g) * (1.0 - lam_init)).reshape(B, L, DIFF_WIDTH)
    return jnp.concatenate([y_pool, y_hy, y_att], axis=-1) @ w_out


def swiglu(u, w_gate, w_up, w_down):
    return (jax.nn.silu(u @ w_gate) * (u @ w_up)) @ w_down


def moe_swiglu(u, router_w, w_gate, w_up, w_down):
    shp = u.shape
    ut = u.reshape(-1, shp[-1])
    logits = jnp.matmul(ut, router_w, preferred_element_type=jnp.float32)
    top_val, top_idx = lax.top_k(logits, TOP_K)
    gate = jax.nn.softmax(top_val, axis=-1)
    comb = jnp.einsum("nk,nke->ne", gate, jax.nn.one_hot(top_idx, N_EXPERTS, dtype=jnp.float32))
    y = jnp.zeros(ut.shape, jnp.float32)
    for e in range(N_EXPERTS):
        he = jax.nn.silu(ut @ w_gate[e]) * (ut @ w_up[e])
        y = y + comb[:, e:e + 1] * (he @ w_down[e])
    return y.astype(u.dtype).reshape(shp)


def setup_inputs(seed: int = 0) -> dict:
    key = jax.random.key(seed)
    ks = iter(jax.random.split(key, 40))

    def nrm(shape, scale):
        return scale * jax.random.normal(next(ks), shape, jnp.float32)

    def gain(shape, scale):
        return 1.0 + nrm(shape, scale)

    D = D_MODEL
    return {
        "x": nrm((BATCH, SEQ, D), 1.0),
        "c": nrm((BATCH, D), 1.0),
        "ctx": nrm((BATCH, CTX_LEN, D), 1.0),
        "c_ctx": nrm((D,), 1.0),
        "mod_w": nrm((DEPTH, D, 6 * D), 0.5 * D ** -0.5),
        "mod_b": nrm((DEPTH, 6 * D), 0.01),
        "norm1_g": gain((DEPTH, D), 0.05),
        "norm2_g": gain((DEPTH, D), 0.05),
        "w_in": nrm((DEPTH, D, IN_WIDTH), D ** -0.5),
        "w_out": nrm((DEPTH, MIX_WIDTH, D), MIX_WIDTH ** -0.5),
        "pool_lin": nrm((DEPTH, POOL_GROUPS, POOL_GROUP_DIM, POOL_GROUP_DIM), POOL_GROUP_DIM ** -0.5),
        "pool_scale": gain((DEPTH, POOL_WIDTH), 0.1),
        "hy_short_w": nrm((DEPTH, HYENA_SHORT, 3 * HYENA_WIDTH), HYENA_SHORT ** -0.5),
        "hy_short_b": nrm((DEPTH, 3 * HYENA_WIDTH), 0.02),
        "hy_f_w1": nrm((DEPTH, HYENA_EMB, HYENA_FILTER_HIDDEN), HYENA_EMB ** -0.5),
        "hy_f_b1": nrm((DEPTH, HYENA_FILTER_HIDDEN), 0.1),
        "hy_f_freq1": gain((DEPTH, HYENA_FILTER_HIDDEN), 0.01),
        "hy_f_w2": nrm((DEPTH, HYENA_FILTER_HIDDEN, HYENA_FILTER_HIDDEN), HYENA_FILTER_HIDDEN ** -0.5),
        "hy_f_b2": nrm((DEPTH, HYENA_FILTER_HIDDEN), 0.1),
        "hy_f_freq2": gain((DEPTH, HYENA_FILTER_HIDDEN), 0.01),
        "hy_f_w3": nrm((DEPTH, HYENA_FILTER_HIDDEN, 2 * HYENA_WIDTH), HYENA_FILTER_HIDDEN ** -0.5),
        "hy_bias": nrm((DEPTH, HYENA_WIDTH), 1.0),
        "qk_norm_g": gain((DEPTH, 2, DIFF_HEAD_DIM), 0.05),
        "diff_lambda": nrm((DEPTH, 4, DIFF_HEAD_DIM), 0.1),
        "subln_g": gain((DEPTH, DIFF_V_DIM), 0.05),
        "ffn_w_gate": nrm((N_DENSE, D, D_FF), D ** -0.5),
        "ffn_w_up": nrm((N_DENSE, D, D_FF), D ** -0.5),
        "ffn_w_down": nrm((N_DENSE, D_FF, D), D_FF ** -0.5),
        "router_w": nrm((N_MOE, D, N_EXPERTS), D ** -0.5),
        "moe_w_gate": nrm((N_MOE, N_EXPERTS, D, D_FF_EXPERT), D ** -0.5),
        "moe_w_up": nrm((N_MOE, N_EXPERTS, D, D_FF_EXPERT), D ** -0.5),
        "moe_w_down": nrm((N_MOE, N_EXPERTS, D_FF_EXPERT, D), D_FF_EXPERT ** -0.5),
    }


def reference(x, c, ctx, c_ctx, mod_w, mod_b, norm1_g, norm2_g, w_in, w_out,
              pool_lin, pool_scale, hy_short_w, hy_short_b, hy_f_w1, hy_f_b1, hy_f_freq1,
              hy_f_w2, hy_f_b2, hy_f_freq2, hy_f_w3, hy_bias, qk_norm_g, diff_lambda, subln_g,
              ffn_w_gate, ffn_w_up, ffn_w_down, router_w, moe_w_gate, moe_w_up, moe_w_down):
    L = x.shape[1]
    rope = axial_rope_tables(L)
    silu_c = jax.nn.silu(c)[:, None, :]
    silu_cc = jax.nn.silu(c_ctx)
    h = ctx
    for i in range(DEPTH):
        last = i == DEPTH - 1
        m_lat = jnp.split(silu_c @ mod_w[i] + mod_b[i], 6, axis=-1)
        m_ctx = jnp.split(silu_cc @ mod_w[i] + mod_b[i], 6, axis=-1)
        lam_init = 0.8 - 0.6 * math.exp(-0.3 * i)
        lam = diff_lambda_value(diff_lambda[i], lam_init)
        filt = (hy_f_w1[i], hy_f_b1[i], hy_f_freq1[i], hy_f_w2[i], hy_f_b2[i], hy_f_freq2[i], hy_f_w3[i])
        merge = functools.partial(mix_merge, lam_init=lam_init, pool_lin=pool_lin[i], pool_scale=pool_scale[i],
                                  sw=hy_short_w[i], sb=hy_short_b[i], filt=filt, hy_bias=hy_bias[i],
                                  subln_g=subln_g[i], w_out=w_out[i])
        z_lat = modulate(rmsnorm(x, norm1_g[i]), m_lat[0], m_lat[1]) @ w_in[i]
        z_ctx = modulate(rmsnorm(h, norm1_g[i]), m_ctx[0], m_ctx[1]) @ w_in[i]
        q_l, k_l, v_l = attn_qkv(z_lat[..., ATT_OFF:], qk_norm_g[i], rope)
        q_c, k_c, v_c = attn_qkv(z_ctx[..., ATT_OFF:], qk_norm_g[i], None)
        att_lat = diff_attend_blocks(q_l, jnp.concatenate([k_c, k_l], axis=3),
                                     jnp.concatenate([v_c, v_l], axis=2), lam)
        x = x + m_lat[2] * merge(z_lat, att_lat)
        if not last:
            att_ctx = diff_attend(q_c, k_c, v_c, lam)
            h = h + m_ctx[2] * merge(z_ctx, att_ctx)
        if i % 2 == 0:
            j = i // 2
            ffn = functools.partial(swiglu, w_gate=ffn_w_gate[j], w_up=ffn_w_up[j], w_down=ffn_w_down[j])
        else:
            j = i // 2
            ffn = functools.partial(moe_swiglu, router_w=router_w[j], w_gate=moe_w_gate[j],
                                    w_up=moe_w_up[j], w_down=moe_w_down[j])
        x = x + m_lat[5] * ffn(modulate(rmsnorm(x, norm2_g[i]), m_lat[3], m_lat[4]))
        if not last:
            h = h + m_ctx[5] * ffn(modulate(rmsnorm(h, norm2_g[i]), m_ctx[3], m_ctx[4]))
    return x
```

```python
import math
import numpy as np
import ml_dtypes
import concourse.bass as bass
import concourse.mybir as mybir
from concourse.bass_utils import run_bass_kernel_spmd

F32 = mybir.dt.float32
BF16 = mybir.dt.bfloat16
I32 = mybir.dt.int32
AF = mybir.ActivationFunctionType
ALU = mybir.AluOpType
AX = mybir.AxisListType

ENGS = ["tensor", "vector", "scalar", "gpsimd", "sync"]


class Buf:
    __slots__ = ("t", "w", "r", "name")

    def __init__(self, t, name=""):
        self.t = t
        self.w = None
        self.r = {}
        self.name = name

    def __getitem__(self, idx):
        return self.t[idx]


class FW:
    def __init__(self, nc, dma_ring=6):
        self.nc = nc
        self.prog = {e: [] for e in ENGS}
        self.sems = {}
        self.seq = {}
        self.known = {e: {} for e in ENGS}
        for e in ENGS:
            self.sems[e] = nc.alloc_semaphore("s_" + e)
            self.seq[e] = 0
        self.dma_ring = dma_ring
        self.dq = {}
        for q in ["sync", "gpsimd", "scalar"]:
            ring = []
            for i in range(dma_ring):
                key = "d_%s_%d" % (q, i)
                self.sems[key] = nc.alloc_semaphore(key)
                ring.append(key)
            self.dq[q] = {"ring": ring, "n": 0, "hist": []}
        self.n_inst = 0

    def sb(self, name, shape, dtype):
        return Buf(self.nc.alloc_sbuf_tensor("sb_" + name, list(shape), dtype), name)

    def ps(self, name, shape, dtype=F32):
        return Buf(self.nc.alloc_psum_tensor("ps_" + name, list(shape), dtype), name)

    def dram(self, name, shape, dtype, kind="Internal"):
        return Buf(self.nc.dram_tensor(name, list(shape), dtype, kind=kind), name)

    def _deps(self, eng, reads, writes):
        need = {}

        def add(ev):
            if ev is None:
                return
            k, v = ev
            if need.get(k, 0) < v:
                need[k] = v
        for b in reads:
            add(b.w)
        for b in writes:
            add(b.w)
            for k, v in b.r.items():
                add((k, v))
        kn = self.known[eng]
        out = []
        for k, v in need.items():
            if kn.get(k, 0) < v:
                kn[k] = v
                out.append((k, v))
        return out

    def _commit(self, ev, reads, writes):
        k, v = ev
        for b in reads:
            if b.r.get(k, 0) < v:
                b.r[k] = v
        for b in writes:
            b.w = ev
            b.r = {}

    def op(self, eng, fn, reads=(), writes=(), same_eng_sync=True):
        waits = self._deps(eng, reads, writes)
        self.seq[eng] += 1
        ev = (eng, self.seq[eng])
        if not same_eng_sync:
            waits = [w for w in waits if w[0] != eng]
        self.prog[eng].append((waits, fn, ev, False))
        self._commit(ev, reads, writes)
        self.n_inst += 1
        return ev

    def dma(self, q, out_ap, in_ap, reads=(), writes=(), **kw):
        dq = self.dq[q]
        n = dq["n"]
        dq["n"] += 1
        key = dq["ring"][n % self.dma_ring]
        val = 16 * (n // self.dma_ring + 1)
        waits = self._deps(q, reads, writes)
        if n >= self.dma_ring:
            pk, pv = key, val - 16
            if self.known[q].get(pk, 0) < pv:
                self.known[q][pk] = pv
                waits.append((pk, pv))
        ev = (key, val)

        def fn(e, out_ap=out_ap, in_ap=in_ap, kw=kw):
            return e.dma_start(out=out_ap, in_=in_ap, **kw)
        self.prog[q].append((waits, fn, ev, True))
        self._commit(ev, reads, writes)
        self.n_inst += 1
        return ev

    def finish(self, final_events):
        nc = self.nc
        sems = self.sems
        prog = self.prog
        with nc.Block() as block:
            def make(engname):
                def body(e):
                    for waits, fn, ev, is_dma in prog[engname]:
                        for k, v in waits:
                            e.wait_ge(sems[k], v)
                        ins = fn(e)
                        ins.then_inc(sems[ev[0]], 16 if is_dma else 1)
                    if engname == "sync":
                        fin = {}
                        for k, v in final_events:
                            fin[k] = max(fin.get(k, 0), v)
                        for k, v in fin.items():
                            e.wait_ge(sems[k], v)
                return body
            block.tensor(make("tensor"))
            block.vector(make("vector"))
            block.scalar(make("scalar"))
            block.gpsimd(make("gpsimd"))
            block.sync(make("sync"))


class Pool:
    def __init__(self, fw, name, shape, dtype, n, space="sb"):
        mk = fw.sb if space == "sb" else fw.ps
        self.bufs = [mk("%s_%d" % (name, i), shape, dtype) for i in range(n)]
        self.i = 0

    def next(self):
        b = self.bufs[self.i % len(self.bufs)]
        self.i += 1
        return b


def _lst(x):
    if x is None:
        return []
    if isinstance(x, (list, tuple)):
        return list(x)
    return [x]


class OPS:
    def __init__(self, fw):
        self.fw = fw

    def act(self, ob, oap, ib, iap, func, bias=None, scale=None, accum=None, extra_r=(), eng="scalar"):
        kw = {}
        reads = _lst(ib) + list(extra_r)
        writes = _lst(ob)
        if bias is not None:
            kw["bias"] = bias
        if scale is not None:
            kw["scale"] = scale
        if accum is not None:
            kw["accum_out"] = accum[1]
            writes.append(accum[0])
        return self.fw.op("scalar", lambda e: e.activation(out=oap, in_=iap, func=func, **kw), reads, writes)

    def mm(self, ob, oap, lb, lap, rb, rap, start=True, stop=True, **kw):
        return self.fw.op("tensor", lambda e: e.matmul(oap, lhsT=lap, rhs=rap, start=start, stop=stop, **kw),
                          _lst(lb) + _lst(rb), _lst(ob), same_eng_sync=False)

    def tr(self, ob, oap, ib, iap, idb, idap):
        return self.fw.op("tensor", lambda e: e.transpose(oap, iap, idap), _lst(ib) + _lst(idb), _lst(ob),
                          same_eng_sync=False)

    def tt(self, eng, ob, oap, ab, aap, bb, bap, op):
        return self.fw.op(eng, lambda e: e.tensor_tensor(out=oap, in0=aap, in1=bap, op=op),
                          _lst(ab) + _lst(bb), _lst(ob))

    def ts(self, eng, ob, oap, ab, aap, s1, s2, op0, op1=None, extra_r=(), accum=None):
        kw = {}
        writes = _lst(ob)
        if op1 is not None:
            kw["op1"] = op1
        if accum is not None:
            kw["accum_out"] = accum[1]
            writes.append(accum[0])
        return self.fw.op(eng, lambda e: e.tensor_scalar(out=oap, in0=aap, scalar1=s1, scalar2=s2, op0=op0, **kw),
                          _lst(ab) + list(extra_r), writes)

    def stt(self, ob, oap, ab, aap, scalar, bb, bap, op0, op1, extra_r=()):
        return self.fw.op("vector", lambda e: e.scalar_tensor_tensor(out=oap, in0=aap, scalar=scalar, in1=bap,
                                                                      op0=op0, op1=op1),
                          _lst(ab) + _lst(bb) + list(extra_r), _lst(ob))

    def cp(self, eng, ob, oap, ib, iap):
        return self.fw.op(eng, lambda e: e.tensor_copy(out=oap, in_=iap), _lst(ib), _lst(ob))

    def recip(self, ob, oap, ib, iap):
        return self.fw.op("vector", lambda e: e.reciprocal(out=oap, in_=iap), _lst(ib), _lst(ob))

    def memset(self, eng, ob, oap, val):
        return self.fw.op(eng, lambda e: e.memset(oap, val), [], _lst(ob))


D = 1024
INW = 2560
EPS = 1e-6

PV = {}
_c = 0
def _add(name, n):
    global _c
    PV[name] = (_c, n)
    _c += n
_add("g1", 8)
_add("sh_l", 8)
_add("sc_l", 8)
_add("sh_c", 8)
_add("sc_c", 8)
_add("cw", 18)
_add("cb", 6)
_add("gq", 1)
_add("gk", 1)
_add("psc", 2)
_add("mL", 1)
_add("mR", 1)
_add("zero", 1)
_add("eps", 1)
NPV = _c


def build_p0(ncols=768, nlayers=4):
    nc = bass.Bass("TRN2", target_bir_lowering=False)
    cT = nc.dram_tensor("cT", [D, 3], F32, kind="ExternalInput").ap()
    mw = nc.dram_tensor("mw", [nlayers, D, ncols], F32, kind="ExternalInput").ap()
    mb = nc.dram_tensor("mb", [nlayers, 128, ncols // 128], F32, kind="ExternalInput").ap()
    out = nc.dram_tensor("out", [nlayers, ncols, 3], F32, kind="ExternalOutput").ap()
    fw = FW(nc)
    o = OPS(fw)
    nt = ncols // 128
    ct = fw.sb("ct", [128, 8, 3], F32)
    sc = fw.sb("sc", [128, 8, 3], F32)
    fw.dma("sync", ct[:], cT.rearrange("(k p) g -> p k g", p=128), writes=[ct])
    o.act(sc, sc[:], ct, ct[:], AF.Silu)
    wpool = Pool(fw, "w", [128, 8, ncols], F32, 2)
    bpool = Pool(fw, "b", [128, nt], F32, 2)
    opool = Pool(fw, "o", [128, nt, 3], F32, 2)
    pp = Pool(fw, "pp", [128, 512], F32, 2, space="ps")
    evs = []
    for l in range(nlayers):
        w = wpool.next()
        for k in range(8):
            fw.dma("sync" if k % 2 == 0 else "gpsimd", w[:, k, :], mw[l, k * 128:(k + 1) * 128, :], writes=[w])
        b = bpool.next()
        fw.dma("sync", b[:], mb[l], writes=[b])
        ot = opool.next()
        for n in range(nt):
            p = pp.next()
            for k in range(8):
                o.mm(p, p[:, 0:3], w, w[:, k, n * 128:(n + 1) * 128], sc, sc[:, k, :], start=(k == 0), stop=(k == 7))
            o.ts("vector", ot, ot[:, n, :], p, p[:, 0:3], b[:, n:n + 1], None, ALU.add, extra_r=[b])
        evs.append(fw.dma("gpsimd", out[l].rearrange("(n p) g -> p n g", p=128), ot[:], reads=[ot]))
    fw.finish(evs)
    return nc


def build_p1(TL, TC=256, NCH=512):
    T = TL + TC
    nc = bass.Bass("TRN2", target_bir_lowering=False)
    xT = nc.dram_tensor("xT", [D, TL + 16], F32, kind="ExternalInput").ap()
    hT = nc.dram_tensor("hT", [D, TC + 16], F32, kind="ExternalInput").ap()
    pvec_d = nc.dram_tensor("pvec", [128, NPV], F32, kind="ExternalInput").ap()
    win_d = nc.dram_tensor("w_in", [D, INW], F32, kind="ExternalInput").ap()
    ropeC_d = nc.dram_tensor("ropeC", [128, TL], F32, kind="ExternalInput").ap()
    ropeS_d = nc.dram_tensor("ropeS", [128, TL], F32, kind="ExternalInput").ap()
    cm_d = nc.dram_tensor("cmats", [128, 4, 128], F32, kind="ExternalInput").ap()
    plin_d = nc.dram_tensor("plin", [128, 2, 128], F32, kind="ExternalInput").ap()
    invc_d = nc.dram_tensor("invc", [128, 2, 2, 16], F32, kind="ExternalInput").ap()
    ypool_d = nc.dram_tensor("ypool", [256, T], BF16, kind="ExternalOutput").ap()
    x0c_d = nc.dram_tensor("x0c", [256, T], BF16, kind="ExternalOutput").ap()
    v2_d = nc.dram_tensor("v2", [256, T], F32, kind="ExternalOutput").ap()
    q_d = nc.dram_tensor("qT", [512, T], BF16, kind="ExternalOutput").ap()
    k_d = nc.dram_tensor("kT", [512, T], BF16, kind="ExternalOutput").ap()
    v_d = nc.dram_tensor("v", [T, 512], BF16, kind="ExternalOutput").ap()

    fw = FW(nc)
    o = OPS(fw)
    NW = NCH + 16
    pvec = fw.sb("pvec", [128, NPV], F32)
    fw.dma("sync", pvec[:], pvec_d, writes=[pvec])
    def pv(name, i=0):
        c0, n = PV[name]
        return pvec[:, c0 + i:c0 + i + 1]
    cm32 = fw.sb("cm32", [128, 4, 128], F32)
    fw.dma("sync", cm32[:], cm_d, writes=[cm32])
    cmb = fw.sb("cmb", [128, 4, 128], BF16)
    o.cp("vector", cmb, cmb[:], cm32, cm32[:])
    pl32 = fw.sb("pl32", [128, 2, 128], F32)
    fw.dma("sync", pl32[:], plin_d, writes=[pl32])
    plb = fw.sb("plb", [128, 2, 128], BF16)
    o.cp("vector", plb, plb[:], pl32, pl32[:])
    invc = fw.sb("invc", [128, 2, 2, 16], F32)
    fw.dma("sync", invc[:], invc_d, writes=[invc])
    Amod = fw.sb("Amod", [128, 2, 8], F32)
    for si, nm in enumerate(["sc_l", "sc_c"]):
        c0, _ = PV[nm]
        g0, _ = PV["g1"]
        o.stt(Amod, Amod[:, si, :], pvec, pvec[:, c0:c0 + 8], 1.0, pvec, pvec[:, g0:g0 + 8], ALU.add, ALU.mult)
    wb = fw.sb("wb", [128, 8, INW], BF16)
    stg = Pool(fw, "wstg", [128, INW // 2], F32, 2)
    for k in range(8):
        for hf in range(2):
            s = stg.next()
            c0_ = hf * (INW // 2)
            fw.dma("sync" if hf == 0 else "gpsimd", s[:], win_d[k * 128:(k + 1) * 128, c0_:c0_ + INW // 2], writes=[s])
            o.cp("gpsimd" if hf == 0 else "vector", wb, wb[:, k, c0_:c0_ + INW // 2], s, s[:])

    xin = Pool(fw, "xin", [128, 8, NW], F32, 2)
    sqp = Pool(fw, "sq", [128, NW], BF16, 2)
    rstdp = Pool(fw, "rstd", [128, NW], F32, 2)
    up = Pool(fw, "u", [128, 8, NW], BF16, 2)
    tmpn = Pool(fw, "tmpn", [128, NW], F32, 2)
    zp = Pool(fw, "z", [128, NW], F32, 3)
    psA = Pool(fw, "psA", [128, 1024], F32, 2, space="ps")
    psB = Pool(fw, "psB", [128, 512], F32, 3, space="ps")
    sA = Pool(fw, "sA", [128, NW], F32, 2)
    sB = Pool(fw, "sB", [128, NW], F32, 2)
    dpool = Pool(fw, "dpl", [128, NCH], BF16, 2)
    ob16 = Pool(fw, "ob16", [128, NCH], BF16, 4)
    of32 = Pool(fw, "of32", [128, NCH], F32, 3)
    cvp = Pool(fw, "cv", [128, NCH], F32, 4)
    ropeCp = Pool(fw, "rC", [128, NCH], F32, 2)
    ropeSp = Pool(fw, "rS", [128, NCH], F32, 2)
    qf = Pool(fw, "qf", [128, NCH], F32, 2)
    qsq = Pool(fw, "qsq", [128, NCH], BF16, 2)
    qr = Pool(fw, "qr", [128, NCH], F32, 2)
    qn = Pool(fw, "qn", [128, NCH], BF16, 2)
    t1p = Pool(fw, "t1", [128, NCH], F32, 2)
    t2p = Pool(fw, "t2", [128, NCH], F32, 2)
    vout = Pool(fw, "vout", [128, 512], BF16, 2)
    out_evs = []
    stq = ["gpsimd"]

    def store(dst_ap, buf, ap):
        out_evs.append(fw.dma("gpsimd", dst_ap, ap, reads=[buf]))

    def chunk(seg, src, c0, N, first, last, col0, rope_c0):
        W = N + 16
        x = xin.next()
        for k in range(8):
            fw.dma("sync", x[:, k, 0:W], src[k * 128:(k + 1) * 128, c0:c0 + W], writes=[x])
        pst = psA.next()
        for k in range(8):
            sq = sqp.next()
            o.act(sq, sq[:, 0:W], x, x[:, k, 0:W], AF.Square)
            a = min(W, 512)
            o.mm(pst, pst[:, 0:a], cmb, cmb[:, 0, :], sq, sq[:, 0:a], start=(k == 0), stop=(k == 7))
            if W > 512:
                o.mm(pst, pst[:, 512:W], cmb, cmb[:, 0, :], sq, sq[:, 512:W], start=(k == 0), stop=(k == 7))
        rstd = rstdp.next()
        o.act(rstd, rstd[:, 0:W], pst, pst[:, 0:W], AF.Sqrt, bias=pv("eps"), scale=1.0, extra_r=[pvec])
        o.recip(rstd, rstd[:, 0:W], rstd, rstd[:, 0:W])
        u = up.next()
        shn = "sh_l" if seg == 0 else "sh_c"
        for k in range(8):
            t = tmpn.next()
            o.stt(t, t[:, 0:W], x, x[:, k, 0:W], Amod[:, seg, k:k + 1], rstd, rstd[:, 0:W], ALU.mult, ALU.mult,
                  extra_r=[Amod])
            o.act(u, u[:, k, 0:W], t, t[:, 0:W], AF.Identity, bias=pv(shn, k), scale=1.0, extra_r=[pvec])

        def proj(n, c_lo, c_hi):
            p = psA.next()
            w = c_hi - c_lo
            for k in range(8):
                a = min(w, 512)
                o.mm(p, p[:, 0:a], wb, wb[:, k, n * 128:(n + 1) * 128], u, u[:, k, c_lo:c_lo + a],
                     start=(k == 0), stop=(k == 7))
                if w > 512:
                    o.mm(p, p[:, 512:512 + w - 512], wb, wb[:, k, n * 128:(n + 1) * 128], u,
                         u[:, k, c_lo + 512:c_hi], start=(k == 0), stop=(k == 7))
            return p

        def evac_masked(n):
            p = proj(n, 0, W)
            z = zp.next()
            o.act(z, z[:, 0:W], p, p[:, 0:W], AF.Copy)
            if first:
                o.ts("vector", z, z[:, 0:8], z, z[:, 0:8], pv("mL") if seg == 0 else pv("zero"), None, ALU.mult,
                     extra_r=[pvec])
            if last:
                o.ts("vector", z, z[:, N + 8:W], z, z[:, N + 8:W], pv("mR") if seg == 0 else pv("zero"), None,
                     ALU.mult, extra_r=[pvec])
            return z

        for pt in range(2):
            z = evac_masked(pt)
            a = sA.next()
            b = sB.next()
            o.tt("vector", a, a[:, 1:W], z, z[:, 0:W - 1], z, z[:, 1:W], ALU.add)
            o.tt("vector", b, b[:, 2:W - 1], a, a[:, 1:W - 2], a, a[:, 3:W], ALU.add)
            if pt == 0:
                lo, hi, wl, wh = a, b, 2.0, 4.0
            else:
                a2 = sA.next()
                o.tt("vector", a2, a2[:, 4:W - 3], b, b[:, 2:W - 5], b, b[:, 6:W - 1], ALU.add)
                b2 = sB.next()
                o.tt("vector", b2, b2[:, 8:W - 7], a2, a2[:, 4:W - 11], a2, a2[:, 12:W - 3], ALU.add)
                lo, hi, wl, wh = a2, b2, 8.0, 16.0
            d = dpool.next()
            dd = of32.next()
            o.stt(dd, dd[0:64, 0:N], lo, lo[0:64, 8:N + 8], 1.0 / wl, z, z[0:64, 8:N + 8], ALU.mult, ALU.subtract)
            o.stt(dd, dd[64:128, 0:N], hi, hi[64:128, 8:N + 8], 1.0 / wh, z, z[64:128, 8:N + 8], ALU.mult,
                  ALU.subtract)
            for (flag, cs, ic0) in ((first, 0, 0), (last, N - 8, 8)):
                if not flag:
                    continue
                for (src_b, r0, r1) in ((lo, 0, 64), (hi, 64, 128)):
                    o.tt("vector", dd, dd[r0:r1, cs:cs + 8], src_b, src_b[r0:r1, cs + 8:cs + 16], invc,
                         invc[r0:r1, seg, pt, ic0:ic0 + 8], ALU.mult)
                    o.tt("vector", dd, dd[r0:r1, cs:cs + 8], dd, dd[r0:r1, cs:cs + 8], z, z[r0:r1, cs + 8:cs + 16],
                         ALU.subtract)
            o.cp("gpsimd", d, d[:, 0:N], dd, dd[:, 0:N])
            pp = psB.next()
            o.mm(pp, pp[:, 0:N], plb, plb[:, pt, :], d, d[:, 0:N])
            yb = ob16.next()
            o.act(yb, yb[:, 0:N], pp, pp[:, 0:N], AF.Identity, bias=pv("zero"), scale=pv("psc", pt), extra_r=[pvec])
            store(ypool_d[pt * 128:(pt + 1) * 128, col0:col0 + N], yb, yb[:, 0:N])

        conv = {}
        for ht in range(6):
            z = evac_masked(2 + ht)
            c = cvp.next()
            cw0, _ = PV["cw"]
            o.act(c, c[:, 0:N], z, z[:, 7:N + 7], AF.Identity, bias=pv("cb", ht), scale=pv("cw", ht * 3 + 0),
                  extra_r=[pvec])
            o.stt(c, c[:, 0:N], z, z[:, 8:N + 8], pv("cw", ht * 3 + 1), c, c[:, 0:N], ALU.mult, ALU.add,
                  extra_r=[pvec])
            o.stt(c, c[:, 0:N], z, z[:, 9:N + 9], pv("cw", ht * 3 + 2), c, c[:, 0:N], ALU.mult, ALU.add,
                  extra_r=[pvec])
            if ht < 2:
                xb = ob16.next()
                o.cp("gpsimd", xb, xb[:, 0:N], c, c[:, 0:N])
                store(x0c_d[ht * 128:(ht + 1) * 128, col0:col0 + N], xb, xb[:, 0:N])
            elif ht < 4:
                conv[ht] = c
            else:
                x1 = conv[ht - 2]
                vv = of32.next()
                o.tt("vector", vv, vv[:, 0:N], c, c[:, 0:N], x1, x1[:, 0:N], ALU.mult)
                store(v2_d[(ht - 4) * 128:(ht - 3) * 128, col0:col0 + N], vv, vv[:, 0:N])

        if seg == 0:
            rc = ropeCp.next()
            rs = ropeSp.next()
            fw.dma("sync", rc[:, 0:N], ropeC_d[:, rope_c0:rope_c0 + N], writes=[rc])
            fw.dma("sync", rs[:, 0:N], ropeS_d[:, rope_c0:rope_c0 + N], writes=[rs])
        for qt in range(8):
            p = proj(8 + qt, 8, N + 8)
            f = qf.next()
            o.act(f, f[:, 0:N], p, p[:, 0:N], AF.Copy)
            s = qsq.next()
            o.act(s, s[:, 0:N], p, p[:, 0:N], AF.Square)
            pss = psB.next()
            o.mm(pss, pss[:, 0:N], cmb, cmb[:, 1, :], s, s[:, 0:N])
            r = qr.next()
            o.act(r, r[:, 0:N], pss, pss[:, 0:N], AF.Sqrt, bias=pv("eps"), scale=1.0, extra_r=[pvec])
            o.recip(r, r[:, 0:N], r, r[:, 0:N])
            gname = "gq" if qt < 4 else "gk"
            dst = q_d if qt < 4 else k_d
            hh = qt % 4
            if seg == 0:
                n_ = qn.next()
                o.stt(n_, n_[:, 0:N], f, f[:, 0:N], pv(gname), r, r[:, 0:N], ALU.mult, ALU.mult, extra_r=[pvec])
                pp2 = psB.next()
                o.mm(pp2, pp2[:, 0:N], cmb, cmb[:, 2, :], n_, n_[:, 0:N])
                t1 = t1p.next()
                o.tt("gpsimd", t1, t1[:, 0:N], n_, n_[:, 0:N], rc, rc[:, 0:N], ALU.mult)
                t2 = t2p.next()
                o.tt("vector", t2, t2[:, 0:N], pp2, pp2[:, 0:N], rs, rs[:, 0:N], ALU.mult)
                qo = ob16.next()
                o.tt("gpsimd", qo, qo[:, 0:N], t1, t1[:, 0:N], t2, t2[:, 0:N], ALU.add)
            else:
                qo = ob16.next()
                o.stt(qo, qo[:, 0:N], f, f[:, 0:N], pv(gname), r, r[:, 0:N], ALU.mult, ALU.mult, extra_r=[pvec])
            store(dst[hh * 128:(hh + 1) * 128, col0:col0 + N], qo, qo[:, 0:N])

        for tb in range(N // 128):
            p = psB.next()
            for k in range(8):
                o.mm(p, p[:, :], u, u[:, k, 8 + tb * 128:8 + (tb + 1) * 128], wb, wb[:, k, 2048:2560],
                     start=(k == 0), stop=(k == 7))
            vb = vout.next()
            o.act(vb, vb[:, :], p, p[:, :], AF.Copy)
            store(v_d[col0 + tb * 128:col0 + (tb + 1) * 128, :], vb, vb[:, :])

    nlc = TL // NCH
    for ci in range(nlc):
        chunk(0, xT, ci * NCH, NCH, ci == 0, ci == nlc - 1, ci * NCH, ci * NCH)
    chunk(1, hT, 0, TC, True, True, TL, 0)
    fw.finish(out_evs)
    return nc


NPV2 = 5


def build_p2(L, C=256, with_ctx=True, NQ=512):
    nc = bass.Bass("TRN2", target_bir_lowering=False)
    LQ = L + (C if with_ctx else 0)
    LK = C + L
    nkt = LK // 128
    q_d = nc.dram_tensor("qT", [128, LQ], BF16, kind="ExternalInput").ap()
    k_d = nc.dram_tensor("kT", [128, LK], BF16, kind="ExternalInput").ap()
    v_d = nc.dram_tensor("v", [LK, 128], BF16, kind="ExternalInput").ap()
    dl_d = nc.dram_tensor("dlam", [128, 4, 64], F32, kind="ExternalInput").ap()
    pv_d = nc.dram_tensor("pvec", [128, NPV2], F32, kind="ExternalInput").ap()
    id_d = nc.dram_tensor("ident", [128, 128], F32, kind="ExternalInput").ap()
    out_d = nc.dram_tensor("attT", [128, LQ], BF16, kind="ExternalOutput").ap()
    fw = FW(nc)
    o = OPS(fw)
    pvec = fw.sb("pvec", [128, NPV2], F32)
    fw.dma("sync", pvec[:], pv_d, writes=[pvec])
    id32 = fw.sb("id32", [128, 128], F32)
    fw.dma("sync", id32[:], id_d, writes=[id32])
    idb = fw.sb("idb", [128, 128], BF16)
    o.cp("vector", idb, idb[:], id32, id32[:])
    dl = fw.sb("dl", [128, 4, 64], F32)
    fw.dma("sync", dl[:], dl_d, writes=[dl])
    pr = fw.sb("pr", [128, 2, 64], F32)
    o.tt("vector", pr, pr[:, 0, :], dl, dl[:, 0, :], dl, dl[:, 1, :], ALU.mult)
    o.tt("vector", pr, pr[:, 1, :], dl, dl[:, 2, :], dl, dl[:, 3, :], ALU.mult)
    sm = fw.sb("sm", [128, 2], F32)
    fw.op("vector", lambda e: e.reduce_sum(out=sm[:], in_=pr[:], axis=AX.X), [pr], [sm])
    ex = fw.sb("ex", [128, 2], F32)
    o.act(ex, ex[:], sm, sm[:], AF.Exp)
    neglam = fw.sb("neglam", [128, 1], F32)
    o.tt("vector", neglam, neglam[:], ex, ex[:, 1:2], ex, ex[:, 0:1], ALU.subtract)
    o.tt("vector", neglam, neglam[:], neglam, neglam[:], pvec, pvec[:, 1:2], ALU.subtract)
    gsc = fw.sb("gsc", [128, 1], F32)
    o.tt("vector", gsc, gsc[:], pvec, pvec[:, 2:3], pvec, pvec[:, 3:4], ALU.mult)

    kT = fw.sb("kT", [128, LK], BF16)
    step = 2048
    for c0 in range(0, LK, step):
        c1 = min(LK, c0 + step)
        fw.dma("sync", kT[:, c0:c1], k_d[:, c0:c1], writes=[kT])
    vt = fw.sb("vt", [128, nkt, 129], BF16)
    o.memset("gpsimd", vt, vt[:, :, 128:129], 1.0)
    vsrc = v_d.rearrange("(n p) d -> p n d", p=128)
    for n0 in range(0, nkt, 16):
        n1 = min(nkt, n0 + 16)
        fw.dma("gpsimd", vt[:, n0:n1, 0:128], vsrc[:, n0:n1, :], writes=[vt])

    qp = Pool(fw, "q", [128, NQ], BF16, 2)
    psS = Pool(fw, "psS", [128, 512], F32, 4, space="ps")
    accb = [fw.ps("acc%d" % i, [128, 512], F32) for i in range(3)]
    trb = fw.ps("trb", [128, 512], BF16)
    Pp = Pool(fw, "P", [128, NQ], BF16, 4)
    small = Pool(fw, "small", [128, 8], F32, 4)
    o1p = Pool(fw, "o1", [128, 128], F32, 2)
    o2p = Pool(fw, "o2", [128, 128], F32, 2)
    junk = Pool(fw, "junk", [128, 128], F32, 2)
    onp = Pool(fw, "on", [128, 128], BF16, 2)
    outp = Pool(fw, "outp", [128, NQ], BF16, 2)
    out_evs = []

    def qchunk(q0, N, kts):
        q = qp.next()
        fw.dma("sync", q[:, 0:N], q_d[:, q0:q0 + N], writes=[q])
        nqb = N // 128
        started = set()
        for ki, kt in enumerate(kts):
            for m in range(2):
                ps = psS.next()
                o.mm(ps, ps[:, 0:N], kT, kT[64 * m:64 * m + 64, kt * 128:(kt + 1) * 128], q, q[64 * m:64 * m + 64, 0:N])
                P = Pp.next()
                o.act(P, P[:, 0:N], ps, ps[:, 0:N], AF.Exp, scale=0.125)
                for qb in range(nqb):
                    a = m * 4 + qb
                    bank, slot = a // 3, a % 3
                    ab = accb[bank]
                    st = bank not in started
                    started.add(bank)
                    o.mm(ab, ab[:, slot * 129:(slot + 1) * 129], P, P[:, qb * 128:(qb + 1) * 128], vt, vt[:, kt, :],
                         start=st, stop=(ki == len(kts) - 1), skip_group_check=True)
        for qb in range(nqb):
            a0, a1 = qb, 4 + qb
            A0 = accb[a0 // 3]; s0 = (a0 % 3) * 129
            A1 = accb[a1 // 3]; s1 = (a1 % 3) * 129
            sm_ = small.next()
            o.recip(sm_, sm_[:, 0:1], A0, A0[:, s0 + 128:s0 + 129])
            o.recip(sm_, sm_[:, 1:2], A1, A1[:, s1 + 128:s1 + 129])
            o.tt("vector", sm_, sm_[:, 2:3], sm_, sm_[:, 1:2], neglam, neglam[:], ALU.mult)
            o1 = o1p.next()
            o.ts("vector", o1, o1[:], A0, A0[:, s0:s0 + 128], sm_[:, 0:1], None, ALU.mult, extra_r=[sm_])
            o2 = o2p.next()
            o.stt(o2, o2[:], A1, A1[:, s1:s1 + 128], sm_[:, 2:3], o1, o1[:], ALU.mult, ALU.add, extra_r=[sm_])
            jk = junk.next()
            o.act(jk, jk[:], o2, o2[:], AF.Square, accum=(sm_, sm_[:, 3:4]))
            o.act(sm_, sm_[:, 4:5], sm_, sm_[:, 3:4], AF.Sqrt, bias=pvec[:, 0:1], scale=1.0 / 128.0, extra_r=[pvec])
            o.recip(sm_, sm_[:, 5:6], sm_, sm_[:, 4:5])
            on = onp.next()
            o.ts("vector", on, on[:], o2, o2[:], sm_[:, 5:6], None, ALU.mult, extra_r=[sm_])
            o.tr(trb, trb[:, qb * 128:(qb + 1) * 128], on, on[:], idb, idb[:])
        ot = outp.next()
        o.act(ot, ot[:, 0:N], trb, trb[:, 0:N], AF.Identity, bias=pvec[:, 4:5], scale=gsc[:, 0:1], extra_r=[pvec, gsc])
        out_evs.append(fw.dma("gpsimd", out_d[:, q0:q0 + N], ot[:, 0:N], reads=[ot]))

    allk = list(range(nkt))
    for qc in range(L // NQ):
        qchunk(qc * NQ, NQ, allk)
    if with_ctx:
        qchunk(L, C, list(range(C // 128)))
    fw.finish(out_evs)
    return nc


PI = float(np.pi)


def lc_tables(L):
    M = L // 128
    N2 = 2 * M
    N = 2 * L
    kb = min(128, N2)
    nb2 = (N2 + 127) // 128
    f64 = np.float64
    n2 = np.arange(M, dtype=f64)[:, None]; k2 = np.arange(N2, dtype=f64)[None, :]
    ang = 2 * np.pi * n2 * k2 / N2
    F2 = np.concatenate([np.cos(ang), -np.sin(ang)], 1)
    n1 = np.arange(128, dtype=f64)[:, None]
    ang = 2 * np.pi * n1 * k2 / N
    Tr, Ti = np.cos(ang), -np.sin(ang)
    TA = np.concatenate([Tr, Tr], 1); TB = np.concatenate([Ti, Ti], 1)
    k1 = np.arange(128, dtype=f64)[None, :]
    ang = 2 * np.pi * n1 * k1 / 128
    Fr, Fi = np.cos(ang), -np.sin(ang)
    F1 = np.stack([Fr, Fi, -Fi], 1)
    Gr, Gi = np.cos(ang), np.sin(ang)
    G1 = np.stack([np.concatenate([Gr, Gi], 1), np.concatenate([-Gi, Gr], 1)], 1)
    T2 = np.zeros((kb, nb2, 2, 256)); G2 = np.zeros((kb, nb2, 2, M))
    for j in range(nb2):
        kk = (np.arange(kb, dtype=f64) + j * 128)[:, None]
        ang = 2 * np.pi * kk * np.arange(128, dtype=f64)[None, :] / N
        T2[:, j, 0] = np.concatenate([np.cos(ang), np.cos(ang)], 1)
        T2[:, j, 1] = np.concatenate([np.sin(ang), np.sin(ang)], 1)
        ang = 2 * np.pi * kk * np.arange(M, dtype=f64)[None, :] / N2
        G2[:, j, 0] = np.cos(ang) / N
        G2[:, j, 1] = -np.sin(ang) / N
    f = lambda a: np.ascontiguousarray(a.astype(np.float32))
    return {"F2": f(F2), "TA": f(TA), "TB": f(TB), "F1": f(F1), "G1": f(G1), "T2": f(T2), "G2": f(G2)}


def filt_tables(L, ch0, nch=32):
    f32 = np.float32
    t = np.linspace(0.0, 1.0, L, dtype=f32)[:, None]
    w = (f32(2.0 * np.pi / L) * np.arange(L, dtype=f32))[:, None]
    bands = np.linspace(1e-4, 15, 16, dtype=f32)[None, :]
    feat = np.concatenate([t, np.cos(bands * w), -np.sin(bands * w)], -1).astype(f32)
    max_decay = np.log(1.0 / 1e-2) / 0.3
    min_decay = np.log(1.0 / 1e-2) / 1.5
    deltas = np.linspace(min_decay, max_decay, 256, dtype=f32)
    dec = np.exp(-t * deltas[None, ch0:ch0 + nch]).astype(f32)
    dec2 = np.concatenate([dec, dec], 1).T
    return np.ascontiguousarray(feat.T), np.ascontiguousarray(dec2)


def build_p3(Ls=(16384, 256), nch=32):
    nc = bass.Bass("TRN2", target_bir_lowering=False)
    fw = FW(nc)
    o = OPS(fw)
    R = 2 * nch
    w1_d = nc.dram_tensor("w1", [33, 64], F32, kind="ExternalInput").ap()
    w2_d = nc.dram_tensor("w2", [64, 64], F32, kind="ExternalInput").ap()
    w3_d = nc.dram_tensor("w3s", [64, R], F32, kind="ExternalInput").ap()
    fpv_d = nc.dram_tensor("fpv", [64, 4], F32, kind="ExternalInput").ap()
    sel_d = nc.dram_tensor("sel", [R, nch], F32, kind="ExternalInput").ap()
    w1 = fw.sb("w1", [33, 64], F32); w2 = fw.sb("w2", [64, 64], F32); w3s = fw.sb("w3s", [64, R], F32)
    fpv = fw.sb("fpv", [64, 6], F32); sel = fw.sb("sel", [R, nch], F32)
    fw.dma("sync", w1[:], w1_d, writes=[w1]); fw.dma("sync", w2[:], w2_d, writes=[w2])
    fw.dma("sync", w3s[:], w3_d, writes=[w3s]); fw.dma("sync", fpv[:, 0:4], fpv_d, writes=[fpv])
    fw.dma("sync", sel[:], sel_d, writes=[sel])
    o.tt("vector", fpv, fpv[:, 4:5], fpv, fpv[:, 0:1], fpv, fpv[:, 1:2], ALU.mult)
    o.tt("vector", fpv, fpv[:, 5:6], fpv, fpv[:, 2:3], fpv, fpv[:, 3:4], ALU.mult)
    ones = fw.sb("ones", [128, 128], F32)
    o.memset("vector", ones, ones[:], 1.0)

    PS = Pool(fw, "PS", [128, 512], F32, 6, space="ps")
    featp = Pool(fw, "feat", [33, 512], F32, 2)
    decp = Pool(fw, "dec", [R, 512], F32, 2)
    prep = Pool(fw, "pre", [64, 512], F32, 2)
    mkp = Pool(fw, "mk", [64, 512], F32, 2)
    hp = Pool(fw, "hh", [64, 512], F32, 3)
    kp = Pool(fw, "kk", [R, 512], F32, 2)
    jkp = Pool(fw, "jk", [R, 512], F32, 2)
    xin = Pool(fw, "xin", [128, 128], F32, 6)
    sP = Pool(fw, "sP", [128, 512], F32, 3)
    sA = Pool(fw, "sAA", [128, 512], F32, 3)
    sB = Pool(fw, "sBB", [128, 512], F32, 3)
    sZ = Pool(fw, "sZ", [128, 512], F32, 3)
    sK = Pool(fw, "sK", [128, 2, 512], F32, 2)
    sXa = Pool(fw, "sXa", [128, 512], F32, 2)
    sQ = Pool(fw, "sQ", [128, 256], F32, 3)
    sQA = Pool(fw, "sQA", [128, 256], F32, 3)
    sQB = Pool(fw, "sQB", [128, 256], F32, 3)
    sZ2 = Pool(fw, "sZ2", [128, 2, 256], F32, 2)
    sY = Pool(fw, "sY", [128, 128], F32, 3)
    out_evs = []

    for L in Ls:
        sfx = "_%d" % L
        M = L // 128
        N2 = 2 * M
        W2 = 2 * N2
        kb = min(128, N2)
        nb2 = (N2 + 127) // 128
        d = {}
        for nm, shp in (("F2", [M, W2]), ("TA", [128, W2]), ("TB", [128, W2]), ("F1", [128, 3, 128]),
                        ("G1", [128, 2, 256]), ("T2", [kb, nb2, 2, 256]), ("G2", [kb, nb2, 2, M])):
            d[nm] = nc.dram_tensor(nm + sfx, shp, F32, kind="ExternalInput").ap()
        feat_d = nc.dram_tensor("feat" + sfx, [33, L], F32, kind="ExternalInput").ap()
        dec_d = nc.dram_tensor("dec" + sfx, [R, L], F32, kind="ExternalInput").ap()
        v_d = nc.dram_tensor("vin" + sfx, [R, L], F32, kind="ExternalInput").ap()
        y_d = nc.dram_tensor("yout" + sfx, [R, L], F32, kind="ExternalOutput").ap()
        kf = fw.dram("kf" + sfx, [R, L], F32)
        tb = {}
        for nm in d:
            tb[nm] = fw.sb("t" + nm + sfx, list(d[nm].shape), F32)
            fw.dma("sync", tb[nm][:], d[nm], writes=[tb[nm]])
        CH = min(512, L)
        nchk = L // CH
        rs = fw.sb("rs" + sfx, [R, nchk], F32)

        def sin_layer(ps, fcol, bcol):
            pre = prep.next()
            o.ts("vector", pre, pre[:, 0:CH], ps, ps[0:64, 0:CH], fpv[:, fcol:fcol + 1], fpv[:, bcol:bcol + 1],
                 ALU.mult, ALU.add, extra_r=[fpv])
            mk = mkp.next()
            o.ts("gpsimd", mk, mk[:, 0:CH], pre, pre[:, 0:CH], PI, None, ALU.is_gt)
            o.stt(pre, pre[:, 0:CH], mk, mk[:, 0:CH], -2.0 * PI, pre, pre[:, 0:CH], ALU.mult, ALU.add)
            mk2 = mkp.next()
            o.ts("gpsimd", mk2, mk2[:, 0:CH], pre, pre[:, 0:CH], -PI, None, ALU.is_lt)
            o.stt(pre, pre[:, 0:CH], mk2, mk2[:, 0:CH], 2.0 * PI, pre, pre[:, 0:CH], ALU.mult, ALU.add)
            h = hp.next()
            o.act(h, h[:, 0:CH], pre, pre[:, 0:CH], AF.Sin)
            return h

        for ci in range(nchk):
            c0 = ci * CH
            ft = featp.next()
            fw.dma("sync", ft[:, 0:CH], feat_d[:, c0:c0 + CH], writes=[ft])
            dc = decp.next()
            fw.dma("sync", dc[:, 0:CH], dec_d[:, c0:c0 + CH], writes=[dc])
            ps = PS.next()
            o.mm(ps, ps[0:64, 0:CH], w1, w1[:, :], ft, ft[:, 0:CH])
            h1 = sin_layer(ps, 0, 4)
            ps = PS.next()
            o.mm(ps, ps[0:64, 0:CH], w2, w2[:, :], h1, h1[:, 0:CH])
            h2 = sin_layer(ps, 2, 5)
            ps = PS.next()
            o.mm(ps, ps[0:R, 0:CH], w3s, w3s[:, :], h2, h2[:, 0:CH])
            k = kp.next()
            o.tt("vector", k, k[:, 0:CH], ps, ps[0:R, 0:CH], dc, dc[:, 0:CH], ALU.mult)
            if ci == 0:
                o.memset("vector", k, k[nch:R, 0:1], 0.0)
            j = jkp.next()
            o.act(j, j[:, 0:CH], k, k[:, 0:CH], AF.Abs, accum=(rs, rs[:, ci:ci + 1]))
            fw.dma("gpsimd", kf[:, c0:c0 + CH], k[:, 0:CH], reads=[k], writes=[kf])
        rtot = fw.sb("rtot" + sfx, [R, 1], F32)
        fw.op("vector", lambda e, rtot=rtot, rs=rs: e.reduce_sum(out=rtot[:], in_=rs[:], axis=AX.X), [rs], [rtot])
        Rb = fw.sb("Rb" + sfx, [R, 128], F32)
        o.ts("vector", Rb, Rb[:], ones, ones[0:R, :], rtot[:, 0:1], None, ALU.mult, extra_r=[rtot])
        pss = PS.next()
        o.mm(pss, pss[:, 0:nch], Rb, Rb[:, :], sel, sel[:, :])
        Sinv = fw.sb("Sinv" + sfx, [128, 2, nch], F32)
        o.recip(Sinv, Sinv[:, 0, :], pss, pss[:, 0:nch])
        o.ts("vector", Sinv, Sinv[:, 1, :], Sinv, Sinv[:, 0, :], -1.0, None, ALU.mult)

        kfv = kf.t.ap().rearrange("r (a i) -> r a i", i=128)
        vv = v_d.rearrange("r (a i) -> r a i", i=128)
        yv = y_d.rearrange("r (a i) -> r a i", i=128)

        def fwd(src_ap, src_bufs):
            x = xin.next()
            fw.dma("sync", x[0:M, :], src_ap, reads=src_bufs, writes=[x])
            P = PS.next()
            o.mm(P, P[:, 0:W2], x, x[0:M, :], tb["F2"], tb["F2"][:, :])
            Ps = sP.next()
            o.act(Ps, Ps[:, 0:W2], P, P[:, 0:W2], AF.Copy)
            A = sA.next(); B = sB.next()
            o.tt("vector", A, A[:, 0:W2], Ps, Ps[:, 0:W2], tb["TA"], tb["TA"][:, :], ALU.mult)
            o.tt("gpsimd", B, B[:, 0:W2], Ps, Ps[:, 0:W2], tb["TB"], tb["TB"][:, :], ALU.mult)
            Z = sZ.next()
            o.tt("vector", Z, Z[:, 0:N2], A, A[:, 0:N2], B, B[:, N2:W2], ALU.subtract)
            o.tt("gpsimd", Z, Z[:, N2:W2], B, B[:, 0:N2], A, A[:, N2:W2], ALU.add)
            X = PS.next()
            F1 = tb["F1"]
            o.mm(X, X[:, 0:N2], F1, F1[:, 0, :], Z, Z[:, 0:N2], start=True, stop=False)
            o.mm(X, X[:, 0:N2], F1, F1[:, 2, :], Z, Z[:, N2:W2], start=False, stop=True)
            o.mm(X, X[:, N2:W2], F1, F1[:, 1, :], Z, Z[:, 0:N2], start=True, stop=False, skip_group_check=True)
            o.mm(X, X[:, N2:W2], F1, F1[:, 0, :], Z, Z[:, N2:W2], start=False, stop=True, skip_group_check=True)
            return X

        for c in range(nch):
            Xa = fwd(kfv[c], [kf])
            Xas = sXa.next()
            o.act(Xas, Xas[:, 0:W2], Xa, Xa[:, 0:W2], AF.Identity, bias=0.0, scale=Sinv[:, 0, c:c + 1], extra_r=[Sinv])
            Xb = fwd(kfv[nch + c], [kf])
            K = sK.next()
            o.stt(K, K[:, 0, 0:N2], Xb, Xb[:, 0:N2], Sinv[:, 0, c:c + 1], Xas, Xas[:, 0:N2], ALU.mult, ALU.add,
                  extra_r=[Sinv])
            o.stt(K, K[:, 1, 0:N2], Xb, Xb[:, N2:W2], Sinv[:, 1, c:c + 1], Xas, Xas[:, N2:W2], ALU.mult, ALU.add,
                  extra_r=[Sinv])
            o.cp("gpsimd", K, K[:, 0, N2:W2], K, K[:, 0, 0:N2])
            o.cp("gpsimd", K, K[:, 1, N2:W2], K, K[:, 1, 0:N2])
            for b in range(2):
                r = b * nch + c
                X = fwd(vv[r], [])
                Xs = sP.next()
                o.act(Xs, Xs[:, 0:W2], X, X[:, 0:W2], AF.Copy)
                A = sA.next(); B = sB.next()
                o.tt("vector", A, A[:, 0:W2], Xs, Xs[:, 0:W2], K, K[:, 0, 0:W2], ALU.mult)
                o.tt("gpsimd", B, B[:, 0:W2], Xs, Xs[:, 0:W2], K, K[:, 1, 0:W2], ALU.mult)
                Y = sZ.next()
                o.tt("vector", Y, Y[:, 0:N2], A, A[:, 0:N2], B, B[:, N2:W2], ALU.subtract)
                o.tt("gpsimd", Y, Y[:, N2:W2], B, B[:, 0:N2], A, A[:, N2:W2], ALU.add)
                Z2 = sZ2.next()
                G1 = tb["G1"]; T2 = tb["T2"]; G2 = tb["G2"]
                for j in range(nb2):
                    Q = PS.next()
                    o.mm(Q, Q[0:kb, 0:256], Y, Y[:, j * 128:j * 128 + kb], G1, G1[:, 0, :], start=True, stop=False)
                    o.mm(Q, Q[0:kb, 0:256], Y, Y[:, N2 + j * 128:N2 + j * 128 + kb], G1, G1[:, 1, :], start=False,
                         stop=True)
                    Qs = sQ.next()
                    o.act(Qs, Qs[0:kb, :], Q, Q[0:kb, 0:256], AF.Copy)
                    QA = sQA.next(); QB = sQB.next()
                    o.tt("vector", QA, QA[0:kb, :], Qs, Qs[0:kb, :], T2, T2[:, j, 0, :], ALU.mult)
                    o.tt("gpsimd", QB, QB[0:kb, :], Qs, Qs[0:kb, :], T2, T2[:, j, 1, :], ALU.mult)
                    o.tt("vector", Z2, Z2[0:kb, j, 0:128], QA, QA[0:kb, 0:128], QB, QB[0:kb, 128:256], ALU.subtract)
                    o.tt("gpsimd", Z2, Z2[0:kb, j, 128:256], QB, QB[0:kb, 0:128], QA, QA[0:kb, 128:256], ALU.add)
                yp = PS.next()
                for j in range(nb2):
                    o.mm(yp, yp[0:M, 0:128], G2, G2[:, j, 0, :], Z2, Z2[0:kb, j, 0:128], start=(j == 0), stop=False)
                    o.mm(yp, yp[0:M, 0:128], G2, G2[:, j, 1, :], Z2, Z2[0:kb, j, 128:256], start=False,
                         stop=(j == nb2 - 1))
                ys = sY.next()
                o.act(ys, ys[0:M, :], yp, yp[0:M, 0:128], AF.Copy)
                out_evs.append(fw.dma("gpsimd", yv[r], ys[0:M, :], reads=[ys]))
    fw.finish(out_evs)
    return nc


D = 1024
PV4 = {}
_c4 = 0
def _add4(name, n):
    global _c4
    PV4[name] = (_c4, n)
    _c4 += n
_add4("hyb", 2)
_add4("g2_l", 8); _add4("g2_c", 8)
_add4("n2g", 8)
_add4("sh_l", 8); _add4("sc_l", 8); _add4("sh_c", 8); _add4("sc_c", 8)
_add4("g5_l", 8); _add4("g5_c", 8)
_add4("eps", 1); _add4("zero", 1)
NPV4 = _c4


def build_p4(TL, TC=256, moe=False, NCH=512):
    T = TL + TC
    nc = bass.Bass("TRN2", target_bir_lowering=False)
    xT = nc.dram_tensor("xT", [D, T], F32, kind="ExternalInput").ap()
    ypool_d = nc.dram_tensor("ypool", [256, T], BF16, kind="ExternalInput").ap()
    x0c_d = nc.dram_tensor("x0c", [256, T], BF16, kind="ExternalInput").ap()
    v2_d = nc.dram_tensor("v2", [256, T], F32, kind="ExternalInput").ap()
    conv_d = nc.dram_tensor("conv", [256, T], F32, kind="ExternalInput").ap()
    att_d = nc.dram_tensor("attT", [512, T], BF16, kind="ExternalInput").ap()
    wout_d = nc.dram_tensor("w_out", [D, D], F32, kind="ExternalInput").ap()
    pvec_d = nc.dram_tensor("pvec", [128, NPV4], F32, kind="ExternalInput").ap()
    ones_d = nc.dram_tensor("onesD", [128, 128], F32, kind="ExternalInput").ap()
    x1_d = nc.dram_tensor("x1T", [D, T], F32, kind="ExternalOutput").ap()
    u2_d = nc.dram_tensor("u2T", [D, T], BF16, kind="ExternalOutput").ap()
    if moe:
        rw_d = nc.dram_tensor("rw", [128, 8, 8], F32, kind="ExternalInput").ap()
        id_d = nc.dram_tensor("ident", [128, 128], F32, kind="ExternalInput").ap()
        comb_d = nc.dram_tensor("combT", [8, T], F32, kind="ExternalOutput").ap()
    fw = FW(nc)
    o = OPS(fw)
    pvec = fw.sb("pvec", [128, NPV4], F32)
    fw.dma("sync", pvec[:], pvec_d, writes=[pvec])
    def pv(name, i=0):
        c0, n = PV4[name]
        return pvec[:, c0 + i:c0 + i + 1]
    on32 = fw.sb("on32", [128, 128], F32)
    fw.dma("sync", on32[:], ones_d, writes=[on32])
    onb = fw.sb("onb", [128, 128], BF16)
    o.cp("vector", onb, onb[:], on32, on32[:])
    Amod = fw.sb("Amod", [128, 2, 8], F32)
    for si, nm in enumerate(["sc_l", "sc_c"]):
        c0, _ = PV4[nm]
        g0, _ = PV4["n2g"]
        o.stt(Amod, Amod[:, si, :], pvec, pvec[:, c0:c0 + 8], 1.0, pvec, pvec[:, g0:g0 + 8], ALU.add, ALU.mult)
    wb = fw.sb("wb", [128, 8, D], BF16)
    stg = Pool(fw, "wstg", [128, D], F32, 2)
    for k in range(8):
        s = stg.next()
        fw.dma("sync" if k % 2 == 0 else "gpsimd", s[:], wout_d[k * 128:(k + 1) * 128, :], writes=[s])
        o.cp("gpsimd" if k % 2 == 0 else "vector", wb, wb[:, k, :], s, s[:])
    if moe:
        rw = fw.sb("rw", [128, 8, 8], F32)
        fw.dma("sync", rw[:], rw_d, writes=[rw])
        ident = fw.sb("ident", [128, 128], F32)
        fw.dma("sync", ident[:], id_d, writes=[ident])

    xin = Pool(fw, "xin", [128, 8, NCH], F32, 2)
    yin = Pool(fw, "yin", [128, 8, NCH], BF16, 2)
    x0p = Pool(fw, "x0", [128, 2, NCH], BF16, 2)
    v2p = Pool(fw, "v2", [128, 2, NCH], F32, 2)
    cvp = Pool(fw, "cv", [128, 2, NCH], F32, 2)
    tmp = Pool(fw, "tmp", [128, NCH], F32, 3)
    x1p = Pool(fw, "x1", [128, 8, NCH], F32, 2)
    sqp = Pool(fw, "sq", [128, NCH], BF16, 2)
    rstdp = Pool(fw, "rstd", [128, NCH], F32, 2)
    u2f = Pool(fw, "u2f", [128, 8, NCH], F32, 1 if moe else 1)
    u2b = Pool(fw, "u2b", [128, 8, NCH], BF16, 2)
    PS = Pool(fw, "PS", [128, 512], F32, 6, space="ps")
    sm = Pool(fw, "sm", [128, 64], F32, 4)
    cbo = Pool(fw, "cbo", [8, NCH], F32, 2)
    out_evs = []

    def chunk(seg, c0, N):
        x = xin.next()
        for k in range(8):
            fw.dma("sync", x[:, k, 0:N], xT[k * 128:(k + 1) * 128, c0:c0 + N], writes=[x])
        y = yin.next()
        for k in range(2):
            fw.dma("sync", y[:, k, 0:N], ypool_d[k * 128:(k + 1) * 128, c0:c0 + N], writes=[y])
        for k in range(4):
            fw.dma("sync", y[:, 4 + k, 0:N], att_d[k * 128:(k + 1) * 128, c0:c0 + N], writes=[y])
        x0 = x0p.next(); v2 = v2p.next(); cv = cvp.next()
        for k in range(2):
            fw.dma("gpsimd", x0[:, k, 0:N], x0c_d[k * 128:(k + 1) * 128, c0:c0 + N], writes=[x0])
            fw.dma("gpsimd", v2[:, k, 0:N], v2_d[k * 128:(k + 1) * 128, c0:c0 + N], writes=[v2])
            fw.dma("gpsimd", cv[:, k, 0:N], conv_d[k * 128:(k + 1) * 128, c0:c0 + N], writes=[cv])
        for k in range(2):
            t = tmp.next()
            o.stt(t, t[:, 0:N], v2, v2[:, k, 0:N], pv("hyb", k), cv, cv[:, k, 0:N], ALU.mult, ALU.add, extra_r=[pvec])
            o.tt("vector", y, y[:, 2 + k, 0:N], t, t[:, 0:N], x0, x0[:, k, 0:N], ALU.mult)
        x1 = x1p.next()
        gn = "g2_l" if seg == 0 else "g2_c"
        for n in range(8):
            p = PS.next()
            for k in range(8):
                o.mm(p, p[:, 0:N], wb, wb[:, k, n * 128:(n + 1) * 128], y, y[:, k, 0:N], start=(k == 0), stop=(k == 7))
            o.stt(x1, x1[:, n, 0:N], p, p[:, 0:N], pv(gn, n), x, x[:, n, 0:N], ALU.mult, ALU.add, extra_r=[pvec])
            out_evs.append(fw.dma("gpsimd", x1_d[n * 128:(n + 1) * 128, c0:c0 + N], x1[:, n, 0:N], reads=[x1]))
        pst = PS.next()
        for k in range(8):
            sq = sqp.next()
            o.act(sq, sq[:, 0:N], x1, x1[:, k, 0:N], AF.Square)
            o.mm(pst, pst[:, 0:N], onb, onb[:, :], sq, sq[:, 0:N], start=(k == 0), stop=(k == 7))
        rstd = rstdp.next()
        o.act(rstd, rstd[:, 0:N], pst, pst[:, 0:N], AF.Sqrt, bias=pv("eps"), scale=1.0, extra_r=[pvec])
        o.recip(rstd, rstd[:, 0:N], rstd, rstd[:, 0:N])
        uf = u2f.next(); ub = u2b.next()
        shn = "sh_l" if seg == 0 else "sh_c"
        for k in range(8):
            t = tmp.next()
            o.stt(t, t[:, 0:N], x1, x1[:, k, 0:N], Amod[:, seg, k:k + 1], rstd, rstd[:, 0:N], ALU.mult, ALU.mult,
                  extra_r=[Amod])
            if moe:
                o.act(uf, uf[:, k, 0:N], t, t[:, 0:N], AF.Identity, bias=pv(shn, k), scale=1.0, extra_r=[pvec])
                o.cp("gpsimd", ub, ub[:, k, 0:N], uf, uf[:, k, 0:N])
            else:
                o.act(ub, ub[:, k, 0:N], t, t[:, 0:N], AF.Identity, bias=pv(shn, k), scale=1.0, extra_r=[pvec])
            out_evs.append(fw.dma("gpsimd", u2_d[k * 128:(k + 1) * 128, c0:c0 + N], ub[:, k, 0:N], reads=[ub]))
        if moe:
            cps = PS.next()
            for tb in range(N // 128):
                p = PS.next()
                for k in range(8):
                    o.mm(p, p[:, 0:8], uf, uf[:, k, tb * 128:(tb + 1) * 128], rw, rw[:, k, :], start=(k == 0),
                         stop=(k == 7))
                s = sm.next()
                o.cp("vector", s, s[:, 0:8], p, p[:, 0:8])
                fw.op("vector", lambda e, s=s: e.reduce_max(out=s[:, 8:9], in_=s[:, 0:8], axis=AX.X), [s], [s])
                o.ts("vector", s, s[:, 9:10], s, s[:, 8:9], -1.0, None, ALU.mult)
                o.ts("vector", s, s[:, 10:18], s, s[:, 0:8], s[:, 8:9], None, ALU.is_equal)
                o.stt(s, s[:, 18:26], s, s[:, 10:18], -1e30, s, s[:, 0:8], ALU.mult, ALU.add)
                fw.op("vector", lambda e, s=s: e.reduce_max(out=s[:, 26:27], in_=s[:, 18:26], axis=AX.X), [s], [s])
                o.ts("vector", s, s[:, 27:35], s, s[:, 0:8], s[:, 26:27], None, ALU.is_ge)
                o.act(s, s[:, 35:43], s, s[:, 0:8], AF.Exp, bias=s[:, 9:10], scale=1.0)
                o.tt("vector", s, s[:, 35:43], s, s[:, 35:43], s, s[:, 27:35], ALU.mult)
                fw.op("vector", lambda e, s=s: e.reduce_sum(out=s[:, 43:44], in_=s[:, 35:43], axis=AX.X), [s], [s])
                o.recip(s, s[:, 44:45], s, s[:, 43:44])
                o.ts("vector", s, s[:, 45:53], s, s[:, 35:43], s[:, 44:45], None, ALU.mult)
                o.tr(cps, cps[0:8, tb * 128:(tb + 1) * 128], s, s[:, 45:53], ident, ident[:, :])
            cb = cbo.next()
            o.act(cb, cb[:, 0:N], cps, cps[0:8, 0:N], AF.Copy)
            out_evs.append(fw.dma("gpsimd", comb_d[:, c0:c0 + N], cb[:, 0:N], reads=[cb]))

    for ci in range(TL // NCH):
        chunk(0, ci * NCH, NCH)
    chunk(1, TL, TC)
    fw.finish(out_evs)
    return nc


def build_p5(TL, TC=256, moe=False, NCH=512, E=None, F=None, SCN=3):
    T = TL + TC
    if E is None:
        E = 8 if moe else 1
    if F is None:
        F = 3584 if moe else 2816
    nc = bass.Bass("TRN2", target_bir_lowering=False)
    x1_d = nc.dram_tensor("x1T", [D, T], F32, kind="ExternalInput").ap()
    u2_d = nc.dram_tensor("u2T", [D, T], BF16, kind="ExternalInput").ap()
    wg_d = nc.dram_tensor("wg", [E, D, F], F32, kind="ExternalInput").ap()
    wu_d = nc.dram_tensor("wu", [E, D, F], F32, kind="ExternalInput").ap()
    wd_d = nc.dram_tensor("wd", [E, F, D], F32, kind="ExternalInput").ap()
    pvec_d = nc.dram_tensor("pvec", [128, NPV4], F32, kind="ExternalInput").ap()
    x2_d = nc.dram_tensor("x2T", [D, T], F32, kind="ExternalOutput").ap()
    if moe:
        comb_d = nc.dram_tensor("combT", [8, T], F32, kind="ExternalInput").ap()
        sel_d = nc.dram_tensor("selE", [8, 8, 128], F32, kind="ExternalInput").ap()
    fw = FW(nc)
    o = OPS(fw)
    pvec = fw.sb("pvec", [128, NPV4], F32)
    fw.dma("sync", pvec[:], pvec_d, writes=[pvec])
    def pv(name, i=0):
        c0, n = PV4[name]
        return pvec[:, c0 + i:c0 + i + 1]
    if moe:
        selE = fw.sb("selE", [8, 8, 128], F32)
        fw.dma("sync", selE[:], sel_d, writes=[selE])
    chunks = [(0, ci * NCH, NCH) for ci in range(TL // NCH)] + [(1, TL, TC)]
    scs = []
    per = (len(chunks) + SCN - 1) // SCN
    for i in range(0, len(chunks), per):
        scs.append(chunks[i:i + per])
    maxw = max(sum(c[2] for c in sc) for sc in scs)
    maxc = max(len(sc) for sc in scs)
    yacc = fw.sb("yacc", [128, 8, maxw], F32)
    u2 = fw.sb("u2", [128, 8, maxw], BF16)
    GF = 512
    groups = []
    for e in range(E):
        for f0 in range(0, F, GF):
            groups.append((e, f0, min(GF, F - f0)))
    wgp = Pool(fw, "wgb", [128, 8, GF], BF16, 2)
    wup = Pool(fw, "wub", [128, 8, GF], BF16, 2)
    wdp = Pool(fw, "wdb", [128, GF // 128, D], BF16, 2)
    stg = Pool(fw, "stg", [128, 2048], F32, 3)
    PSg = Pool(fw, "PSg", [128, 512], F32, 2, space="ps")
    PSu = Pool(fw, "PSu", [128, 512], F32, 2, space="ps")
    PSy = Pool(fw, "PSy", [128, 512], F32, 2, space="ps")
    PSc = Pool(fw, "PSc", [128, 512], F32, 1, space="ps")
    sgp = Pool(fw, "sg", [128, NCH], F32, 2)
    hp = Pool(fw, "h", [128, GF // 128, NCH], BF16, 2)
    cbp = Pool(fw, "cb", [128, maxc, NCH], F32, 2)
    cin = Pool(fw, "cin", [8, maxw], F32, 1)
    x1p = Pool(fw, "x1", [128, NCH], F32, 3)
    x2p = Pool(fw, "x2", [128, NCH], F32, 3)
    out_evs = []
    cast_engs = ["gpsimd", "vector", "gpsimd", "scalar"]
    cast_i = [0]

    def cast(dst_b, dst_ap, src_b, src_ap):
        e = cast_engs[cast_i[0] % len(cast_engs)]
        cast_i[0] += 1
        if e == "scalar":
            o.act(dst_b, dst_ap, src_b, src_ap, AF.Copy)
        else:
            o.cp(e, dst_b, dst_ap, src_b, src_ap)

    dq = ["sync", "gpsimd"]
    dqi = [0]
    def ldq():
        dqi[0] += 1
        return dq[dqi[0] % 2]

    def load_group(e, f0, fw_):
        wg = wgp.next(); wu = wup.next(); wd = wdp.next()
        for (dst, src) in ((wg, wg_d), (wu, wu_d)):
            for k2 in range(0, 8, 4):
                s = stg.next()
                for kk in range(4):
                    k = k2 + kk
                    fw.dma("sync", s[:, kk * 512:kk * 512 + fw_], src[e, k * 128:(k + 1) * 128, f0:f0 + fw_], writes=[s])
                for kk in range(4):
                    cast(dst, dst[:, k2 + kk, 0:fw_], s, s[:, kk * 512:kk * 512 + fw_])
        nf = fw_ // 128
        for f2 in range(0, nf, 2):
            s = stg.next()
            for ff in range(min(2, nf - f2)):
                f = f2 + ff
                fw.dma("sync", s[:, ff * 1024:(ff + 1) * 1024], wd_d[e, f0 + f * 128:f0 + (f + 1) * 128, :], writes=[s])
            for ff in range(min(2, nf - f2)):
                cast(wd, wd[:, f2 + ff, :], s, s[:, ff * 1024:(ff + 1) * 1024])
        return wg, wu, wd

    for sc in scs:
        off = 0
        offs = []
        for (seg, c0, N) in sc:
            offs.append(off)
            for k in range(8):
                fw.dma("gpsimd", u2[:, k, off:off + N], u2_d[k * 128:(k + 1) * 128, c0:c0 + N], writes=[u2])
            off += N
        if moe:
            ci_ = cin.next()
            off2 = 0
            for (seg, c0, N) in sc:
                fw.dma("gpsimd", ci_[:, off2:off2 + N], comb_d[:, c0:c0 + N], writes=[ci_])
                off2 += N
        cur_e = -1
        cb = None
        for gi, (e, f0, fw_) in enumerate(groups):
            wg, wu, wd = load_group(e, f0, fw_)
            nf = fw_ // 128
            if moe and e != cur_e:
                cur_e = e
                cb = cbp.next()
                for ci2, (seg, c0, N) in enumerate(sc):
                    p = PSc.next()
                    o.mm(p, p[:, 0:N], selE, selE[:, e, :], ci_, ci_[:, offs[ci2]:offs[ci2] + N])
                    o.act(cb, cb[:, ci2, 0:N], p, p[:, 0:N], AF.Copy)
            for ci2, (seg, c0, N) in enumerate(sc):
                of = offs[ci2]
                h = hp.next()
                for f in range(nf):
                    pg = PSg.next(); pu = PSu.next()
                    for k in range(8):
                        o.mm(pg, pg[:, 0:N], wg, wg[:, k, f * 128:(f + 1) * 128], u2, u2[:, k, of:of + N],
                             start=(k == 0), stop=(k == 7))
                    for k in range(8):
                        o.mm(pu, pu[:, 0:N], wu, wu[:, k, f * 128:(f + 1) * 128], u2, u2[:, k, of:of + N],
                             start=(k == 0), stop=(k == 7))
                    sg = sgp.next()
                    o.act(sg, sg[:, 0:N], pg, pg[:, 0:N], AF.Silu)
                    if moe:
                        sg2 = sgp.next()
                        o.tt("vector", sg2, sg2[:, 0:N], sg, sg[:, 0:N], pu, pu[:, 0:N], ALU.mult)
                        o.tt("gpsimd", h, h[:, f, 0:N], sg2, sg2[:, 0:N], cb, cb[:, ci2, 0:N], ALU.mult)
                    else:
                        o.tt("vector", h, h[:, f, 0:N], sg, sg[:, 0:N], pu, pu[:, 0:N], ALU.mult)
                for n in range(8):
                    py = PSy.next()
                    for f in range(nf):
                        o.mm(py, py[:, 0:N], wd, wd[:, f, n * 128:(n + 1) * 128], h, h[:, f, 0:N], start=(f == 0),
                             stop=(f == nf - 1))
                    if gi == 0:
                        o.act(yacc, yacc[:, n, of:of + N], py, py[:, 0:N], AF.Copy)
                    else:
                        o.tt("vector", yacc, yacc[:, n, of:of + N], yacc, yacc[:, n, of:of + N], py, py[:, 0:N], ALU.add)
        for ci2, (seg, c0, N) in enumerate(sc):
            of = offs[ci2]
            gn = "g5_l" if seg == 0 else "g5_c"
            for n in range(8):
                x1 = x1p.next()
                fw.dma("sync", x1[:, 0:N], x1_d[n * 128:(n + 1) * 128, c0:c0 + N], writes=[x1])
                x2 = x2p.next()
                o.stt(x2, x2[:, 0:N], yacc, yacc[:, n, of:of + N], pv(gn, n), x1, x1[:, 0:N], ALU.mult, ALU.add,
                      extra_r=[pvec])
                out_evs.append(fw.dma("gpsimd", x2_d[n * 128:(n + 1) * 128, c0:c0 + N], x2[:, 0:N], reads=[x2]))
    fw.finish(out_evs)
    return nc


POOL_WINDOWS = (2, 4, 8, 16)

def mod_inputs(inputs, nl=4):
    cT = np.stack([inputs["c"][0], inputs["c"][1], inputs["c_ctx"]], axis=1).astype(np.float32)
    maps = []
    for j in range(8):
        maps.append({"cT": cT, "mw": np.ascontiguousarray(inputs["mod_w"][:nl, :, j * 768:(j + 1) * 768]),
                     "mb": np.ascontiguousarray(inputs["mod_b"][:nl, j * 768:(j + 1) * 768].reshape(nl, 6, 128).transpose(0, 2, 1))})
    return maps

def mod_gather(results):
    return np.concatenate([r["out"] for r in results], axis=1)

def cols(v):
    return np.ascontiguousarray(v.reshape(-1, 128).T)

def rope_tables(L, grid_w=64):
    rows = L // grid_w
    row = np.repeat(np.arange(rows, dtype=np.float32), grid_w)
    col = np.tile(np.arange(grid_w, dtype=np.float32), rows)
    inv = np.power(np.float32(10000.0), -np.arange(16, dtype=np.float32) / np.float32(16)).astype(np.float32)
    C = np.zeros((128, L), np.float32); S = np.zeros((128, L), np.float32)
    for p in range(128):
        dh = p % 64
        axis = dh // 32; ab = (dh % 32) // 16; f = dh % 16
        pos = row if axis == 0 else col
        ang = (pos * inv[f]).astype(np.float32)
        C[p] = np.cos(ang); S[p] = np.sin(ang) * (-1.0 if ab == 0 else 1.0)
    return C, S

def const_mats():
    cm = np.zeros((128, 4, 128), np.float32)
    cm[:, 0, :] = 1.0 / 1024.0
    for p in range(128):
        for k in range(128):
            if p // 64 == k // 64:
                cm[k, 1, p] = 1.0 / 64.0
    for p in range(128):
        dh = p % 64; ab = (dh % 32) // 16
        partner = p + 16 if ab == 0 else p - 16
        cm[partner, 2, p] = 1.0
    cm[:, 3, :] = np.eye(128, dtype=np.float32)
    return cm

def invc_table(qd, nq, L, Lc=256):
    t = np.zeros((128, 2, 2, 16), np.float32)
    for p in range(128):
        for pt in range(2):
            g = 2 * pt + (1 if p >= 64 else 0)
            w = POOL_WINDOWS[g]
            for e in range(16):
                for seg, Ls, realL, realR in ((0, L, qd == 0, qd == nq - 1), (1, Lc, True, True)):
                    if e < 8:
                        tt = e
                        cnt = min(tt + w // 2, Ls) - max(tt - w // 2, 0) if realL else w
                    else:
                        tt = Ls - 16 + e
                        cnt = min(tt + w // 2, Ls) - max(tt - w // 2, 0) if realR else w
                    t[p, seg, pt, e] = 1.0 / cnt
    return t

def p1_inputs(inputs, l, mvec, x, h, ropeC, ropeS, cm):
    B, L, D = x.shape
    nq = 8 // B
    TL = L // nq
    maps = []
    pl = inputs["pool_lin"][l]
    plin = np.zeros((128, 2, 128), np.float32)
    for pt in range(2):
        plin[0:64, pt, 0:64] = pl[2 * pt]; plin[64:128, pt, 64:128] = pl[2 * pt + 1]
    for j in range(8):
        b, qd = j // nq, j % nq
        xp = np.zeros((TL + 16, D), np.float32)
        lo, hi = qd * TL - 8, qd * TL + TL + 8
        slo, shi = max(lo, 0), min(hi, L)
        xp[slo - lo:shi - lo] = x[b, slo:shi]
        hp = np.zeros((h.shape[1] + 16, D), np.float32)
        hp[8:8 + h.shape[1]] = h[b]
        pv = np.zeros((128, NPV), np.float32)
        def put(name, arr):
            c0, n = PV[name]; pv[:, c0:c0 + n] = arr.reshape(128, n)
        m = mvec[l]
        put("g1", cols(inputs["norm1_g"][l]))
        put("sh_l", cols(m[0:1024, b])); put("sc_l", cols(m[1024:2048, b]))
        put("sh_c", cols(m[0:1024, 2])); put("sc_c", cols(m[1024:2048, 2]))
        sw = inputs["hy_short_w"][l]
        cw = np.zeros((128, 18), np.float32)
        for t in range(6):
            for tap in range(3):
                cw[:, t * 3 + tap] = sw[tap, t * 128:(t + 1) * 128]
        put("cw", cw); put("cb", cols(inputs["hy_short_b"][l]))
        put("gq", np.tile(inputs["qk_norm_g"][l, 0], 2)[:, None]); put("gk", np.tile(inputs["qk_norm_g"][l, 1], 2)[:, None])
        put("psc", cols(inputs["pool_scale"][l]))
        put("mL", np.full((128, 1), 0.0 if qd == 0 else 1.0, np.float32))
        put("mR", np.full((128, 1), 0.0 if qd == nq - 1 else 1.0, np.float32))
        put("eps", np.full((128, 1), 1e-6, np.float32))
        maps.append({"xT": np.ascontiguousarray(xp.T), "hT": np.ascontiguousarray(hp.T), "pvec": pv,
                     "w_in": np.ascontiguousarray(inputs["w_in"][l]),
                     "ropeC": np.ascontiguousarray(ropeC[:, qd * TL:(qd + 1) * TL]),
                     "ropeS": np.ascontiguousarray(ropeS[:, qd * TL:(qd + 1) * TL]),
                     "cmats": cm, "plin": plin, "invc": invc_table(qd, nq, L, h.shape[1])})
    return maps


def pvec4(inputs, l, mvec, b):
    pv = np.zeros((128, NPV4), np.float32)
    def put(name, arr):
        c0, n = PV4[name]; pv[:, c0:c0 + n] = arr.reshape(128, n)
    m = mvec[l]
    put("hyb", cols(inputs["hy_bias"][l]))
    put("g2_l", cols(m[2048:3072, b])); put("g2_c", cols(m[2048:3072, 2]))
    put("n2g", cols(inputs["norm2_g"][l]))
    put("sh_l", cols(m[3072:4096, b])); put("sc_l", cols(m[4096:5120, b]))
    put("sh_c", cols(m[3072:4096, 2])); put("sc_c", cols(m[4096:5120, 2]))
    put("g5_l", cols(m[5120:6144, b])); put("g5_c", cols(m[5120:6144, 2]))
    put("eps", np.full((128, 1), 1e-6, np.float32))
    return pv

ONESD = np.full((128, 128), 1.0 / 1024.0, np.float32)
IDENT = np.eye(128, dtype=np.float32)
SELE = np.zeros((8, 8, 128), np.float32)
for _e in range(8):
    SELE[_e, _e, :] = 1.0


SEL32 = np.ascontiguousarray(np.concatenate([np.eye(32), np.eye(32)], 0).astype(np.float32))


def pvec1(inputs, l, mvec, b, qd, nq):
    pv = np.zeros((128, NPV), np.float32)
    def put(name, arr):
        c0, n = PV[name]; pv[:, c0:c0 + n] = arr.reshape(128, n)
    m = mvec[l]
    put("g1", cols(inputs["norm1_g"][l]))
    put("sh_l", cols(m[0:1024, b])); put("sc_l", cols(m[1024:2048, b]))
    put("sh_c", cols(m[0:1024, 2])); put("sc_c", cols(m[1024:2048, 2]))
    sw = inputs["hy_short_w"][l]
    cw = np.zeros((128, 18), np.float32)
    for t in range(6):
        for tap in range(3):
            cw[:, t * 3 + tap] = sw[tap, t * 128:(t + 1) * 128]
    put("cw", cw); put("cb", cols(inputs["hy_short_b"][l]))
    put("gq", np.tile(inputs["qk_norm_g"][l, 0], 2)[:, None]); put("gk", np.tile(inputs["qk_norm_g"][l, 1], 2)[:, None])
    put("psc", cols(inputs["pool_scale"][l]))
    put("mL", np.full((128, 1), 0.0 if qd == 0 else 1.0, np.float32))
    put("mR", np.full((128, 1), 0.0 if qd == nq - 1 else 1.0, np.float32))
    put("eps", np.full((128, 1), 1e-6, np.float32))
    return pv

_PROGS = {}


def _prog(key, builder):
    if key not in _PROGS:
        _PROGS[key] = builder()
    return _PROGS[key]


def _run(nc, maps):
    import time as _t
    t0 = _t.time()
    res = run_bass_kernel_spmd(nc, maps, core_ids=list(range(8)))
    if _VERBOSE:
        print("[kernel] launch done in %.1fs" % (_t.time() - t0), flush=True)
    return res.results


_VERBOSE = False


_BF = ml_dtypes.bfloat16


def kernel(**inputs):
    inp = {k: np.asarray(v) for k, v in inputs.items()}
    x0 = inp["x"]
    B, L, Dm = x0.shape
    C = inp["ctx"].shape[1]
    nq = 8 // B
    TL = L // nq
    T = TL + C
    NL = inp["mod_w"].shape[0]
    mvec = mod_gather(_run(_prog(("p0", NL), lambda: build_p0(768, NL)), mod_inputs(inp, NL)))
    ropeC, ropeS = rope_tables(L)
    cm = const_mats()
    tabs = {LL: lc_tables(LL) for LL in (L, C)}
    ftabs = {(LL, j): filt_tables(LL, 32 * j) for LL in (L, C) for j in range(8)}
    xcur = []
    for j in range(8):
        b, qd = j // nq, j % nq
        xcur.append(np.ascontiguousarray(np.concatenate([x0[b, qd * TL:(qd + 1) * TL], inp["ctx"][b]], 0).T))
    z8 = np.zeros((Dm, 8), np.float32)
    for l in range(NL):
        moe = (l % 2 == 1)
        jl = l // 2
        lam_init = 0.8 - 0.6 * math.exp(-0.3 * l)
        maps = []
        pl = inp["pool_lin"][l]
        plin = np.zeros((128, 2, 128), np.float32)
        for pt in range(2):
            plin[0:64, pt, 0:64] = pl[2 * pt]
            plin[64:128, pt, 64:128] = pl[2 * pt + 1]
        for j in range(8):
            b, qd = j // nq, j % nq
            left = xcur[j - 1][:, TL - 8:TL] if qd > 0 else z8
            right = xcur[j + 1][:, 0:8] if qd < nq - 1 else z8
            xT = np.ascontiguousarray(np.concatenate([left, xcur[j][:, :TL], right], 1))
            hT = np.ascontiguousarray(np.concatenate([z8, xcur[j][:, TL:], z8], 1))
            maps.append({"xT": xT, "hT": hT, "pvec": pvec1(inp, l, mvec, b, qd, nq),
                         "w_in": np.ascontiguousarray(inp["w_in"][l]),
                         "ropeC": np.ascontiguousarray(ropeC[:, qd * TL:(qd + 1) * TL]),
                         "ropeS": np.ascontiguousarray(ropeS[:, qd * TL:(qd + 1) * TL]),
                         "cmats": cm, "plin": plin, "invc": invc_table(qd, nq, L, C)})
        r1 = _run(_prog(("p1", TL, C), lambda: build_p1(TL, C)), maps)
        r1 = [{k: np.asarray(v) for k, v in r.items()} for r in r1]
        maps = []
        pv2 = np.zeros((128, NPV2), np.float32)
        pv2[:, 0] = 1e-6
        pv2[:, 1] = lam_init
        pv2[:, 2] = inp["subln_g"][l]
        pv2[:, 3] = 1.0 - lam_init
        dlam = np.ascontiguousarray(np.broadcast_to(inp["diff_lambda"][l][None], (128, 4, 64))).astype(np.float32)
        for j in range(8):
            b, hd = j // 4, j % 4
            cores = [b * nq + qd for qd in range(nq)]
            rs_ = slice(hd * 128, (hd + 1) * 128)
            qT = np.concatenate([r1[c]["qT"][rs_, :TL] for c in cores] + [r1[cores[0]]["qT"][rs_, TL:]], 1)
            kT = np.concatenate([r1[cores[0]]["kT"][rs_, TL:]] + [r1[c]["kT"][rs_, :TL] for c in cores], 1)
            v = np.concatenate([r1[cores[0]]["v"][TL:, rs_]] + [r1[c]["v"][:TL, rs_] for c in cores], 0)
            maps.append({"qT": np.ascontiguousarray(qT), "kT": np.ascontiguousarray(kT), "v": np.ascontiguousarray(v),
                         "dlam": dlam, "pvec": pv2, "ident": IDENT})
        r2 = _run(_prog(("p2", L, C), lambda: build_p2(L, C, True)), maps)
        r2 = [np.asarray(r["attT"]) for r in r2]
        maps = []
        for j in range(8):
            ch0 = 32 * j
            w3 = inp["hy_f_w3"][l]
            m = {"w1": np.ascontiguousarray(inp["hy_f_w1"][l]), "w2": np.ascontiguousarray(inp["hy_f_w2"][l]),
                 "w3s": np.ascontiguousarray(np.concatenate([w3[:, ch0:ch0 + 32], w3[:, 256 + ch0:256 + ch0 + 32]], 1)),
                 "fpv": np.ascontiguousarray(np.stack([inp["hy_f_freq1"][l], inp["hy_f_b1"][l], inp["hy_f_freq2"][l],
                                                       inp["hy_f_b2"][l]], 1)),
                 "sel": SEL32}
            for LL in (L, C):
                sfx = "_%d" % LL
                for k_, v_ in tabs[LL].items():
                    m[k_ + sfx] = v_
                m["feat" + sfx], m["dec" + sfx] = ftabs[(LL, j)]
                rows = []
                for b in range(B):
                    if LL == L:
                        rows.append(np.concatenate([r1[b * nq + qd]["v2"][ch0:ch0 + 32, :TL] for qd in range(nq)], 1))
                    else:
                        rows.append(r1[b * nq]["v2"][ch0:ch0 + 32, TL:])
                m["vin" + sfx] = np.ascontiguousarray(np.concatenate(rows, 0))
            maps.append(m)
        r3 = _run(_prog(("p3", L, C), lambda: build_p3((L, C))), maps)
        r3 = [{k: np.asarray(v) for k, v in r.items()} for r in r3]
        maps = []
        for j in range(8):
            b, qd = j // nq, j % nq
            conv = np.zeros((256, T), np.float32)
            att = np.zeros((512, T), _BF)
            for jj in range(8):
                conv[32 * jj:32 * jj + 32, :TL] = r3[jj]["yout_%d" % L][b * 32:(b + 1) * 32, qd * TL:(qd + 1) * TL]
                conv[32 * jj:32 * jj + 32, TL:] = r3[jj]["yout_%d" % C][b * 32:(b + 1) * 32, :]
            for hd in range(4):
                a = r2[b * 4 + hd]
                att[hd * 128:(hd + 1) * 128, :TL] = a[:, qd * TL:(qd + 1) * TL]
                att[hd * 128:(hd + 1) * 128, TL:] = a[:, L:L + C]
            mp = {"xT": xcur[j], "ypool": r1[j]["ypool"], "x0c": r1[j]["x0c"], "v2": r1[j]["v2"], "conv": conv,
                  "attT": att, "w_out": np.ascontiguousarray(inp["w_out"][l]), "pvec": pvec4(inp, l, mvec, b),
                  "onesD": ONESD}
            if moe:
                mp["rw"] = np.ascontiguousarray(inp["router_w"][jl].reshape(8, 128, 8).transpose(1, 0, 2))
                mp["ident"] = IDENT
            maps.append(mp)
        r4 = _run(_prog(("p4", TL, C, moe), lambda: build_p4(TL, C, moe)), maps)
        r4 = [{k: np.asarray(v) for k, v in r.items()} for r in r4]
        del r1, r2, r3
        maps = []
        if moe:
            wg = np.ascontiguousarray(inp["moe_w_gate"][jl]); wu = np.ascontiguousarray(inp["moe_w_up"][jl])
            wd = np.ascontiguousarray(inp["moe_w_down"][jl])
        else:
            wg = np.ascontiguousarray(inp["ffn_w_gate"][jl][None]); wu = np.ascontiguousarray(inp["ffn_w_up"][jl][None])
            wd = np.ascontiguousarray(inp["ffn_w_down"][jl][None])
        for j in range(8):
            b = j // nq
            mp = {"x1T": r4[j]["x1T"], "u2T": r4[j]["u2T"], "pvec": pvec4(inp, l, mvec, b), "wg": wg, "wu": wu, "wd": wd}
            if moe:
                mp["combT"] = r4[j]["combT"]
                mp["selE"] = SELE
            maps.append(mp)
        r5 = _run(_prog(("p5", TL, C, moe), lambda: build_p5(TL, C, moe)), maps)
        xcur = [np.asarray(r["x2T"]) for r in r5]
        del r4, r5
    out = np.zeros((B, L, Dm), np.float32)
    for j in range(8):
        b, qd = j // nq, j % nq
        out[b, qd * TL:(qd + 1) * TL] = xcur[j][:, :TL].T
    return out
```

```python
import math
import numpy as np
import ml_dtypes
import concourse.bass as bass
import concourse.mybir as mybir
from concourse.bass_utils import run_bass_kernel_spmd

F32 = mybir.dt.float32
BF16 = mybir.dt.bfloat16
I32 = mybir.dt.int32
AF = mybir.ActivationFunctionType
ALU = mybir.AluOpType
AX = mybir.AxisListType

ENGS = ["tensor", "vector", "scalar", "gpsimd", "sync"]


class Buf:
    __slots__ = ("t", "w", "r", "name")

    def __init__(self, t, name=""):
        self.t = t
        self.w = None
        self.r = {}
        self.name = name

    def __getitem__(self, idx):
        return self.t[idx]


class FW:
    def __init__(self, nc, dma_ring=6, fused=False):
        self.nc = nc
        self.fused = fused
        self.phase = "g"
        self.uid = 0
        if fused:
            self.sb_lo = (int(nc.sbuf_base) + 63) // 64 * 64
            self.sb_hi = int(nc.sbuf_top)
            self.floor = self.sb_lo
            self.off = self.sb_lo
            self.pall = nc.alloc_psum_tensor("ps_all", [128, 4096], F32)
            self.psb = 0
        self.prog = {e: [] for e in ENGS}
        self.sems = {}
        self.seq = {}
        self.known = {e: {} for e in ENGS}
        for e in ENGS:
            self.sems[e] = nc.alloc_semaphore("s_" + e)
            self.seq[e] = 0
        self.dma_ring = dma_ring
        self.dq = {}
        for q in ["sync", "gpsimd", "scalar"]:
            ring = []
            for i in range(dma_ring):
                key = "d_%s_%d" % (q, i)
                self.sems[key] = nc.alloc_semaphore(key)
                ring.append(key)
            self.dq[q] = {"ring": ring, "n": 0, "hist": []}
        self.n_inst = 0

    def sb(self, name, shape, dtype, persist=False):
        if not self.fused:
            return Buf(self.nc.alloc_sbuf_tensor("sb_" + name, list(shape), dtype), name)
        n = 1
        for d_ in shape[1:]:
            n *= int(d_)
        nbytes = (n * mybir.dt.size(dtype) + 63) // 64 * 64
        self.uid += 1
        if persist:
            assert self.off == self.floor, "persistent allocs must precede phase allocs"
            off = self.floor
            self.floor += nbytes
            self.off = self.floor
        else:
            off = self.off
            self.off += nbytes
        assert off + nbytes <= self.sb_hi, "SBUF overflow in phase %s at %s (%d > %d)" % (self.phase, name, off + nbytes, self.sb_hi)
        return Buf(self.nc.alloc_sbuf_tensor_at("sb_%s_%d_%s" % (self.phase, self.uid, name), list(shape), dtype, offset=off), name)

    def ps(self, name, shape, dtype=F32):
        if not self.fused:
            return Buf(self.nc.alloc_psum_tensor("ps_" + name, list(shape), dtype), name)
        n = 1
        for d_ in shape[1:]:
            n *= int(d_)
        nbytes = n * mybir.dt.size(dtype)
        nb = (nbytes + 2047) // 2048
        assert self.psb + nb <= 8, "PSUM overflow in phase %s at %s" % (self.phase, name)
        v = self.pall[0:shape[0], self.psb * 512:(self.psb + nb) * 512]
        self.psb += nb
        if dtype != F32:
            v = v.bitcast(dtype)
        v = v[:, 0:n]
        if len(shape) > 2:
            raise NotImplementedError("psum views are 2-D")
        return Buf(v, name)

    def begin_phase(self, name):
        self.barrier()
        self.phase = name
        if self.fused:
            self.off = self.floor
            self.psb = 0

    def barrier(self):
        latest = {}
        for e in ENGS:
            if self.seq[e] > 0:
                latest[e] = self.seq[e]
        for q, dq in self.dq.items():
            n = dq["n"]
            for i, key in enumerate(dq["ring"]):
                cnt = (n - i + self.dma_ring - 1) // self.dma_ring if n > i else 0
                if cnt > 0:
                    latest[key] = 16 * cnt
        for e in ENGS:
            kn = self.known[e]
            waits = []
            for k, v in latest.items():
                if kn.get(k, 0) < v:
                    kn[k] = v
                    waits.append((k, v))
            if waits:
                self.prog[e].append((waits, None, None, False))

    def coll(self, kind, in_buf, out_buf, groups):
        dq = self.dq["gpsimd"]
        n = dq["n"]
        dq["n"] += 1
        key = dq["ring"][n % self.dma_ring]
        val = 16 * (n // self.dma_ring + 1)
        waits = self._deps("gpsimd", [in_buf], [out_buf])
        if n >= self.dma_ring:
            pk, pv = key, val - 16
            if self.known["gpsimd"].get(pk, 0) < pv:
                self.known["gpsimd"][pk] = pv
                waits.append((pk, pv))
        ev = (key, val)
        ia, oa = in_buf.t.ap(), out_buf.t.ap()

        def fn(e):
            return e.collective_compute(kind, ALU.bypass, replica_groups=groups, ins=[ia], outs=[oa])
        self.prog["gpsimd"].append((waits, fn, ev, True))
        self._commit(ev, [in_buf], [out_buf])
        return ev

    def dram(self, name, shape, dtype, kind="Internal"):
        self.uid += 1
        nm = "%s_%s_%d" % (name, self.phase, self.uid) if self.fused else name
        return Buf(self.nc.dram_tensor(nm, list(shape), dtype, kind=kind), name)

    def _deps(self, eng, reads, writes):
        need = {}

        def add(ev):
            if ev is None:
                return
            k, v = ev
            if need.get(k, 0) < v:
                need[k] = v
        for b in reads:
            add(b.w)
        for b in writes:
            add(b.w)
            for k, v in b.r.items():
                add((k, v))
        kn = self.known[eng]
        out = []
        for k, v in need.items():
            if kn.get(k, 0) < v:
                kn[k] = v
                out.append((k, v))
        return out

    def _commit(self, ev, reads, writes):
        k, v = ev
        for b in reads:
            if b.r.get(k, 0) < v:
                b.r[k] = v
        for b in writes:
            b.w = ev
            b.r = {}

    def op(self, eng, fn, reads=(), writes=(), same_eng_sync=True):
        waits = self._deps(eng, reads, writes)
        self.seq[eng] += 1
        ev = (eng, self.seq[eng])
        if not same_eng_sync:
            waits = [w for w in waits if w[0] != eng]
        self.prog[eng].append((waits, fn, ev, False))
        self._commit(ev, reads, writes)
        self.n_inst += 1
        return ev

    def dma(self, q, out_ap, in_ap, reads=(), writes=(), **kw):
        dq = self.dq[q]
        n = dq["n"]
        dq["n"] += 1
        key = dq["ring"][n % self.dma_ring]
        val = 16 * (n // self.dma_ring + 1)
        waits = self._deps(q, reads, writes)
        if n >= self.dma_ring:
            pk, pv = key, val - 16
            if self.known[q].get(pk, 0) < pv:
                self.known[q][pk] = pv
                waits.append((pk, pv))
        ev = (key, val)

        def fn(e, out_ap=out_ap, in_ap=in_ap, kw=kw):
            return e.dma_start(out=out_ap, in_=in_ap, **kw)
        self.prog[q].append((waits, fn, ev, True))
        self._commit(ev, reads, writes)
        self.n_inst += 1
        return ev

    def finish(self, final_events):
        nc = self.nc
        sems = self.sems
        prog = self.prog
        with nc.Block() as block:
            def make(engname):
                def body(e):
                    for waits, fn, ev, is_dma in prog[engname]:
                        for k, v in waits:
                            e.wait_ge(sems[k], v)
                        if fn is None:
                            continue
                        ins = fn(e)
                        ins.then_inc(sems[ev[0]], 16 if is_dma else 1)
                    if engname == "sync":
                        fin = {}
                        for k, v in final_events:
                            fin[k] = max(fin.get(k, 0), v)
                        for k, v in fin.items():
                            e.wait_ge(sems[k], v)
                return body
            block.tensor(make("tensor"))
            block.vector(make("vector"))
            block.scalar(make("scalar"))
            block.gpsimd(make("gpsimd"))
            block.sync(make("sync"))


class Pool:
    def __init__(self, fw, name, shape, dtype, n, space="sb"):
        mk = fw.sb if space == "sb" else fw.ps
        self.bufs = [mk("%s_%d" % (name, i), shape, dtype) for i in range(n)]
        self.i = 0

    def next(self):
        b = self.bufs[self.i % len(self.bufs)]
        self.i += 1
        return b


def _lst(x):
    if x is None:
        return []
    if isinstance(x, (list, tuple)):
        return list(x)
    return [x]


class OPS:
    def __init__(self, fw):
        self.fw = fw

    def act(self, ob, oap, ib, iap, func, bias=None, scale=None, accum=None, extra_r=(), eng="scalar"):
        kw = {}
        reads = _lst(ib) + list(extra_r)
        writes = _lst(ob)
        if bias is not None:
            kw["bias"] = bias
        if scale is not None:
            kw["scale"] = scale
        if accum is not None:
            kw["accum_out"] = accum[1]
            writes.append(accum[0])
        return self.fw.op("scalar", lambda e: e.activation(out=oap, in_=iap, func=func, **kw), reads, writes)

    def mm(self, ob, oap, lb, lap, rb, rap, start=True, stop=True, **kw):
        return self.fw.op("tensor", lambda e: e.matmul(oap, lhsT=lap, rhs=rap, start=start, stop=stop, **kw),
                          _lst(lb) + _lst(rb), _lst(ob), same_eng_sync=False)

    def tr(self, ob, oap, ib, iap, idb, idap):
        return self.fw.op("tensor", lambda e: e.transpose(oap, iap, idap), _lst(ib) + _lst(idb), _lst(ob),
                          same_eng_sync=False)

    def tt(self, eng, ob, oap, ab, aap, bb, bap, op):
        return self.fw.op(eng, lambda e: e.tensor_tensor(out=oap, in0=aap, in1=bap, op=op),
                          _lst(ab) + _lst(bb), _lst(ob))

    def ts(self, eng, ob, oap, ab, aap, s1, s2, op0, op1=None, extra_r=(), accum=None):
        kw = {}
        writes = _lst(ob)
        if op1 is not None:
            kw["op1"] = op1
        if accum is not None:
            kw["accum_out"] = accum[1]
            writes.append(accum[0])
        return self.fw.op(eng, lambda e: e.tensor_scalar(out=oap, in0=aap, scalar1=s1, scalar2=s2, op0=op0, **kw),
                          _lst(ab) + list(extra_r), writes)

    def stt(self, ob, oap, ab, aap, scalar, bb, bap, op0, op1, extra_r=()):
        return self.fw.op("vector", lambda e: e.scalar_tensor_tensor(out=oap, in0=aap, scalar=scalar, in1=bap,
                                                                      op0=op0, op1=op1),
                          _lst(ab) + _lst(bb) + list(extra_r), _lst(ob))

    def cp(self, eng, ob, oap, ib, iap):
        return self.fw.op(eng, lambda e: e.tensor_copy(out=oap, in_=iap), _lst(ib), _lst(ob))

    def recip(self, ob, oap, ib, iap):
        return self.fw.op("vector", lambda e: e.reciprocal(out=oap, in_=iap), _lst(ib), _lst(ob))

    def memset(self, eng, ob, oap, val):
        return self.fw.op(eng, lambda e: e.memset(oap, val), [], _lst(ob))


class Env:
    def __init__(self, nc=None, fw=None, bind=None):
        self.fused = nc is not None
        self.nc = nc if nc is not None else bass.Bass("TRN2", target_bir_lowering=False)
        self.fw = fw if fw is not None else FW(self.nc)
        self.o = OPS(self.fw)
        self.bind = bind or {}

    def io(self, name, shape, dtype, kind):
        if self.fused:
            ap = self.bind[name]
            assert tuple(int(x) for x in ap.shape) == tuple(int(x) for x in shape), (name, ap.shape, shape)
            return ap
        return self.nc.dram_tensor(name, list(shape), dtype, kind=kind).ap()

    def done(self, evs):
        if self.fused:
            return evs
        self.fw.finish(evs)
        return self.nc


D = 1024
INW = 2560
EPS = 1e-6

PV = {}
_c = 0
def _add(name, n):
    global _c
    PV[name] = (_c, n)
    _c += n
_add("g1", 8)
_add("sh_l", 8)
_add("sc_l", 8)
_add("sh_c", 8)
_add("sc_c", 8)
_add("cw", 18)
_add("cb", 6)
_add("gq", 1)
_add("gk", 1)
_add("psc", 2)
_add("mL", 1)
_add("mR", 1)
_add("zero", 1)
_add("eps", 1)
NPV = _c


def build_p0(ncols=768, nlayers=4, env=None):
    env = env or Env()
    nc, fw, o = env.nc, env.fw, env.o
    cT = env.io("cT", [D, 3], F32, "ExternalInput")
    mw = env.io("mw", [nlayers, D, ncols], F32, "ExternalInput")
    mb = env.io("mb", [nlayers, 128, ncols // 128], F32, "ExternalInput")
    out = env.io("out", [nlayers, ncols, 3], F32, "ExternalOutput")
    nt = ncols // 128
    ct = fw.sb("ct", [128, 8, 3], F32)
    sc = fw.sb("sc", [128, 8, 3], F32)
    fw.dma("sync", ct[:], cT.rearrange("(k p) g -> p k g", p=128), writes=[ct])
    o.act(sc, sc[:], ct, ct[:], AF.Silu)
    wpool = Pool(fw, "w", [128, 8, ncols], F32, 2)
    bpool = Pool(fw, "b", [128, nt], F32, 2)
    opool = Pool(fw, "o", [128, nt, 3], F32, 2)
    pp = Pool(fw, "pp", [128, 512], F32, 2, space="ps")
    evs = []
    for l in range(nlayers):
        w = wpool.next()
        for k in range(8):
            fw.dma("sync" if k % 2 == 0 else "gpsimd", w[:, k, :], mw[l, k * 128:(k + 1) * 128, :], writes=[w])
        b = bpool.next()
        fw.dma("sync", b[:], mb[l], writes=[b])
        ot = opool.next()
        for n in range(nt):
            p = pp.next()
            for k in range(8):
                o.mm(p, p[:, 0:3], w, w[:, k, n * 128:(n + 1) * 128], sc, sc[:, k, :], start=(k == 0), stop=(k == 7))
            o.ts("vector", ot, ot[:, n, :], p, p[:, 0:3], b[:, n:n + 1], None, ALU.add, extra_r=[b])
        evs.append(fw.dma("gpsimd", out[l].rearrange("(n p) g -> p n g", p=128), ot[:], reads=[ot]))
    return env.done(evs)


def build_p1(TL, TC=256, NCH=512, env=None, vsplit=False, mv=None):
    T = TL + TC
    env = env or Env()
    nc, fw, o = env.nc, env.fw, env.o
    xT = env.io("xT", [D, TL + 16], F32, "ExternalInput")
    hT = env.io("hT", [D, TC + 16], F32, "ExternalInput")
    pvec_d = env.io("pvec", [128, NPV], F32, "ExternalInput")
    win_d = env.io("w_in", [D, INW], F32, "ExternalInput")
    ropeC_d = env.io("ropeC", [128, TL], F32, "ExternalInput")
    ropeS_d = env.io("ropeS", [128, TL], F32, "ExternalInput")
    cm_d = env.io("cmats", [128, 4, 128], F32, "ExternalInput")
    plin_d = env.io("plin", [128, 2, 128], F32, "ExternalInput")
    invc_d = env.io("invc", [128, 2, 2, 16], F32, "ExternalInput")
    ypool_d = env.io("ypool", [256, T], BF16, "ExternalOutput")
    x0c_d = env.io("x0c", [256, T], BF16, "ExternalOutput")
    v2_d = env.io("v2", [256, T], F32, "ExternalOutput")
    q_d = env.io("qT", [512, T], BF16, "ExternalOutput")
    k_d = env.io("kT", [512, T], BF16, "ExternalOutput")
    v_d = env.io("v", [4, T, 128] if vsplit else [T, 512], BF16, "ExternalOutput")

    NW = NCH + 16
    pvec = fw.sb("pvec", [128, NPV], F32)
    fw.dma("sync", pvec[:], pvec_d, writes=[pvec])
    if mv is not None:
        mvb, l_ = mv
        for nm_, t0_, g_ in (("sh_l", 0, 0), ("sc_l", 8, 0), ("sh_c", 0, 1), ("sc_c", 8, 1)):
            c0_, _ = PV[nm_]
            o.cp("vector", pvec, pvec[:, c0_:c0_ + 8], mvb, mvb[:, l_, t0_:t0_ + 8, g_])
    def pv(name, i=0):
        c0, n = PV[name]
        return pvec[:, c0 + i:c0 + i + 1]
    cm32 = fw.sb("cm32", [128, 4, 128], F32)
    fw.dma("sync", cm32[:], cm_d, writes=[cm32])
    cmb = fw.sb("cmb", [128, 4, 128], BF16)
    o.cp("vector", cmb, cmb[:], cm32, cm32[:])
    pl32 = fw.sb("pl32", [128, 2, 128], F32)
    fw.dma("sync", pl32[:], plin_d, writes=[pl32])
    plb = fw.sb("plb", [128, 2, 128], BF16)
    o.cp("vector", plb, plb[:], pl32, pl32[:])
    invc = fw.sb("invc", [128, 2, 2, 16], F32)
    fw.dma("sync", invc[:], invc_d, writes=[invc])
    Amod = fw.sb("Amod", [128, 2, 8], F32)
    for si, nm in enumerate(["sc_l", "sc_c"]):
        c0, _ = PV[nm]
        g0, _ = PV["g1"]
        o.stt(Amod, Amod[:, si, :], pvec, pvec[:, c0:c0 + 8], 1.0, pvec, pvec[:, g0:g0 + 8], ALU.add, ALU.mult)
    wb = fw.sb("wb", [128, 8, INW], BF16)
    stg = Pool(fw, "wstg", [128, INW // 2], F32, 2)
    for k in range(8):
        for hf in range(2):
            s = stg.next()
            c0_ = hf * (INW // 2)
            fw.dma("sync" if hf == 0 else "gpsimd", s[:], win_d[k * 128:(k + 1) * 128, c0_:c0_ + INW // 2], writes=[s])
            o.cp("gpsimd" if hf == 0 else "vector", wb, wb[:, k, c0_:c0_ + INW // 2], s, s[:])

    xin = Pool(fw, "xin", [128, 8, NW], F32, 2)
    sqp = Pool(fw, "sq", [128, NW], BF16, 2)
    rstdp = Pool(fw, "rstd", [128, NW], F32, 2)
    up = Pool(fw, "u", [128, 8, NW], BF16, 2)
    tmpn = Pool(fw, "tmpn", [128, NW], F32, 2)
    zp = Pool(fw, "z", [128, NW], F32, 3)
    psA = Pool(fw, "psA", [128, 1024], F32, 2, space="ps")
    psB = Pool(fw, "psB", [128, 512], F32, 3, space="ps")
    sA = Pool(fw, "sA", [128, NW], F32, 2)
    sB = Pool(fw, "sB", [128, NW], F32, 2)
    dpool = Pool(fw, "dpl", [128, NCH], BF16, 2)
    ob16 = Pool(fw, "ob16", [128, NCH], BF16, 4)
    of32 = Pool(fw, "of32", [128, NCH], F32, 3)
    cvp = Pool(fw, "cv", [128, NCH], F32, 4)
    ropeCp = Pool(fw, "rC", [128, NCH], F32, 2)
    ropeSp = Pool(fw, "rS", [128, NCH], F32, 2)
    qf = Pool(fw, "qf", [128, NCH], F32, 2)
    qsq = Pool(fw, "qsq", [128, NCH], BF16, 2)
    qr = Pool(fw, "qr", [128, NCH], F32, 2)
    qn = Pool(fw, "qn", [128, NCH], BF16, 2)
    t1p = Pool(fw, "t1", [128, NCH], F32, 2)
    t2p = Pool(fw, "t2", [128, NCH], F32, 2)
    vout = Pool(fw, "vout", [128, 512], BF16, 2)
    out_evs = []
    stq = ["gpsimd"]

    def store(dst_ap, buf, ap):
        out_evs.append(fw.dma("gpsimd", dst_ap, ap, reads=[buf]))

    def chunk(seg, src, c0, N, first, last, col0, rope_c0):
        W = N + 16
        x = xin.next()
        for k in range(8):
            fw.dma("sync", x[:, k, 0:W], src[k * 128:(k + 1) * 128, c0:c0 + W], writes=[x])
        pst = psA.next()
        for k in range(8):
            sq = sqp.next()
            o.act(sq, sq[:, 0:W], x, x[:, k, 0:W], AF.Square)
            a = min(W, 512)
            o.mm(pst, pst[:, 0:a], cmb, cmb[:, 0, :], sq, sq[:, 0:a], start=(k == 0), stop=(k == 7))
            if W > 512:
                o.mm(pst, pst[:, 512:W], cmb, cmb[:, 0, :], sq, sq[:, 512:W], start=(k == 0), stop=(k == 7))
        rstd = rstdp.next()
        o.act(rstd, rstd[:, 0:W], pst, pst[:, 0:W], AF.Sqrt, bias=pv("eps"), scale=1.0, extra_r=[pvec])
        o.recip(rstd, rstd[:, 0:W], rstd, rstd[:, 0:W])
        u = up.next()
        shn = "sh_l" if seg == 0 else "sh_c"
        for k in range(8):
            t = tmpn.next()
            o.stt(t, t[:, 0:W], x, x[:, k, 0:W], Amod[:, seg, k:k + 1], rstd, rstd[:, 0:W], ALU.mult, ALU.mult,
                  extra_r=[Amod])
            o.act(u, u[:, k, 0:W], t, t[:, 0:W], AF.Identity, bias=pv(shn, k), scale=1.0, extra_r=[pvec])

        def proj(n, c_lo, c_hi):
            p = psA.next()
            w = c_hi - c_lo
            for k in range(8):
                a = min(w, 512)
                o.mm(p, p[:, 0:a], wb, wb[:, k, n * 128:(n + 1) * 128], u, u[:, k, c_lo:c_lo + a],
                     start=(k == 0), stop=(k == 7))
                if w > 512:
                    o.mm(p, p[:, 512:512 + w - 512], wb, wb[:, k, n * 128:(n + 1) * 128], u,
                         u[:, k, c_lo + 512:c_hi], start=(k == 0), stop=(k == 7))
            return p

        def evac_masked(n):
            p = proj(n, 0, W)
            z = zp.next()
            o.act(z, z[:, 0:W], p, p[:, 0:W], AF.Copy)
            if first:
                o.ts("vector", z, z[:, 0:8], z, z[:, 0:8], pv("mL") if seg == 0 else pv("zero"), None, ALU.mult,
                     extra_r=[pvec])
            if last:
                o.ts("vector", z, z[:, N + 8:W], z, z[:, N + 8:W], pv("mR") if seg == 0 else pv("zero"), None,
                     ALU.mult, extra_r=[pvec])
            return z

        for pt in range(2):
            z = evac_masked(pt)
            a = sA.next()
            b = sB.next()
            o.tt("vector", a, a[:, 1:W], z, z[:, 0:W - 1], z, z[:, 1:W], ALU.add)
            o.tt("vector", b, b[:, 2:W - 1], a, a[:, 1:W - 2], a, a[:, 3:W], ALU.add)
            if pt == 0:
                lo, hi, wl, wh = a, b, 2.0, 4.0
            else:
                a2 = sA.next()
                o.tt("vector", a2, a2[:, 4:W - 3], b, b[:, 2:W - 5], b, b[:, 6:W - 1], ALU.add)
                b2 = sB.next()
                o.tt("vector", b2, b2[:, 8:W - 7], a2, a2[:, 4:W - 11], a2, a2[:, 12:W - 3], ALU.add)
                lo, hi, wl, wh = a2, b2, 8.0, 16.0
            d = dpool.next()
            dd = of32.next()
            o.stt(dd, dd[0:64, 0:N], lo, lo[0:64, 8:N + 8], 1.0 / wl, z, z[0:64, 8:N + 8], ALU.mult, ALU.subtract)
            o.stt(dd, dd[64:128, 0:N], hi, hi[64:128, 8:N + 8], 1.0 / wh, z, z[64:128, 8:N + 8], ALU.mult,
                  ALU.subtract)
            for (flag, cs, ic0) in ((first, 0, 0), (last, N - 8, 8)):
                if not flag:
                    continue
                for (src_b, r0, r1) in ((lo, 0, 64), (hi, 64, 128)):
                    o.tt("vector", dd, dd[r0:r1, cs:cs + 8], src_b, src_b[r0:r1, cs + 8:cs + 16], invc,
                         invc[r0:r1, seg, pt, ic0:ic0 + 8], ALU.mult)
                    o.tt("vector", dd, dd[r0:r1, cs:cs + 8], dd, dd[r0:r1, cs:cs + 8], z, z[r0:r1, cs + 8:cs + 16],
                         ALU.subtract)
            o.cp("gpsimd", d, d[:, 0:N], dd, dd[:, 0:N])
            pp = psB.next()
            o.mm(pp, pp[:, 0:N], plb, plb[:, pt, :], d, d[:, 0:N])
            yb = ob16.next()
            o.act(yb, yb[:, 0:N], pp, pp[:, 0:N], AF.Identity, bias=pv("zero"), scale=pv("psc", pt), extra_r=[pvec])
            store(ypool_d[pt * 128:(pt + 1) * 128, col0:col0 + N], yb, yb[:, 0:N])

        conv = {}
        for ht in range(6):
            z = evac_masked(2 + ht)
            c = cvp.next()
            cw0, _ = PV["cw"]
            o.act(c, c[:, 0:N], z, z[:, 7:N + 7], AF.Identity, bias=pv("cb", ht), scale=pv("cw", ht * 3 + 0),
                  extra_r=[pvec])
            o.stt(c, c[:, 0:N], z, z[:, 8:N + 8], pv("cw", ht * 3 + 1), c, c[:, 0:N], ALU.mult, ALU.add,
                  extra_r=[pvec])
            o.stt(c, c[:, 0:N], z, z[:, 9:N + 9], pv("cw", ht * 3 + 2), c, c[:, 0:N], ALU.mult, ALU.add,
                  extra_r=[pvec])
            if ht < 2:
                xb = ob16.next()
                o.cp("gpsimd", xb, xb[:, 0:N], c, c[:, 0:N])
                store(x0c_d[ht * 128:(ht + 1) * 128, col0:col0 + N], xb, xb[:, 0:N])
            elif ht < 4:
                conv[ht] = c
            else:
                x1 = conv[ht - 2]
                vv = of32.next()
                o.tt("vector", vv, vv[:, 0:N], c, c[:, 0:N], x1, x1[:, 0:N], ALU.mult)
                store(v2_d[(ht - 4) * 128:(ht - 3) * 128, col0:col0 + N], vv, vv[:, 0:N])

        if seg == 0:
            rc = ropeCp.next()
            rs = ropeSp.next()
            fw.dma("sync", rc[:, 0:N], ropeC_d[:, rope_c0:rope_c0 + N], writes=[rc])
            fw.dma("sync", rs[:, 0:N], ropeS_d[:, rope_c0:rope_c0 + N], writes=[rs])
        for qt in range(8):
            p = proj(8 + qt, 8, N + 8)
            f = qf.next()
            o.act(f, f[:, 0:N], p, p[:, 0:N], AF.Copy)
            s = qsq.next()
            o.act(s, s[:, 0:N], p, p[:, 0:N], AF.Square)
            pss = psB.next()
            o.mm(pss, pss[:, 0:N], cmb, cmb[:, 1, :], s, s[:, 0:N])
            r = qr.next()
            o.act(r, r[:, 0:N], pss, pss[:, 0:N], AF.Sqrt, bias=pv("eps"), scale=1.0, extra_r=[pvec])
            o.recip(r, r[:, 0:N], r, r[:, 0:N])
            gname = "gq" if qt < 4 else "gk"
            dst = q_d if qt < 4 else k_d
            hh = qt % 4
            if seg == 0:
                n_ = qn.next()
                o.stt(n_, n_[:, 0:N], f, f[:, 0:N], pv(gname), r, r[:, 0:N], ALU.mult, ALU.mult, extra_r=[pvec])
                pp2 = psB.next()
                o.mm(pp2, pp2[:, 0:N], cmb, cmb[:, 2, :], n_, n_[:, 0:N])
                t1 = t1p.next()
                o.tt("gpsimd", t1, t1[:, 0:N], n_, n_[:, 0:N], rc, rc[:, 0:N], ALU.mult)
                t2 = t2p.next()
                o.tt("vector", t2, t2[:, 0:N], pp2, pp2[:, 0:N], rs, rs[:, 0:N], ALU.mult)
                qo = ob16.next()
                o.tt("gpsimd", qo, qo[:, 0:N], t1, t1[:, 0:N], t2, t2[:, 0:N], ALU.add)
            else:
                qo = ob16.next()
                o.stt(qo, qo[:, 0:N], f, f[:, 0:N], pv(gname), r, r[:, 0:N], ALU.mult, ALU.mult, extra_r=[pvec])
            store(dst[hh * 128:(hh + 1) * 128, col0:col0 + N], qo, qo[:, 0:N])

        for tb in range(N // 128):
            p = psB.next()
            for k in range(8):
                o.mm(p, p[:, :], u, u[:, k, 8 + tb * 128:8 + (tb + 1) * 128], wb, wb[:, k, 2048:2560],
                     start=(k == 0), stop=(k == 7))
            vb = vout.next()
            o.act(vb, vb[:, :], p, p[:, :], AF.Copy)
            if vsplit:
                for hh_ in range(4):
                    store(v_d[hh_, col0 + tb * 128:col0 + (tb + 1) * 128, :], vb, vb[:, hh_ * 128:(hh_ + 1) * 128])
            else:
                store(v_d[col0 + tb * 128:col0 + (tb + 1) * 128, :], vb, vb[:, :])

    nlc = TL // NCH
    for ci in range(nlc):
        chunk(0, xT, ci * NCH, NCH, ci == 0, ci == nlc - 1, ci * NCH, ci * NCH)
    chunk(1, hT, 0, TC, True, True, TL, 0)
    return env.done(out_evs)


NPV2 = 5


def build_p2(L, C=256, with_ctx=True, NQ=512, env=None, nq=None):
    env = env or Env()
    nc, fw, o = env.nc, env.fw, env.o
    LQ = L + (C if with_ctx else 0)
    LK = C + L
    nkt = LK // 128
    if nq is None:
        q_d = env.io("qT", [128, LQ], BF16, "ExternalInput")
        k_d = env.io("kT", [128, LK], BF16, "ExternalInput")
        v_d = env.io("v", [LK, 128], BF16, "ExternalInput")
        out_d = env.io("attT", [128, LQ], BF16, "ExternalOutput")
        qsrc = lambda q0, N: q_d[:, q0:q0 + N]
        ksrcs = [(c0, min(LK, c0 + 2048), k_d[:, c0:min(LK, c0 + 2048)]) for c0 in range(0, LK, 2048)]
        vsrc_ = v_d.rearrange("(n p) d -> p n d", p=128)
        vsrcs = [(n0, min(nkt, n0 + 16), vsrc_[:, n0:min(nkt, n0 + 16), :]) for n0 in range(0, nkt, 16)]
        odst = lambda q0, N: [out_d[:, q0:q0 + N]]
    else:
        TLq = L // nq
        Tq = TLq + C
        q_r = env.io("q_recv", [nq, 128, Tq], BF16, "ExternalInput")
        k_r = env.io("k_recv", [nq, 128, Tq], BF16, "ExternalInput")
        v_r = env.io("v_recv", [nq, Tq, 128], BF16, "ExternalInput")
        a_s = env.io("att_send", [nq, 128, Tq], BF16, "ExternalOutput")
        def qsrc(q0, N):
            if q0 >= L:
                return q_r[0, :, TLq:TLq + N]
            return q_r[q0 // TLq, :, q0 % TLq:q0 % TLq + N]
        ksrcs = [(0, C, k_r[0, :, TLq:Tq])]
        vsrcs = [(0, C // 128, v_r[0, TLq:Tq, :].rearrange("(n p) d -> p n d", p=128))]
        for qd_ in range(nq):
            for c0 in range(0, TLq, 2048):
                c1 = min(TLq, c0 + 2048)
                ksrcs.append((C + qd_ * TLq + c0, C + qd_ * TLq + c1, k_r[qd_, :, c0:c1]))
                vsrcs.append(((C + qd_ * TLq + c0) // 128, (C + qd_ * TLq + c1) // 128,
                              v_r[qd_, c0:c1, :].rearrange("(n p) d -> p n d", p=128)))
        def odst(q0, N):
            if q0 >= L:
                return [a_s[qd_, :, TLq:TLq + N] for qd_ in range(nq)]
            return [a_s[q0 // TLq, :, q0 % TLq:q0 % TLq + N]]
    dl_d = env.io("dlam", [128, 4, 64], F32, "ExternalInput")
    pv_d = env.io("pvec", [128, NPV2], F32, "ExternalInput")
    id_d = env.io("ident", [128, 128], F32, "ExternalInput")
    pvec = fw.sb("pvec", [128, NPV2], F32)
    fw.dma("sync", pvec[:], pv_d, writes=[pvec])
    id32 = fw.sb("id32", [128, 128], F32)
    fw.dma("sync", id32[:], id_d, writes=[id32])
    idb = fw.sb("idb", [128, 128], BF16)
    o.cp("vector", idb, idb[:], id32, id32[:])
    dl = fw.sb("dl", [128, 4, 64], F32)
    fw.dma("sync", dl[:], dl_d, writes=[dl])
    pr = fw.sb("pr", [128, 2, 64], F32)
    o.tt("vector", pr, pr[:, 0, :], dl, dl[:, 0, :], dl, dl[:, 1, :], ALU.mult)
    o.tt("vector", pr, pr[:, 1, :], dl, dl[:, 2, :], dl, dl[:, 3, :], ALU.mult)
    sm = fw.sb("sm", [128, 2], F32)
    fw.op("vector", lambda e: e.reduce_sum(out=sm[:], in_=pr[:], axis=AX.X), [pr], [sm])
    ex = fw.sb("ex", [128, 2], F32)
    o.act(ex, ex[:], sm, sm[:], AF.Exp)
    neglam = fw.sb("neglam", [128, 1], F32)
    o.tt("vector", neglam, neglam[:], ex, ex[:, 1:2], ex, ex[:, 0:1], ALU.subtract)
    o.tt("vector", neglam, neglam[:], neglam, neglam[:], pvec, pvec[:, 1:2], ALU.subtract)
    gsc = fw.sb("gsc", [128, 1], F32)
    o.tt("vector", gsc, gsc[:], pvec, pvec[:, 2:3], pvec, pvec[:, 3:4], ALU.mult)

    kT = fw.sb("kT", [128, LK], BF16)
    for (c0, c1, src_) in ksrcs:
        fw.dma("sync", kT[:, c0:c1], src_, writes=[kT])
    vt = fw.sb("vt", [128, nkt, 129], BF16)
    o.memset("gpsimd", vt, vt[:, :, 128:129], 1.0)
    for (n0, n1, src_) in vsrcs:
        fw.dma("gpsimd", vt[:, n0:n1, 0:128], src_, writes=[vt])

    qp = Pool(fw, "q", [128, NQ], BF16, 2)
    psS = Pool(fw, "psS", [128, 512], F32, 4, space="ps")
    accb = [fw.ps("acc%d" % i, [128, 512], F32) for i in range(3)]
    trb = fw.ps("trb", [128, 512], BF16)
    Pp = Pool(fw, "P", [128, NQ], BF16, 4)
    small = Pool(fw, "small", [128, 8], F32, 4)
    o1p = Pool(fw, "o1", [128, 128], F32, 2)
    o2p = Pool(fw, "o2", [128, 128], F32, 2)
    junk = Pool(fw, "junk", [128, 128], F32, 2)
    onp = Pool(fw, "on", [128, 128], BF16, 2)
    outp = Pool(fw, "outp", [128, NQ], BF16, 2)
    out_evs = []

    def qchunk(q0, N, kts):
        q = qp.next()
        fw.dma("sync", q[:, 0:N], qsrc(q0, N), writes=[q])
        nqb = N // 128
        started = set()
        steps = [(ki, kt, m) for ki, kt in enumerate(kts) for m in range(2)]
        Ps = {}

        def emit_S(i):
            ki, kt, m = steps[i]
            ps = psS.next()
            o.mm(ps, ps[:, 0:N], kT, kT[64 * m:64 * m + 64, kt * 128:(kt + 1) * 128], q, q[64 * m:64 * m + 64, 0:N])
            P = Pp.next()
            o.act(P, P[:, 0:N], ps, ps[:, 0:N], AF.Exp, scale=0.125)
            Ps[i] = P

        def emit_PV(i):
            ki, kt, m = steps[i]
            P = Ps.pop(i)
            for qb in range(nqb):
                a = m * 4 + qb
                bank, slot = a // 3, a % 3
                ab = accb[bank]
                st = bank not in started
                started.add(bank)
                o.mm(ab, ab[:, slot * 129:(slot + 1) * 129], P, P[:, qb * 128:(qb + 1) * 128], vt, vt[:, kt, :],
                     start=st, stop=(ki == len(kts) - 1), skip_group_check=True)

        LOOK = 2
        for i in range(min(LOOK, len(steps))):
            emit_S(i)
        for i in range(len(steps)):
            if i + LOOK < len(steps):
                emit_S(i + LOOK)
            emit_PV(i)
        for qb in range(nqb):
            a0, a1 = qb, 4 + qb
            A0 = accb[a0 // 3]; s0 = (a0 % 3) * 129
            A1 = accb[a1 // 3]; s1 = (a1 % 3) * 129
            sm_ = small.next()
            o.recip(sm_, sm_[:, 0:1], A0, A0[:, s0 + 128:s0 + 129])
            o.recip(sm_, sm_[:, 1:2], A1, A1[:, s1 + 128:s1 + 129])
            o.tt("vector", sm_, sm_[:, 2:3], sm_, sm_[:, 1:2], neglam, neglam[:], ALU.mult)
            o1 = o1p.next()
            o.ts("vector", o1, o1[:], A0, A0[:, s0:s0 + 128], sm_[:, 0:1], None, ALU.mult, extra_r=[sm_])
            o2 = o2p.next()
            o.stt(o2, o2[:], A1, A1[:, s1:s1 + 128], sm_[:, 2:3], o1, o1[:], ALU.mult, ALU.add, extra_r=[sm_])
            jk = junk.next()
            o.act(jk, jk[:], o2, o2[:], AF.Square, accum=(sm_, sm_[:, 3:4]))
            o.act(sm_, sm_[:, 4:5], sm_, sm_[:, 3:4], AF.Sqrt, bias=pvec[:, 0:1], scale=1.0 / 128.0, extra_r=[pvec])
            o.recip(sm_, sm_[:, 5:6], sm_, sm_[:, 4:5])
            on = onp.next()
            o.ts("vector", on, on[:], o2, o2[:], sm_[:, 5:6], None, ALU.mult, extra_r=[sm_])
            o.tr(trb, trb[:, qb * 128:(qb + 1) * 128], on, on[:], idb, idb[:])
        ot = outp.next()
        o.act(ot, ot[:, 0:N], trb, trb[:, 0:N], AF.Identity, bias=pvec[:, 4:5], scale=gsc[:, 0:1], extra_r=[pvec, gsc])
        for dst_ in odst(q0, N):
            out_evs.append(fw.dma("gpsimd", dst_, ot[:, 0:N], reads=[ot]))

    allk = list(range(nkt))
    for qc in range(L // NQ):
        qchunk(qc * NQ, NQ, allk)
    if with_ctx:
        qchunk(L, C, list(range(C // 128)))
    return env.done(out_evs)


PI = float(np.pi)


def lc_tables(L):
    M = L // 128
    N2 = 2 * M
    N = 2 * L
    kb = min(128, N2)
    nb2 = (N2 + 127) // 128
    f64 = np.float64
    n2 = np.arange(M, dtype=f64)[:, None]; k2 = np.arange(N2, dtype=f64)[None, :]
    ang = 2 * np.pi * n2 * k2 / N2
    F2 = np.concatenate([np.cos(ang), -np.sin(ang)], 1)
    n1 = np.arange(128, dtype=f64)[:, None]
    ang = 2 * np.pi * n1 * k2 / N
    Tr, Ti = np.cos(ang), -np.sin(ang)
    TA = np.concatenate([Tr, Tr], 1); TB = np.concatenate([Ti, Ti], 1)
    k1 = np.arange(128, dtype=f64)[None, :]
    ang = 2 * np.pi * n1 * k1 / 128
    Fr, Fi = np.cos(ang), -np.sin(ang)
    F1 = np.stack([Fr, Fi, -Fi], 1)
    Gr, Gi = np.cos(ang), np.sin(ang)
    G1 = np.stack([np.concatenate([Gr, Gi], 1), np.concatenate([-Gi, Gr], 1)], 1)
    T2 = np.zeros((kb, nb2, 2, 256)); G2 = np.zeros((kb, nb2, 2, M))
    for j in range(nb2):
        kk = (np.arange(kb, dtype=f64) + j * 128)[:, None]
        ang = 2 * np.pi * kk * np.arange(128, dtype=f64)[None, :] / N
        T2[:, j, 0] = np.concatenate([np.cos(ang), np.cos(ang)], 1)
        T2[:, j, 1] = np.concatenate([np.sin(ang), np.sin(ang)], 1)
        ang = 2 * np.pi * kk * np.arange(M, dtype=f64)[None, :] / N2
        G2[:, j, 0] = np.cos(ang) / N
        G2[:, j, 1] = -np.sin(ang) / N
    f = lambda a: np.ascontiguousarray(a.astype(np.float32))
    return {"F2": f(F2), "TA": f(TA), "TB": f(TB), "F1": f(F1), "G1": f(G1), "T2": f(T2), "G2": f(G2)}


def filt_tables(L, ch0, nch=32):
    f32 = np.float32
    t = np.linspace(0.0, 1.0, L, dtype=f32)[:, None]
    w = (f32(2.0 * np.pi / L) * np.arange(L, dtype=f32))[:, None]
    bands = np.linspace(1e-4, 15, 16, dtype=f32)[None, :]
    feat = np.concatenate([t, np.cos(bands * w), -np.sin(bands * w)], -1).astype(f32)
    max_decay = np.log(1.0 / 1e-2) / 0.3
    min_decay = np.log(1.0 / 1e-2) / 1.5
    deltas = np.linspace(min_decay, max_decay, 256, dtype=f32)
    dec = np.exp(-t * deltas[None, ch0:ch0 + nch]).astype(f32)
    dec2 = np.concatenate([dec, dec], 1).T
    return np.ascontiguousarray(feat.T), np.ascontiguousarray(dec2)


def build_p3(Ls=(16384, 256), nch=32, env=None, nq=None):
    env = env or Env()
    nc, fw, o = env.nc, env.fw, env.o
    R = 2 * nch
    w1_d = env.io("w1", [33, 64], F32, "ExternalInput")
    w2_d = env.io("w2", [64, 64], F32, "ExternalInput")
    w3_d = env.io("w3s", [64, R], F32, "ExternalInput")
    fpv_d = env.io("fpv", [64, 4], F32, "ExternalInput")
    sel_d = env.io("sel", [R, nch], F32, "ExternalInput")
    w1 = fw.sb("w1", [33, 64], F32); w2 = fw.sb("w2", [64, 64], F32); w3s = fw.sb("w3s", [64, R], F32)
    fpv = fw.sb("fpv", [64, 6], F32); sel = fw.sb("sel", [R, nch], F32)
    fw.dma("sync", w1[:], w1_d, writes=[w1]); fw.dma("sync", w2[:], w2_d, writes=[w2])
    fw.dma("sync", w3s[:], w3_d, writes=[w3s]); fw.dma("sync", fpv[:, 0:4], fpv_d, writes=[fpv])
    fw.dma("sync", sel[:], sel_d, writes=[sel])
    o.tt("vector", fpv, fpv[:, 4:5], fpv, fpv[:, 0:1], fpv, fpv[:, 1:2], ALU.mult)
    o.tt("vector", fpv, fpv[:, 5:6], fpv, fpv[:, 2:3], fpv, fpv[:, 3:4], ALU.mult)
    ones = fw.sb("ones", [128, 128], F32)
    o.memset("vector", ones, ones[:], 1.0)

    PS = Pool(fw, "PS", [128, 512], F32, 6, space="ps")
    featp = Pool(fw, "feat", [33, 512], F32, 2)
    decp = Pool(fw, "dec", [R, 512], F32, 2)
    prep = Pool(fw, "pre", [64, 512], F32, 2)
    mkp = Pool(fw, "mk", [64, 512], F32, 2)
    hp = Pool(fw, "hh", [64, 512], F32, 3)
    kp = Pool(fw, "kk", [R, 512], F32, 2)
    jkp = Pool(fw, "jk", [R, 512], F32, 2)
    xin = Pool(fw, "xin", [128, 128], F32, 6)
    sP = Pool(fw, "sP", [128, 512], F32, 3)
    sA = Pool(fw, "sAA", [128, 512], F32, 3)
    sB = Pool(fw, "sBB", [128, 512], F32, 3)
    sZ = Pool(fw, "sZ", [128, 512], F32, 3)
    sK = Pool(fw, "sK", [128, 2, 512], F32, 2)
    sXa = Pool(fw, "sXa", [128, 512], F32, 2)
    sQ = Pool(fw, "sQ", [128, 256], F32, 3)
    sQA = Pool(fw, "sQA", [128, 256], F32, 3)
    sQB = Pool(fw, "sQB", [128, 256], F32, 3)
    sZ2 = Pool(fw, "sZ2", [128, 2, 256], F32, 2)
    sY = Pool(fw, "sY", [128, 128], F32, 3)
    out_evs = []

    for L in Ls:
        sfx = "_%d" % L
        M = L // 128
        N2 = 2 * M
        W2 = 2 * N2
        kb = min(128, N2)
        nb2 = (N2 + 127) // 128
        d = {}
        for nm, shp in (("F2", [M, W2]), ("TA", [128, W2]), ("TB", [128, W2]), ("F1", [128, 3, 128]),
                        ("G1", [128, 2, 256]), ("T2", [kb, nb2, 2, 256]), ("G2", [kb, nb2, 2, M])):
            d[nm] = env.io(nm + sfx, shp, F32, "ExternalInput")
        feat_d = env.io("feat" + sfx, [33, L], F32, "ExternalInput")
        dec_d = env.io("dec" + sfx, [R, L], F32, "ExternalInput")
        if nq is None:
            v_d = env.io("vin" + sfx, [R, L], F32, "ExternalInput")
            y_d = env.io("yout" + sfx, [R, L], F32, "ExternalOutput")
        else:
            Lmain = Ls[0]
            TLq = Lmain // nq
            Tq = TLq + Ls[1]
            v2r = env.io("v2_recv", [8, nch, Tq], F32, "ExternalInput")
            cvs = env.io("conv_send", [8, nch, Tq], F32, "ExternalOutput")
        kf = fw.dram("kf" + sfx, [R, L], F32)
        tb = {}
        for nm in d:
            tb[nm] = fw.sb("t" + nm + sfx, list(d[nm].shape), F32)
            fw.dma("sync", tb[nm][:], d[nm], writes=[tb[nm]])
        CH = min(512, L)
        nchk = L // CH
        rs = fw.sb("rs" + sfx, [R, nchk], F32)

        def sin_layer(ps, fcol, bcol):
            pre = prep.next()
            o.ts("vector", pre, pre[:, 0:CH], ps, ps[0:64, 0:CH], fpv[:, fcol:fcol + 1], fpv[:, bcol:bcol + 1],
                 ALU.mult, ALU.add, extra_r=[fpv])
            mk = mkp.next()
            o.ts("gpsimd", mk, mk[:, 0:CH], pre, pre[:, 0:CH], PI, None, ALU.is_gt)
            o.stt(pre, pre[:, 0:CH], mk, mk[:, 0:CH], -2.0 * PI, pre, pre[:, 0:CH], ALU.mult, ALU.add)
            mk2 = mkp.next()
            o.ts("gpsimd", mk2, mk2[:, 0:CH], pre, pre[:, 0:CH], -PI, None, ALU.is_lt)
            o.stt(pre, pre[:, 0:CH], mk2, mk2[:, 0:CH], 2.0 * PI, pre, pre[:, 0:CH], ALU.mult, ALU.add)
            h = hp.next()
            o.act(h, h[:, 0:CH], pre, pre[:, 0:CH], AF.Sin)
            return h

        for ci in range(nchk):
            c0 = ci * CH
            ft = featp.next()
            fw.dma("sync", ft[:, 0:CH], feat_d[:, c0:c0 + CH], writes=[ft])
            dc = decp.next()
            fw.dma("sync", dc[:, 0:CH], dec_d[:, c0:c0 + CH], writes=[dc])
            ps = PS.next()
            o.mm(ps, ps[0:64, 0:CH], w1, w1[:, :], ft, ft[:, 0:CH])
            h1 = sin_layer(ps, 0, 4)
            ps = PS.next()
            o.mm(ps, ps[0:64, 0:CH], w2, w2[:, :], h1, h1[:, 0:CH])
            h2 = sin_layer(ps, 2, 5)
            ps = PS.next()
            o.mm(ps, ps[0:R, 0:CH], w3s, w3s[:, :], h2, h2[:, 0:CH])
            k = kp.next()
            o.tt("vector", k, k[:, 0:CH], ps, ps[0:R, 0:CH], dc, dc[:, 0:CH], ALU.mult)
            if ci == 0:
                o.memset("vector", k, k[nch:R, 0:1], 0.0)
            j = jkp.next()
            o.act(j, j[:, 0:CH], k, k[:, 0:CH], AF.Abs, accum=(rs, rs[:, ci:ci + 1]))
            fw.dma("gpsimd", kf[:, c0:c0 + CH], k[:, 0:CH], reads=[k], writes=[kf])
        rtot = fw.sb("rtot" + sfx, [R, 1], F32)
        fw.op("vector", lambda e, rtot=rtot, rs=rs: e.reduce_sum(out=rtot[:], in_=rs[:], axis=AX.X), [rs], [rtot])
        Rb = fw.sb("Rb" + sfx, [R, 128], F32)
        o.ts("vector", Rb, Rb[:], ones, ones[0:R, :], rtot[:, 0:1], None, ALU.mult, extra_r=[rtot])
        pss = PS.next()
        o.mm(pss, pss[:, 0:nch], Rb, Rb[:, :], sel, sel[:, :])
        Sinv = fw.sb("Sinv" + sfx, [128, 2, nch], F32)
        o.recip(Sinv, Sinv[:, 0, :], pss, pss[:, 0:nch])
        o.ts("vector", Sinv, Sinv[:, 1, :], Sinv, Sinv[:, 0, :], -1.0, None, ALU.mult)

        kfv = kf.t.ap().rearrange("r (a i) -> r a i", i=128)
        if nq is None:
            vv = v_d.rearrange("r (a i) -> r a i", i=128)
            yv = y_d.rearrange("r (a i) -> r a i", i=128)
            vin_parts = lambda r: [(0, M, vv[r])]
            yout_parts = lambda r: [(0, M, yv[r])]
        elif L == Ls[0]:
            mq = M // nq
            def vin_parts(r, v2r=v2r, mq=mq, TLq=TLq):
                b_, c_ = r // nch, r % nch
                return [(qd_ * mq, (qd_ + 1) * mq, v2r[b_ * nq + qd_, c_, 0:TLq].rearrange("(a i) -> a i", i=128))
                        for qd_ in range(nq)]
            def yout_parts(r, cvs=cvs, mq=mq, TLq=TLq):
                b_, c_ = r // nch, r % nch
                return [(qd_ * mq, (qd_ + 1) * mq, cvs[b_ * nq + qd_, c_, 0:TLq].rearrange("(a i) -> a i", i=128))
                        for qd_ in range(nq)]
        else:
            def vin_parts(r, v2r=v2r, TLq=TLq, Tq=Tq, M=M):
                b_, c_ = r // nch, r % nch
                return [(0, M, v2r[b_ * nq, c_, TLq:Tq].rearrange("(a i) -> a i", i=128))]
            def yout_parts(r, cvs=cvs, TLq=TLq, Tq=Tq, M=M):
                b_, c_ = r // nch, r % nch
                return [(0, M, cvs[b_ * nq + qd_, c_, TLq:Tq].rearrange("(a i) -> a i", i=128)) for qd_ in range(nq)]

        def fwd(src_parts, src_bufs):
            x = xin.next()
            for (p0_, p1_, ap_) in src_parts:
                fw.dma("sync", x[p0_:p1_, :], ap_, reads=src_bufs, writes=[x])
            P = PS.next()
            o.mm(P, P[:, 0:W2], x, x[0:M, :], tb["F2"], tb["F2"][:, :])
            Ps = sP.next()
            o.act(Ps, Ps[:, 0:W2], P, P[:, 0:W2], AF.Copy)
            A = sA.next(); B = sB.next()
            o.tt("vector", A, A[:, 0:W2], Ps, Ps[:, 0:W2], tb["TA"], tb["TA"][:, :], ALU.mult)
            o.tt("gpsimd", B, B[:, 0:W2], Ps, Ps[:, 0:W2], tb["TB"], tb["TB"][:, :], ALU.mult)
            Z = sZ.next()
            o.tt("vector", Z, Z[:, 0:N2], A, A[:, 0:N2], B, B[:, N2:W2], ALU.subtract)
            o.tt("gpsimd", Z, Z[:, N2:W2], B, B[:, 0:N2], A, A[:, N2:W2], ALU.add)
            X = PS.next()
            F1 = tb["F1"]
            o.mm(X, X[:, 0:N2], F1, F1[:, 0, :], Z, Z[:, 0:N2], start=True, stop=False)
            o.mm(X, X[:, 0:N2], F1, F1[:, 2, :], Z, Z[:, N2:W2], start=False, stop=True)
            o.mm(X, X[:, N2:W2], F1, F1[:, 1, :], Z, Z[:, 0:N2], start=True, stop=False, skip_group_check=True)
            o.mm(X, X[:, N2:W2], F1, F1[:, 0, :], Z, Z[:, N2:W2], start=False, stop=True, skip_group_check=True)
            return X

        for c in range(nch):
            Xa = fwd([(0, M, kfv[c])], [kf])
            Xas = sXa.next()
            o.act(Xas, Xas[:, 0:W2], Xa, Xa[:, 0:W2], AF.Identity, bias=0.0, scale=Sinv[:, 0, c:c + 1], extra_r=[Sinv])
            Xb = fwd([(0, M, kfv[nch + c])], [kf])
            K = sK.next()
            o.stt(K, K[:, 0, 0:N2], Xb, Xb[:, 0:N2], Sinv[:, 0, c:c + 1], Xas, Xas[:, 0:N2], ALU.mult, ALU.add,
                  extra_r=[Sinv])
            o.stt(K, K[:, 1, 0:N2], Xb, Xb[:, N2:W2], Sinv[:, 1, c:c + 1], Xas, Xas[:, N2:W2], ALU.mult, ALU.add,
                  extra_r=[Sinv])
            o.cp("gpsimd", K, K[:, 0, N2:W2], K, K[:, 0, 0:N2])
            o.cp("gpsimd", K, K[:, 1, N2:W2], K, K[:, 1, 0:N2])
            for b in range(2):
                r = b * nch + c
                X = fwd(vin_parts(r), [])
                Xs = sP.next()
                o.act(Xs, Xs[:, 0:W2], X, X[:, 0:W2], AF.Copy)
                A = sA.next(); B = sB.next()
                o.tt("vector", A, A[:, 0:W2], Xs, Xs[:, 0:W2], K, K[:, 0, 0:W2], ALU.mult)
                o.tt("gpsimd", B, B[:, 0:W2], Xs, Xs[:, 0:W2], K, K[:, 1, 0:W2], ALU.mult)
                Y = sZ.next()
                o.tt("vector", Y, Y[:, 0:N2], A, A[:, 0:N2], B, B[:, N2:W2], ALU.subtract)
                o.tt("gpsimd", Y, Y[:, N2:W2], B, B[:, 0:N2], A, A[:, N2:W2], ALU.add)
                Z2 = sZ2.next()
                G1 = tb["G1"]; T2 = tb["T2"]; G2 = tb["G2"]
                for j in range(nb2):
                    Q = PS.next()
                    o.mm(Q, Q[0:kb, 0:256], Y, Y[:, j * 128:j * 128 + kb], G1, G1[:, 0, :], start=True, stop=False)
                    o.mm(Q, Q[0:kb, 0:256], Y, Y[:, N2 + j * 128:N2 + j * 128 + kb], G1, G1[:, 1, :], start=False,
                         stop=True)
                    Qs = sQ.next()
                    o.act(Qs, Qs[0:kb, :], Q, Q[0:kb, 0:256], AF.Copy)
                    QA = sQA.next(); QB = sQB.next()
                    o.tt("vector", QA, QA[0:kb, :], Qs, Qs[0:kb, :], T2, T2[:, j, 0, :], ALU.mult)
                    o.tt("gpsimd", QB, QB[0:kb, :], Qs, Qs[0:kb, :], T2, T2[:, j, 1, :], ALU.mult)
                    o.tt("vector", Z2, Z2[0:kb, j, 0:128], QA, QA[0:kb, 0:128], QB, QB[0:kb, 128:256], ALU.subtract)
                    o.tt("gpsimd", Z2, Z2[0:kb, j, 128:256], QB, QB[0:kb, 0:128], QA, QA[0:kb, 128:256], ALU.add)
                yp = PS.next()
                for j in range(nb2):
                    o.mm(yp, yp[0:M, 0:128], G2, G2[:, j, 0, :], Z2, Z2[0:kb, j, 0:128], start=(j == 0), stop=False)
                    o.mm(yp, yp[0:M, 0:128], G2, G2[:, j, 1, :], Z2, Z2[0:kb, j, 128:256], start=False,
                         stop=(j == nb2 - 1))
                ys = sY.next()
                o.act(ys, ys[0:M, :], yp, yp[0:M, 0:128], AF.Copy)
                for (p0_, p1_, ap_) in yout_parts(r):
                    if p1_ - p0_ == M and p0_ == 0:
                        out_evs.append(fw.dma("gpsimd", ap_, ys[0:M, :], reads=[ys]))
                    else:
                        out_evs.append(fw.dma("gpsimd", ap_, ys[p0_:p1_, :], reads=[ys]))
    return env.done(out_evs)


D = 1024
PV4 = {}
_c4 = 0
def _add4(name, n):
    global _c4
    PV4[name] = (_c4, n)
    _c4 += n
_add4("hyb", 2)
_add4("g2_l", 8); _add4("g2_c", 8)
_add4("n2g", 8)
_add4("sh_l", 8); _add4("sc_l", 8); _add4("sh_c", 8); _add4("sc_c", 8)
_add4("g5_l", 8); _add4("g5_c", 8)
_add4("eps", 1); _add4("zero", 1)
NPV4 = _c4


def build_p4(TL, TC=256, moe=False, NCH=512, env=None, halo=False, mv=None):
    T = TL + TC
    env = env or Env()
    nc, fw, o = env.nc, env.fw, env.o
    if not halo:
        xT = env.io("xT", [D, T], F32, "ExternalInput")
        xsrc = lambda seg, k, c0, N: xT[k * 128:(k + 1) * 128, c0:c0 + N]
    else:
        XH = env.io("XH", [D, TL + 16], F32, "ExternalInput")
        HH = env.io("HH", [D, TC + 16], F32, "ExternalInput")
        xsrc = lambda seg, k, c0, N: (XH[k * 128:(k + 1) * 128, 8 + c0:8 + c0 + N] if seg == 0 else
                                      HH[k * 128:(k + 1) * 128, 8 + c0 - TL:8 + c0 - TL + N])
    ypool_d = env.io("ypool", [256, T], BF16, "ExternalInput")
    x0c_d = env.io("x0c", [256, T], BF16, "ExternalInput")
    v2_d = env.io("v2", [256, T], F32, "ExternalInput")
    conv_d = env.io("conv", [256, T], F32, "ExternalInput")
    att_d = env.io("attT", [512, T], BF16, "ExternalInput")
    wout_d = env.io("w_out", [D, D], F32, "ExternalInput")
    pvec_d = env.io("pvec", [128, NPV4], F32, "ExternalInput")
    ones_d = env.io("onesD", [128, 128], F32, "ExternalInput")
    x1_d = env.io("x1T", [D, T], F32, "ExternalOutput")
    u2_d = env.io("u2T", [D, T], BF16, "ExternalOutput")
    if moe:
        rw_d = env.io("rw", [128, 8, 8], F32, "ExternalInput")
        id_d = env.io("ident", [128, 128], F32, "ExternalInput")
        comb_d = env.io("combT", [8, T], F32, "ExternalOutput")
    pvec = fw.sb("pvec", [128, NPV4], F32)
    fw.dma("sync", pvec[:], pvec_d, writes=[pvec])
    if mv is not None:
        mvb, l_ = mv
        for nm_, t0_, g_ in (("g2_l", 16, 0), ("g2_c", 16, 1), ("sh_l", 24, 0), ("sc_l", 32, 0), ("sh_c", 24, 1),
                             ("sc_c", 32, 1), ("g5_l", 40, 0), ("g5_c", 40, 1)):
            c0_, _ = PV4[nm_]
            o.cp("vector", pvec, pvec[:, c0_:c0_ + 8], mvb, mvb[:, l_, t0_:t0_ + 8, g_])
    def pv(name, i=0):
        c0, n = PV4[name]
        return pvec[:, c0 + i:c0 + i + 1]
    on32 = fw.sb("on32", [128, 128], F32)
    fw.dma("sync", on32[:], ones_d, writes=[on32])
    onb = fw.sb("onb", [128, 128], BF16)
    o.cp("vector", onb, onb[:], on32, on32[:])
    Amod = fw.sb("Amod", [128, 2, 8], F32)
    for si, nm in enumerate(["sc_l", "sc_c"]):
        c0, _ = PV4[nm]
        g0, _ = PV4["n2g"]
        o.stt(Amod, Amod[:, si, :], pvec, pvec[:, c0:c0 + 8], 1.0, pvec, pvec[:, g0:g0 + 8], ALU.add, ALU.mult)
    wb = fw.sb("wb", [128, 8, D], BF16)
    stg = Pool(fw, "wstg", [128, D], F32, 2)
    for k in range(8):
        s = stg.next()
        fw.dma("sync" if k % 2 == 0 else "gpsimd", s[:], wout_d[k * 128:(k + 1) * 128, :], writes=[s])
        o.cp("gpsimd" if k % 2 == 0 else "vector", wb, wb[:, k, :], s, s[:])
    if moe:
        rw = fw.sb("rw", [128, 8, 8], F32)
        fw.dma("sync", rw[:], rw_d, writes=[rw])
        ident = fw.sb("ident", [128, 128], F32)
        fw.dma("sync", ident[:], id_d, writes=[ident])

    xin = Pool(fw, "xin", [128, 8, NCH], F32, 2)
    yin = Pool(fw, "yin", [128, 8, NCH], BF16, 2)
    x0p = Pool(fw, "x0", [128, 2, NCH], BF16, 2)
    v2p = Pool(fw, "v2", [128, 2, NCH], F32, 2)
    cvp = Pool(fw, "cv", [128, 2, NCH], F32, 2)
    tmp = Pool(fw, "tmp", [128, NCH], F32, 3)
    x1p = Pool(fw, "x1", [128, 8, NCH], F32, 2)
    sqp = Pool(fw, "sq", [128, NCH], BF16, 2)
    rstdp = Pool(fw, "rstd", [128, NCH], F32, 2)
    u2f = Pool(fw, "u2f", [128, 8, NCH], F32, 1 if moe else 1)
    u2b = Pool(fw, "u2b", [128, 8, NCH], BF16, 2)
    PS = Pool(fw, "PS", [128, 512], F32, 6, space="ps")
    sm = Pool(fw, "sm", [128, 64], F32, 4)
    cbo = Pool(fw, "cbo", [8, NCH], F32, 2)
    out_evs = []

    def chunk(seg, c0, N):
        x = xin.next()
        for k in range(8):
            fw.dma("sync", x[:, k, 0:N], xsrc(seg, k, c0, N), writes=[x])
        y = yin.next()
        for k in range(2):
            fw.dma("sync", y[:, k, 0:N], ypool_d[k * 128:(k + 1) * 128, c0:c0 + N], writes=[y])
        for k in range(4):
            fw.dma("sync", y[:, 4 + k, 0:N], att_d[k * 128:(k + 1) * 128, c0:c0 + N], writes=[y])
        x0 = x0p.next(); v2 = v2p.next(); cv = cvp.next()
        for k in range(2):
            fw.dma("gpsimd", x0[:, k, 0:N], x0c_d[k * 128:(k + 1) * 128, c0:c0 + N], writes=[x0])
            fw.dma("gpsimd", v2[:, k, 0:N], v2_d[k * 128:(k + 1) * 128, c0:c0 + N], writes=[v2])
            fw.dma("gpsimd", cv[:, k, 0:N], conv_d[k * 128:(k + 1) * 128, c0:c0 + N], writes=[cv])
        for k in range(2):
            t = tmp.next()
            o.stt(t, t[:, 0:N], v2, v2[:, k, 0:N], pv("hyb", k), cv, cv[:, k, 0:N], ALU.mult, ALU.add, extra_r=[pvec])
            o.tt("vector", y, y[:, 2 + k, 0:N], t, t[:, 0:N], x0, x0[:, k, 0:N], ALU.mult)
        x1 = x1p.next()
        gn = "g2_l" if seg == 0 else "g2_c"
        for n in range(8):
            p = PS.next()
            for k in range(8):
                o.mm(p, p[:, 0:N], wb, wb[:, k, n * 128:(n + 1) * 128], y, y[:, k, 0:N], start=(k == 0), stop=(k == 7))
            o.stt(x1, x1[:, n, 0:N], p, p[:, 0:N], pv(gn, n), x, x[:, n, 0:N], ALU.mult, ALU.add, extra_r=[pvec])
            out_evs.append(fw.dma("gpsimd", x1_d[n * 128:(n + 1) * 128, c0:c0 + N], x1[:, n, 0:N], reads=[x1]))
        pst = PS.next()
        for k in range(8):
            sq = sqp.next()
            o.act(sq, sq[:, 0:N], x1, x1[:, k, 0:N], AF.Square)
            o.mm(pst, pst[:, 0:N], onb, onb[:, :], sq, sq[:, 0:N], start=(k == 0), stop=(k == 7))
        rstd = rstdp.next()
        o.act(rstd, rstd[:, 0:N], pst, pst[:, 0:N], AF.Sqrt, bias=pv("eps"), scale=1.0, extra_r=[pvec])
        o.recip(rstd, rstd[:, 0:N], rstd, rstd[:, 0:N])
        uf = u2f.next(); ub = u2b.next()
        shn = "sh_l" if seg == 0 else "sh_c"
        for k in range(8):
            t = tmp.next()
            o.stt(t, t[:, 0:N], x1, x1[:, k, 0:N], Amod[:, seg, k:k + 1], rstd, rstd[:, 0:N], ALU.mult, ALU.mult,
                  extra_r=[Amod])
            if moe:
                o.act(uf, uf[:, k, 0:N], t, t[:, 0:N], AF.Identity, bias=pv(shn, k), scale=1.0, extra_r=[pvec])
                o.cp("gpsimd", ub, ub[:, k, 0:N], uf, uf[:, k, 0:N])
            else:
                o.act(ub, ub[:, k, 0:N], t, t[:, 0:N], AF.Identity, bias=pv(shn, k), scale=1.0, extra_r=[pvec])
            out_evs.append(fw.dma("gpsimd", u2_d[k * 128:(k + 1) * 128, c0:c0 + N], ub[:, k, 0:N], reads=[ub]))
        if moe:
            cps = PS.next()
            for tb in range(N // 128):
                p = PS.next()
                for k in range(8):
                    o.mm(p, p[:, 0:8], uf, uf[:, k, tb * 128:(tb + 1) * 128], rw, rw[:, k, :], start=(k == 0),
                         stop=(k == 7))
                s = sm.next()
                o.cp("vector", s, s[:, 0:8], p, p[:, 0:8])
                fw.op("vector", lambda e, s=s: e.reduce_max(out=s[:, 8:9], in_=s[:, 0:8], axis=AX.X), [s], [s])
                o.ts("vector", s, s[:, 9:10], s, s[:, 8:9], -1.0, None, ALU.mult)
                o.ts("vector", s, s[:, 10:18], s, s[:, 0:8], s[:, 8:9], None, ALU.is_equal)
                o.stt(s, s[:, 18:26], s, s[:, 10:18], -1e30, s, s[:, 0:8], ALU.mult, ALU.add)
                fw.op("vector", lambda e, s=s: e.reduce_max(out=s[:, 26:27], in_=s[:, 18:26], axis=AX.X), [s], [s])
                o.ts("vector", s, s[:, 27:35], s, s[:, 0:8], s[:, 26:27], None, ALU.is_ge)
                o.act(s, s[:, 35:43], s, s[:, 0:8], AF.Exp, bias=s[:, 9:10], scale=1.0)
                o.tt("vector", s, s[:, 35:43], s, s[:, 35:43], s, s[:, 27:35], ALU.mult)
                fw.op("vector", lambda e, s=s: e.reduce_sum(out=s[:, 43:44], in_=s[:, 35:43], axis=AX.X), [s], [s])
                o.recip(s, s[:, 44:45], s, s[:, 43:44])
                o.ts("vector", s, s[:, 45:53], s, s[:, 35:43], s[:, 44:45], None, ALU.mult)
                o.tr(cps, cps[0:8, tb * 128:(tb + 1) * 128], s, s[:, 45:53], ident, ident[:, :])
            cb = cbo.next()
            o.act(cb, cb[:, 0:N], cps, cps[0:8, 0:N], AF.Copy)
            out_evs.append(fw.dma("gpsimd", comb_d[:, c0:c0 + N], cb[:, 0:N], reads=[cb]))

    for ci in range(TL // NCH):
        chunk(0, ci * NCH, NCH)
    chunk(1, TL, TC)
    return env.done(out_evs)


def build_p5(TL, TC=256, moe=False, NCH=512, E=None, F=None, SCN=3, env=None, halo=False, last=False, mv=None, wl=None):
    T = TL + TC
    if E is None:
        E = 8 if moe else 1
    if F is None:
        F = 3584 if moe else 2816
    env = env or Env()
    nc, fw, o = env.nc, env.fw, env.o
    x1_d = env.io("x1T", [D, T], F32, "ExternalInput")
    u2_d = env.io("u2T", [D, T], BF16, "ExternalInput")
    wg_d = env.io("wg", [E, D, F], F32, "ExternalInput")
    wu_d = env.io("wu", [E, D, F], F32, "ExternalInput")
    wd_d = env.io("wd", [E, F, D], F32, "ExternalInput")
    pvec_d = env.io("pvec", [128, NPV4], F32, "ExternalInput")
    if not halo:
        x2_d = env.io("x2T", [D, T], F32, "ExternalOutput")
        x2dst = lambda seg, n, c0, N: [x2_d[n * 128:(n + 1) * 128, c0:c0 + N]]
    else:
        XH = env.io("XH", [D, TL + 16], F32, "ExternalOutput")
        HH = env.io("HH", [D, TC + 16], F32, "ExternalOutput")
        if last:
            OUT = env.io("outT", [D, TL], F32, "ExternalOutput")
        def x2dst(seg, n, c0, N):
            if seg == 1:
                return [HH[n * 128:(n + 1) * 128, 8 + c0 - TL:8 + c0 - TL + N]]
            if last:
                return [OUT[n * 128:(n + 1) * 128, c0:c0 + N]]
            return [XH[n * 128:(n + 1) * 128, 8 + c0:8 + c0 + N]]
    if moe:
        comb_d = env.io("combT", [8, T], F32, "ExternalInput")
        sel_d = env.io("selE", [8, 8, 128], F32, "ExternalInput")
    pvec = fw.sb("pvec", [128, NPV4], F32)
    fw.dma("sync", pvec[:], pvec_d, writes=[pvec])
    if mv is not None:
        mvb, l_ = mv
        for nm_, t0_, g_ in (("g2_l", 16, 0), ("g2_c", 16, 1), ("sh_l", 24, 0), ("sc_l", 32, 0), ("sh_c", 24, 1),
                             ("sc_c", 32, 1), ("g5_l", 40, 0), ("g5_c", 40, 1)):
            c0_, _ = PV4[nm_]
            o.cp("vector", pvec, pvec[:, c0_:c0_ + 8], mvb, mvb[:, l_, t0_:t0_ + 8, g_])
    def pv(name, i=0):
        c0, n = PV4[name]
        return pvec[:, c0 + i:c0 + i + 1]
    if moe:
        selE = fw.sb("selE", [8, 8, 128], F32)
        fw.dma("sync", selE[:], sel_d, writes=[selE])
    chunks = [(0, ci * NCH, NCH) for ci in range(TL // NCH)] + [(1, TL, TC)]
    scs = []
    per = (len(chunks) + SCN - 1) // SCN
    for i in range(0, len(chunks), per):
        scs.append(chunks[i:i + per])
    maxw = max(sum(c[2] for c in sc) for sc in scs)
    maxc = max(len(sc) for sc in scs)
    yacc = fw.sb("yacc", [128, 8, maxw], F32)
    u2 = fw.sb("u2", [128, 8, maxw], BF16)
    GF = 512
    groups = []
    for e in range(E):
        for f0 in range(0, F, GF):
            groups.append((e, f0, min(GF, F - f0)))
    wgp = Pool(fw, "wgb", [128, 8, GF], BF16, 2)
    wup = Pool(fw, "wub", [128, 8, GF], BF16, 2)
    wdp = Pool(fw, "wdb", [128, GF // 128, D], BF16, 2)
    stg = Pool(fw, "stg", [128, 2048], F32, 3)
    PSg = Pool(fw, "PSg", [128, 512], F32, 2, space="ps")
    PSu = Pool(fw, "PSu", [128, 512], F32, 2, space="ps")
    PSy = Pool(fw, "PSy", [128, 512], F32, 2, space="ps")
    PSc = Pool(fw, "PSc", [128, 512], F32, 1, space="ps")
    sgp = Pool(fw, "sg", [128, NCH], F32, 2)
    hp = Pool(fw, "h", [128, GF // 128, NCH], BF16, 2)
    cbp = Pool(fw, "cb", [128, maxc, NCH], F32, 2)
    cin = Pool(fw, "cin", [8, maxw], F32, 1)
    x1p = Pool(fw, "x1", [128, NCH], F32, 3)
    x2p = Pool(fw, "x2", [128, NCH], F32, 3)
    out_evs = []
    cast_engs = ["gpsimd", "vector", "gpsimd", "scalar"]
    cast_i = [0]

    def cast(dst_b, dst_ap, src_b, src_ap):
        e = cast_engs[cast_i[0] % len(cast_engs)]
        cast_i[0] += 1
        if e == "scalar":
            o.act(dst_b, dst_ap, src_b, src_ap, AF.Copy)
        else:
            o.cp(e, dst_b, dst_ap, src_b, src_ap)

    dq = ["sync", "gpsimd"]
    dqi = [0]
    def ldq():
        dqi[0] += 1
        return dq[dqi[0] % 2]

    def load_group(e, f0, fw_):
        wg = wgp.next(); wu = wup.next(); wd = wdp.next()
        for (dst, src) in ((wg, wg_d), (wu, wu_d)):
            for k2 in range(0, 8, 4):
                s = stg.next()
                for kk in range(4):
                    k = k2 + kk
                    fw.dma("sync", s[:, kk * 512:kk * 512 + fw_], src[e, k * 128:(k + 1) * 128, f0:f0 + fw_], writes=[s])
                for kk in range(4):
                    cast(dst, dst[:, k2 + kk, 0:fw_], s, s[:, kk * 512:kk * 512 + fw_])
        nf = fw_ // 128
        for f2 in range(0, nf, 2):
            s = stg.next()
            for ff in range(min(2, nf - f2)):
                f = f2 + ff
                fw.dma("sync", s[:, ff * 1024:(ff + 1) * 1024], wd_d[e, f0 + f * 128:f0 + (f + 1) * 128, :], writes=[s])
            for ff in range(min(2, nf - f2)):
                cast(wd, wd[:, f2 + ff, :], s, s[:, ff * 1024:(ff + 1) * 1024])
        return wg, wu, wd

    for sc in scs:
        off = 0
        offs = []
        for (seg, c0, N) in sc:
            offs.append(off)
            for k in range(8):
                fw.dma("gpsimd", u2[:, k, off:off + N], u2_d[k * 128:(k + 1) * 128, c0:c0 + N], writes=[u2])
            off += N
        if moe:
            ci_ = cin.next()
            off2 = 0
            for (seg, c0, N) in sc:
                fw.dma("gpsimd", ci_[:, off2:off2 + N], comb_d[:, c0:c0 + N], writes=[ci_])
                off2 += N
        cur_e = -1
        cb = None
        for gi, (e, f0, fw_) in enumerate(groups):
            wg, wu, wd = load_group(e, f0, fw_)
            nf = fw_ // 128
            if moe and e != cur_e:
                cur_e = e
                cb = cbp.next()
                for ci2, (seg, c0, N) in enumerate(sc):
                    p = PSc.next()
                    o.mm(p, p[:, 0:N], selE, selE[:, e, :], ci_, ci_[:, offs[ci2]:offs[ci2] + N])
                    o.act(cb, cb[:, ci2, 0:N], p, p[:, 0:N], AF.Copy)
            for ci2, (seg, c0, N) in enumerate(sc):
                of = offs[ci2]
                h = hp.next()
                for f in range(nf):
                    pg = PSg.next(); pu = PSu.next()
                    for k in range(8):
                        o.mm(pg, pg[:, 0:N], wg, wg[:, k, f * 128:(f + 1) * 128], u2, u2[:, k, of:of + N],
                             start=(k == 0), stop=(k == 7))
                    for k in range(8):
                        o.mm(pu, pu[:, 0:N], wu, wu[:, k, f * 128:(f + 1) * 128], u2, u2[:, k, of:of + N],
                             start=(k == 0), stop=(k == 7))
                    sg = sgp.next()
                    o.act(sg, sg[:, 0:N], pg, pg[:, 0:N], AF.Silu)
                    if moe:
                        sg2 = sgp.next()
                        o.tt("vector", sg2, sg2[:, 0:N], sg, sg[:, 0:N], pu, pu[:, 0:N], ALU.mult)
                        o.tt("gpsimd", h, h[:, f, 0:N], sg2, sg2[:, 0:N], cb, cb[:, ci2, 0:N], ALU.mult)
                    else:
                        o.tt("vector", h, h[:, f, 0:N], sg, sg[:, 0:N], pu, pu[:, 0:N], ALU.mult)
                for n in range(8):
                    py = PSy.next()
                    for f in range(nf):
                        o.mm(py, py[:, 0:N], wd, wd[:, f, n * 128:(n + 1) * 128], h, h[:, f, 0:N], start=(f == 0),
                             stop=(f == nf - 1))
                    if gi == 0:
                        o.act(yacc, yacc[:, n, of:of + N], py, py[:, 0:N], AF.Copy)
                    else:
                        o.tt("vector", yacc, yacc[:, n, of:of + N], yacc, yacc[:, n, of:of + N], py, py[:, 0:N], ALU.add)
        for ci2, (seg, c0, N) in enumerate(sc):
            of = offs[ci2]
            gn = "g5_l" if seg == 0 else "g5_c"
            for n in range(8):
                x1 = x1p.next()
                fw.dma("sync", x1[:, 0:N], x1_d[n * 128:(n + 1) * 128, c0:c0 + N], writes=[x1])
                x2 = x2p.next()
                o.stt(x2, x2[:, 0:N], yacc, yacc[:, n, of:of + N], pv(gn, n), x1, x1[:, 0:N], ALU.mult, ALU.add,
                      extra_r=[pvec])
                for dst_ in x2dst(seg, n, c0, N):
                    out_evs.append(fw.dma("gpsimd", dst_, x2[:, 0:N], reads=[x2]))
    return env.done(out_evs)


POOL_WINDOWS = (2, 4, 8, 16)

def mod_inputs(inputs, nl=4):
    cT = np.stack([inputs["c"][0], inputs["c"][1], inputs["c_ctx"]], axis=1).astype(np.float32)
    maps = []
    for j in range(8):
        maps.append({"cT": cT, "mw": np.ascontiguousarray(inputs["mod_w"][:nl, :, j * 768:(j + 1) * 768]),
                     "mb": np.ascontiguousarray(inputs["mod_b"][:nl, j * 768:(j + 1) * 768].reshape(nl, 6, 128).transpose(0, 2, 1))})
    return maps

def mod_gather(results):
    return np.concatenate([r["out"] for r in results], axis=1)

def cols(v):
    return np.ascontiguousarray(v.reshape(-1, 128).T)

def rope_tables(L, grid_w=64):
    rows = L // grid_w
    row = np.repeat(np.arange(rows, dtype=np.float32), grid_w)
    col = np.tile(np.arange(grid_w, dtype=np.float32), rows)
    inv = np.power(np.float32(10000.0), -np.arange(16, dtype=np.float32) / np.float32(16)).astype(np.float32)
    C = np.zeros((128, L), np.float32); S = np.zeros((128, L), np.float32)
    for p in range(128):
        dh = p % 64
        axis = dh // 32; ab = (dh % 32) // 16; f = dh % 16
        pos = row if axis == 0 else col
        ang = (pos * inv[f]).astype(np.float32)
        C[p] = np.cos(ang); S[p] = np.sin(ang) * (-1.0 if ab == 0 else 1.0)
    return C, S

def const_mats():
    cm = np.zeros((128, 4, 128), np.float32)
    cm[:, 0, :] = 1.0 / 1024.0
    for p in range(128):
        for k in range(128):
            if p // 64 == k // 64:
                cm[k, 1, p] = 1.0 / 64.0
    for p in range(128):
        dh = p % 64; ab = (dh % 32) // 16
        partner = p + 16 if ab == 0 else p - 16
        cm[partner, 2, p] = 1.0
    cm[:, 3, :] = np.eye(128, dtype=np.float32)
    return cm

def invc_table(qd, nq, L, Lc=256):
    t = np.zeros((128, 2, 2, 16), np.float32)
    for p in range(128):
        for pt in range(2):
            g = 2 * pt + (1 if p >= 64 else 0)
            w = POOL_WINDOWS[g]
            for e in range(16):
                for seg, Ls, realL, realR in ((0, L, qd == 0, qd == nq - 1), (1, Lc, True, True)):
                    if e < 8:
                        tt = e
                        cnt = min(tt + w // 2, Ls) - max(tt - w // 2, 0) if realL else w
                    else:
                        tt = Ls - 16 + e
                        cnt = min(tt + w // 2, Ls) - max(tt - w // 2, 0) if realR else w
                    t[p, seg, pt, e] = 1.0 / cnt
    return t

def p1_inputs(inputs, l, mvec, x, h, ropeC, ropeS, cm):
    B, L, D = x.shape
    nq = 8 // B
    TL = L // nq
    maps = []
    pl = inputs["pool_lin"][l]
    plin = np.zeros((128, 2, 128), np.float32)
    for pt in range(2):
        plin[0:64, pt, 0:64] = pl[2 * pt]; plin[64:128, pt, 64:128] = pl[2 * pt + 1]
    for j in range(8):
        b, qd = j // nq, j % nq
        xp = np.zeros((TL + 16, D), np.float32)
        lo, hi = qd * TL - 8, qd * TL + TL + 8
        slo, shi = max(lo, 0), min(hi, L)
        xp[slo - lo:shi - lo] = x[b, slo:shi]
        hp = np.zeros((h.shape[1] + 16, D), np.float32)
        hp[8:8 + h.shape[1]] = h[b]
        pv = np.zeros((128, NPV), np.float32)
        def put(name, arr):
            c0, n = PV[name]; pv[:, c0:c0 + n] = arr.reshape(128, n)
        m = mvec[l]
        put("g1", cols(inputs["norm1_g"][l]))
        put("sh_l", cols(m[0:1024, b])); put("sc_l", cols(m[1024:2048, b]))
        put("sh_c", cols(m[0:1024, 2])); put("sc_c", cols(m[1024:2048, 2]))
        sw = inputs["hy_short_w"][l]
        cw = np.zeros((128, 18), np.float32)
        for t in range(6):
            for tap in range(3):
                cw[:, t * 3 + tap] = sw[tap, t * 128:(t + 1) * 128]
        put("cw", cw); put("cb", cols(inputs["hy_short_b"][l]))
        put("gq", np.tile(inputs["qk_norm_g"][l, 0], 2)[:, None]); put("gk", np.tile(inputs["qk_norm_g"][l, 1], 2)[:, None])
        put("psc", cols(inputs["pool_scale"][l]))
        put("mL", np.full((128, 1), 0.0 if qd == 0 else 1.0, np.float32))
        put("mR", np.full((128, 1), 0.0 if qd == nq - 1 else 1.0, np.float32))
        put("eps", np.full((128, 1), 1e-6, np.float32))
        maps.append({"xT": np.ascontiguousarray(xp.T), "hT": np.ascontiguousarray(hp.T), "pvec": pv,
                     "w_in": np.ascontiguousarray(inputs["w_in"][l]),
                     "ropeC": np.ascontiguousarray(ropeC[:, qd * TL:(qd + 1) * TL]),
                     "ropeS": np.ascontiguousarray(ropeS[:, qd * TL:(qd + 1) * TL]),
                     "cmats": cm, "plin": plin, "invc": invc_table(qd, nq, L, h.shape[1])})
    return maps


def pvec4(inputs, l, mvec, b):
    pv = np.zeros((128, NPV4), np.float32)
    def put(name, arr):
        c0, n = PV4[name]; pv[:, c0:c0 + n] = arr.reshape(128, n)
    m = mvec[l]
    put("hyb", cols(inputs["hy_bias"][l]))
    put("g2_l", cols(m[2048:3072, b])); put("g2_c", cols(m[2048:3072, 2]))
    put("n2g", cols(inputs["norm2_g"][l]))
    put("sh_l", cols(m[3072:4096, b])); put("sc_l", cols(m[4096:5120, b]))
    put("sh_c", cols(m[3072:4096, 2])); put("sc_c", cols(m[4096:5120, 2]))
    put("g5_l", cols(m[5120:6144, b])); put("g5_c", cols(m[5120:6144, 2]))
    put("eps", np.full((128, 1), 1e-6, np.float32))
    return pv

ONESD = np.full((128, 128), 1.0 / 1024.0, np.float32)
IDENT = np.eye(128, dtype=np.float32)
SELE = np.zeros((8, 8, 128), np.float32)
for _e in range(8):
    SELE[_e, _e, :] = 1.0


SEL32 = np.ascontiguousarray(np.concatenate([np.eye(32), np.eye(32)], 0).astype(np.float32))


def pvec1(inputs, l, mvec, b, qd, nq):
    pv = np.zeros((128, NPV), np.float32)
    def put(name, arr):
        c0, n = PV[name]; pv[:, c0:c0 + n] = arr.reshape(128, n)
    m = mvec[l]
    put("g1", cols(inputs["norm1_g"][l]))
    put("sh_l", cols(m[0:1024, b])); put("sc_l", cols(m[1024:2048, b]))
    put("sh_c", cols(m[0:1024, 2])); put("sc_c", cols(m[1024:2048, 2]))
    sw = inputs["hy_short_w"][l]
    cw = np.zeros((128, 18), np.float32)
    for t in range(6):
        for tap in range(3):
            cw[:, t * 3 + tap] = sw[tap, t * 128:(t + 1) * 128]
    put("cw", cw); put("cb", cols(inputs["hy_short_b"][l]))
    put("gq", np.tile(inputs["qk_norm_g"][l, 0], 2)[:, None]); put("gk", np.tile(inputs["qk_norm_g"][l, 1], 2)[:, None])
    put("psc", cols(inputs["pool_scale"][l]))
    put("mL", np.full((128, 1), 0.0 if qd == 0 else 1.0, np.float32))
    put("mR", np.full((128, 1), 0.0 if qd == nq - 1 else 1.0, np.float32))
    put("eps", np.full((128, 1), 1e-6, np.float32))
    return pv

_PROGS = {}


def _prog(key, builder):
    if key not in _PROGS:
        _PROGS[key] = builder()
    return _PROGS[key]


def _run(nc, maps):
    import time as _t
    t0 = _t.time()
    res = run_bass_kernel_spmd(nc, maps, core_ids=list(range(8)))
    if _VERBOSE:
        print("[kernel] launch done in %.1fs" % (_t.time() - t0), flush=True)
    return res.results


_VERBOSE = False


_BF = ml_dtypes.bfloat16


def kernel(**inputs):
    inp = {k: np.asarray(v) for k, v in inputs.items()}
    x0 = inp["x"]
    B, L, Dm = x0.shape
    C = inp["ctx"].shape[1]
    nq = 8 // B
    TL = L // nq
    T = TL + C
    NL = inp["mod_w"].shape[0]
    mvec = mod_gather(_run(_prog(("p0", NL), lambda: build_p0(768, NL)), mod_inputs(inp, NL)))
    ropeC, ropeS = rope_tables(L)
    cm = const_mats()
    tabs = {LL: lc_tables(LL) for LL in (L, C)}
    ftabs = {(LL, j): filt_tables(LL, 32 * j) for LL in (L, C) for j in range(8)}
    xcur = []
    for j in range(8):
        b, qd = j // nq, j % nq
        xcur.append(np.ascontiguousarray(np.concatenate([x0[b, qd * TL:(qd + 1) * TL], inp["ctx"][b]], 0).T))
    z8 = np.zeros((Dm, 8), np.float32)
    for l in range(NL):
        moe = (l % 2 == 1)
        jl = l // 2
        lam_init = 0.8 - 0.6 * math.exp(-0.3 * l)
        maps = []
        pl = inp["pool_lin"][l]
        plin = np.zeros((128, 2, 128), np.float32)
        for pt in range(2):
            plin[0:64, pt, 0:64] = pl[2 * pt]
            plin[64:128, pt, 64:128] = pl[2 * pt + 1]
        for j in range(8):
            b, qd = j // nq, j % nq
            left = xcur[j - 1][:, TL - 8:TL] if qd > 0 else z8
            right = xcur[j + 1][:, 0:8] if qd < nq - 1 else z8
            xT = np.ascontiguousarray(np.concatenate([left, xcur[j][:, :TL], right], 1))
            hT = np.ascontiguousarray(np.concatenate([z8, xcur[j][:, TL:], z8], 1))
            maps.append({"xT": xT, "hT": hT, "pvec": pvec1(inp, l, mvec, b, qd, nq),
                         "w_in": np.ascontiguousarray(inp["w_in"][l]),
                         "ropeC": np.ascontiguousarray(ropeC[:, qd * TL:(qd + 1) * TL]),
                         "ropeS": np.ascontiguousarray(ropeS[:, qd * TL:(qd + 1) * TL]),
                         "cmats": cm, "plin": plin, "invc": invc_table(qd, nq, L, C)})
        r1 = _run(_prog(("p1", TL, C), lambda: build_p1(TL, C)), maps)
        r1 = [{k: np.asarray(v) for k, v in r.items()} for r in r1]
        maps = []
        pv2 = np.zeros((128, NPV2), np.float32)
        pv2[:, 0] = 1e-6
        pv2[:, 1] = lam_init
        pv2[:, 2] = inp["subln_g"][l]
        pv2[:, 3] = 1.0 - lam_init
        dlam = np.ascontiguousarray(np.broadcast_to(inp["diff_lambda"][l][None], (128, 4, 64))).astype(np.float32)
        for j in range(8):
            b, hd = j // 4, j % 4
            cores = [b * nq + qd for qd in range(nq)]
            rs_ = slice(hd * 128, (hd + 1) * 128)
            qT = np.concatenate([r1[c]["qT"][rs_, :TL] for c in cores] + [r1[cores[0]]["qT"][rs_, TL:]], 1)
            kT = np.concatenate([r1[cores[0]]["kT"][rs_, TL:]] + [r1[c]["kT"][rs_, :TL] for c in cores], 1)
            v = np.concatenate([r1[cores[0]]["v"][TL:, rs_]] + [r1[c]["v"][:TL, rs_] for c in cores], 0)
            maps.append({"qT": np.ascontiguousarray(qT), "kT": np.ascontiguousarray(kT), "v": np.ascontiguousarray(v),
                         "dlam": dlam, "pvec": pv2, "ident": IDENT})
        r2 = _run(_prog(("p2", L, C), lambda: build_p2(L, C, True)), maps)
        r2 = [np.asarray(r["attT"]) for r in r2]
        maps = []
        for j in range(8):
            ch0 = 32 * j
            w3 = inp["hy_f_w3"][l]
            m = {"w1": np.ascontiguousarray(inp["hy_f_w1"][l]), "w2": np.ascontiguousarray(inp["hy_f_w2"][l]),
                 "w3s": np.ascontiguousarray(np.concatenate([w3[:, ch0:ch0 + 32], w3[:, 256 + ch0:256 + ch0 + 32]], 1)),
                 "fpv": np.ascontiguousarray(np.stack([inp["hy_f_freq1"][l], inp["hy_f_b1"][l], inp["hy_f_freq2"][l],
                                                       inp["hy_f_b2"][l]], 1)),
                 "sel": SEL32}
            for LL in (L, C):
                sfx = "_%d" % LL
                for k_, v_ in tabs[LL].items():
                    m[k_ + sfx] = v_
                m["feat" + sfx], m["dec" + sfx] = ftabs[(LL, j)]
                rows = []
                for b in range(B):
                    if LL == L:
                        rows.append(np.concatenate([r1[b * nq + qd]["v2"][ch0:ch0 + 32, :TL] for qd in range(nq)], 1))
                    else:
                        rows.append(r1[b * nq]["v2"][ch0:ch0 + 32, TL:])
                m["vin" + sfx] = np.ascontiguousarray(np.concatenate(rows, 0))
            maps.append(m)
        r3 = _run(_prog(("p3", L, C), lambda: build_p3((L, C))), maps)
        r3 = [{k: np.asarray(v) for k, v in r.items()} for r in r3]
        maps = []
        for j in range(8):
            b, qd = j // nq, j % nq
            conv = np.zeros((256, T), np.float32)
            att = np.zeros((512, T), _BF)
            for jj in range(8):
                conv[32 * jj:32 * jj + 32, :TL] = r3[jj]["yout_%d" % L][b * 32:(b + 1) * 32, qd * TL:(qd + 1) * TL]
                conv[32 * jj:32 * jj + 32, TL:] = r3[jj]["yout_%d" % C][b * 32:(b + 1) * 32, :]
            for hd in range(4):
                a = r2[b * 4 + hd]
                att[hd * 128:(hd + 1) * 128, :TL] = a[:, qd * TL:(qd + 1) * TL]
                att[hd * 128:(hd + 1) * 128, TL:] = a[:, L:L + C]
            mp = {"xT": xcur[j], "ypool": r1[j]["ypool"], "x0c": r1[j]["x0c"], "v2": r1[j]["v2"], "conv": conv,
                  "attT": att, "w_out": np.ascontiguousarray(inp["w_out"][l]), "pvec": pvec4(inp, l, mvec, b),
                  "onesD": ONESD}
            if moe:
                mp["rw"] = np.ascontiguousarray(inp["router_w"][jl].reshape(8, 128, 8).transpose(1, 0, 2))
                mp["ident"] = IDENT
            maps.append(mp)
        r4 = _run(_prog(("p4", TL, C, moe), lambda: build_p4(TL, C, moe)), maps)
        r4 = [{k: np.asarray(v) for k, v in r.items()} for r in r4]
        del r1, r2, r3
        maps = []
        if moe:
            wg = np.ascontiguousarray(inp["moe_w_gate"][jl]); wu = np.ascontiguousarray(inp["moe_w_up"][jl])
            wd = np.ascontiguousarray(inp["moe_w_down"][jl])
        else:
            wg = np.ascontiguousarray(inp["ffn_w_gate"][jl][None]); wu = np.ascontiguousarray(inp["ffn_w_up"][jl][None])
            wd = np.ascontiguousarray(inp["ffn_w_down"][jl][None])
        for j in range(8):
            b = j // nq
            mp = {"x1T": r4[j]["x1T"], "u2T": r4[j]["u2T"], "pvec": pvec4(inp, l, mvec, b), "wg": wg, "wu": wu, "wd": wd}
            if moe:
                mp["combT"] = r4[j]["combT"]
                mp["selE"] = SELE
            maps.append(mp)
        r5 = _run(_prog(("p5", TL, C, moe), lambda: build_p5(TL, C, moe)), maps)
        xcur = [np.asarray(r["x2T"]) for r in r5]
        del r4, r5
    out = np.zeros((B, L, Dm), np.float32)
    for j in range(8):
        b, qd = j // nq, j % nq
        out[b, qd * TL:(qd + 1) * TL] = xcur[j][:, :TL].T
    return out
```

```python
import math
import numpy as np
import ml_dtypes
import concourse.bass as bass
import concourse.mybir as mybir
from concourse.bass_utils import run_bass_kernel_spmd

F32 = mybir.dt.float32
BF16 = mybir.dt.bfloat16
I32 = mybir.dt.int32
AF = mybir.ActivationFunctionType
ALU = mybir.AluOpType
AX = mybir.AxisListType

ENGS = ["tensor", "vector", "scalar", "gpsimd", "sync"]


class Buf:
    __slots__ = ("t", "w", "r", "name")

    def __init__(self, t, name=""):
        self.t = t
        self.w = None
        self.r = {}
        self.name = name

    def __getitem__(self, idx):
        return self.t[idx]


class FW:
    def __init__(self, nc, dma_ring=6, fused=False):
        self.nc = nc
        self.fused = fused
        self.phase = "g"
        self.uid = 0
        if fused:
            self.sb_lo = (int(nc.sbuf_base) + 63) // 64 * 64
            self.sb_hi = int(nc.sbuf_top)
            self.floor = self.sb_lo
            self.off = self.sb_lo
            self.pall = nc.alloc_psum_tensor("ps_all", [128, 4096], F32)
            self.psb = 0
        self.prog = {e: [] for e in ENGS}
        self.sems = {}
        self.seq = {}
        self.known = {e: {} for e in ENGS}
        for e in ENGS:
            self.sems[e] = nc.alloc_semaphore("s_" + e)
            self.seq[e] = 0
        self.dma_ring = dma_ring
        self.dq = {}
        for q in ["sync", "gpsimd", "scalar"]:
            ring = []
            for i in range(dma_ring):
                key = "d_%s_%d" % (q, i)
                self.sems[key] = nc.alloc_semaphore(key)
                ring.append(key)
            self.dq[q] = {"ring": ring, "n": 0, "hist": []}
        self.n_inst = 0

    def sb(self, name, shape, dtype, persist=False):
        if not self.fused:
            return Buf(self.nc.alloc_sbuf_tensor("sb_" + name, list(shape), dtype), name)
        n = 1
        for d_ in shape[1:]:
            n *= int(d_)
        nbytes = (n * mybir.dt.size(dtype) + 63) // 64 * 64
        self.uid += 1
        if persist:
            assert self.off == self.floor, "persistent allocs must precede phase allocs"
            off = self.floor
            self.floor += nbytes
            self.off = self.floor
        else:
            off = self.off
            self.off += nbytes
        assert off + nbytes <= self.sb_hi, "SBUF overflow in phase %s at %s (%d > %d)" % (self.phase, name, off + nbytes, self.sb_hi)
        return Buf(self.nc.alloc_sbuf_tensor_at("sb_%s_%d_%s" % (self.phase, self.uid, name), list(shape), dtype, offset=off), name)

    def ps(self, name, shape, dtype=F32):
        if not self.fused:
            return Buf(self.nc.alloc_psum_tensor("ps_" + name, list(shape), dtype), name)
        n = 1
        for d_ in shape[1:]:
            n *= int(d_)
        nbytes = n * mybir.dt.size(dtype)
        nb = (nbytes + 2047) // 2048
        assert self.psb + nb <= 8, "PSUM overflow in phase %s at %s" % (self.phase, name)
        v = self.pall[0:shape[0], self.psb * 512:(self.psb + nb) * 512]
        self.psb += nb
        if dtype != F32:
            v = v.bitcast(dtype)
        v = v[:, 0:n]
        if len(shape) > 2:
            raise NotImplementedError("psum views are 2-D")
        return Buf(v, name)

    def begin_phase(self, name):
        self.barrier()
        self.phase = name
        if self.fused:
            self.off = self.floor
            self.psb = 0

    def barrier(self):
        latest = {}
        for e in ENGS:
            if self.seq[e] > 0:
                latest[e] = self.seq[e]
        for q, dq in self.dq.items():
            n = dq["n"]
            for i, key in enumerate(dq["ring"]):
                cnt = (n - i + self.dma_ring - 1) // self.dma_ring if n > i else 0
                if cnt > 0:
                    latest[key] = 16 * cnt
        for e in ENGS:
            kn = self.known[e]
            waits = []
            for k, v in latest.items():
                if kn.get(k, 0) < v:
                    kn[k] = v
                    waits.append((k, v))
            if waits:
                self.prog[e].append((waits, None, None, False))

    def coll(self, kind, in_buf, out_buf, groups):
        dq = self.dq["gpsimd"]
        n = dq["n"]
        dq["n"] += 1
        key = dq["ring"][n % self.dma_ring]
        val = 16 * (n // self.dma_ring + 1)
        waits = self._deps("gpsimd", [in_buf], [out_buf])
        if n >= self.dma_ring:
            pk, pv = key, val - 16
            if self.known["gpsimd"].get(pk, 0) < pv:
                self.known["gpsimd"][pk] = pv
                waits.append((pk, pv))
        ev = (key, val)
        ia, oa = in_buf.t.ap(), out_buf.t.ap()

        def fn(e):
            return e.collective_compute(kind, ALU.bypass, replica_groups=groups, ins=[ia], outs=[oa])
        self.prog["gpsimd"].append((waits, fn, ev, True))
        self._commit(ev, [in_buf], [out_buf])
        return ev

    def dram(self, name, shape, dtype, kind="Internal"):
        self.uid += 1
        nm = "%s_%s_%d" % (name, self.phase, self.uid) if self.fused else name
        return Buf(self.nc.dram_tensor(nm, list(shape), dtype, kind=kind), name)

    def _deps(self, eng, reads, writes):
        need = {}

        def add(ev):
            if ev is None:
                return
            k, v = ev
            if need.get(k, 0) < v:
                need[k] = v
        for b in reads:
            add(b.w)
        for b in writes:
            add(b.w)
            for k, v in b.r.items():
                add((k, v))
        kn = self.known[eng]
        out = []
        for k, v in need.items():
            if kn.get(k, 0) < v:
                kn[k] = v
                out.append((k, v))
        return out

    def _commit(self, ev, reads, writes):
        k, v = ev
        for b in reads:
            if b.r.get(k, 0) < v:
                b.r[k] = v
        for b in writes:
            b.w = ev
            b.r = {}

    def op(self, eng, fn, reads=(), writes=(), same_eng_sync=True):
        waits = self._deps(eng, reads, writes)
        self.seq[eng] += 1
        ev = (eng, self.seq[eng])
        if not same_eng_sync:
            waits = [w for w in waits if w[0] != eng]
        self.prog[eng].append((waits, fn, ev, False))
        self._commit(ev, reads, writes)
        self.n_inst += 1
        return ev

    def dma(self, q, out_ap, in_ap, reads=(), writes=(), **kw):
        dq = self.dq[q]
        n = dq["n"]
        dq["n"] += 1
        key = dq["ring"][n % self.dma_ring]
        val = 16 * (n // self.dma_ring + 1)
        waits = self._deps(q, reads, writes)
        if n >= self.dma_ring:
            pk, pv = key, val - 16
            if self.known[q].get(pk, 0) < pv:
                self.known[q][pk] = pv
                waits.append((pk, pv))
        ev = (key, val)

        def fn(e, out_ap=out_ap, in_ap=in_ap, kw=kw):
            return e.dma_start(out=out_ap, in_=in_ap, **kw)
        self.prog[q].append((waits, fn, ev, True))
        self._commit(ev, reads, writes)
        self.n_inst += 1
        return ev

    def finish(self, final_events):
        nc = self.nc
        sems = self.sems
        prog = self.prog
        with nc.Block() as block:
            def make(engname):
                def body(e):
                    for waits, fn, ev, is_dma in prog[engname]:
                        for k, v in waits:
                            e.wait_ge(sems[k], v)
                        if fn is None:
                            continue
                        ins = fn(e)
                        ins.then_inc(sems[ev[0]], 16 if is_dma else 1)
                    if engname == "sync":
                        fin = {}
                        for k, v in final_events:
                            fin[k] = max(fin.get(k, 0), v)
                        for k, v in fin.items():
                            e.wait_ge(sems[k], v)
                return body
            block.tensor(make("tensor"))
            block.vector(make("vector"))
            block.scalar(make("scalar"))
            block.gpsimd(make("gpsimd"))
            block.sync(make("sync"))


class Pool:
    def __init__(self, fw, name, shape, dtype, n, space="sb"):
        mk = fw.sb if space == "sb" else fw.ps
        self.bufs = [mk("%s_%d" % (name, i), shape, dtype) for i in range(n)]
        self.i = 0

    def next(self):
        b = self.bufs[self.i % len(self.bufs)]
        self.i += 1
        return b


def _lst(x):
    if x is None:
        return []
    if isinstance(x, (list, tuple)):
        return list(x)
    return [x]


class OPS:
    def __init__(self, fw):
        self.fw = fw

    def act(self, ob, oap, ib, iap, func, bias=None, scale=None, accum=None, extra_r=(), eng="scalar"):
        kw = {}
        reads = _lst(ib) + list(extra_r)
        writes = _lst(ob)
        if bias is not None:
            kw["bias"] = bias
        if scale is not None:
            kw["scale"] = scale
        if accum is not None:
            kw["accum_out"] = accum[1]
            writes.append(accum[0])
        return self.fw.op("scalar", lambda e: e.activation(out=oap, in_=iap, func=func, **kw), reads, writes)

    def mm(self, ob, oap, lb, lap, rb, rap, start=True, stop=True, **kw):
        return self.fw.op("tensor", lambda e: e.matmul(oap, lhsT=lap, rhs=rap, start=start, stop=stop, **kw),
                          _lst(lb) + _lst(rb), _lst(ob), same_eng_sync=False)

    def tr(self, ob, oap, ib, iap, idb, idap):
        return self.fw.op("tensor", lambda e: e.transpose(oap, iap, idap), _lst(ib) + _lst(idb), _lst(ob),
                          same_eng_sync=False)

    def tt(self, eng, ob, oap, ab, aap, bb, bap, op):
        return self.fw.op(eng, lambda e: e.tensor_tensor(out=oap, in0=aap, in1=bap, op=op),
                          _lst(ab) + _lst(bb), _lst(ob))

    def ts(self, eng, ob, oap, ab, aap, s1, s2, op0, op1=None, extra_r=(), accum=None):
        kw = {}
        writes = _lst(ob)
        if op1 is not None:
            kw["op1"] = op1
        if accum is not None:
            kw["accum_out"] = accum[1]
            writes.append(accum[0])
        return self.fw.op(eng, lambda e: e.tensor_scalar(out=oap, in0=aap, scalar1=s1, scalar2=s2, op0=op0, **kw),
                          _lst(ab) + list(extra_r), writes)

    def stt(self, ob, oap, ab, aap, scalar, bb, bap, op0, op1, extra_r=()):
        return self.fw.op("vector", lambda e: e.scalar_tensor_tensor(out=oap, in0=aap, scalar=scalar, in1=bap,
                                                                      op0=op0, op1=op1),
                          _lst(ab) + _lst(bb) + list(extra_r), _lst(ob))

    def cp(self, eng, ob, oap, ib, iap):
        return self.fw.op(eng, lambda e: e.tensor_copy(out=oap, in_=iap), _lst(ib), _lst(ob))

    def recip(self, ob, oap, ib, iap):
        return self.fw.op("vector", lambda e: e.reciprocal(out=oap, in_=iap), _lst(ib), _lst(ob))

    def memset(self, eng, ob, oap, val):
        return self.fw.op(eng, lambda e: e.memset(oap, val), [], _lst(ob))


class Env:
    def __init__(self, nc=None, fw=None, bind=None):
        self.fused = nc is not None
        self.nc = nc if nc is not None else bass.Bass("TRN2", target_bir_lowering=False)
        self.fw = fw if fw is not None else FW(self.nc)
        self.o = OPS(self.fw)
        self.bind = bind or {}

    def io(self, name, shape, dtype, kind):
        if self.fused:
            ap = self.bind[name]
            assert tuple(int(x) for x in ap.shape) == tuple(int(x) for x in shape), (name, ap.shape, shape)
            return ap
        return self.nc.dram_tensor(name, list(shape), dtype, kind=kind).ap()

    def done(self, evs):
        if self.fused:
            return evs
        self.fw.finish(evs)
        return self.nc


D = 1024
INW = 2560
EPS = 1e-6

PV = {}
_c = 0
def _add(name, n):
    global _c
    PV[name] = (_c, n)
    _c += n
_add("g1", 8)
_add("sh_l", 8)
_add("sc_l", 8)
_add("sh_c", 8)
_add("sc_c", 8)
_add("cw", 18)
_add("cb", 6)
_add("gq", 1)
_add("gk", 1)
_add("psc", 2)
_add("mL", 1)
_add("mR", 1)
_add("zero", 1)
_add("eps", 1)
NPV = _c


def build_p0(ncols=768, nlayers=4, env=None):
    env = env or Env()
    nc, fw, o = env.nc, env.fw, env.o
    cT = env.io("cT", [D, 3], F32, "ExternalInput")
    mw = env.io("mw", [nlayers, D, ncols], F32, "ExternalInput")
    mb = env.io("mb", [nlayers, 128, ncols // 128], F32, "ExternalInput")
    out = env.io("out", [nlayers, ncols, 3], F32, "ExternalOutput")
    nt = ncols // 128
    ct = fw.sb("ct", [128, 8, 3], F32)
    sc = fw.sb("sc", [128, 8, 3], F32)
    fw.dma("sync", ct[:], cT.rearrange("(k p) g -> p k g", p=128), writes=[ct])
    o.act(sc, sc[:], ct, ct[:], AF.Silu)
    wpool = Pool(fw, "w", [128, 8, ncols], F32, 2)
    bpool = Pool(fw, "b", [128, nt], F32, 2)
    opool = Pool(fw, "o", [128, nt, 3], F32, 2)
    pp = Pool(fw, "pp", [128, 512], F32, 2, space="ps")
    evs = []
    for l in range(nlayers):
        w = wpool.next()
        for k in range(8):
            fw.dma("sync" if k % 2 == 0 else "gpsimd", w[:, k, :], mw[l, k * 128:(k + 1) * 128, :], writes=[w])
        b = bpool.next()
        fw.dma("sync", b[:], mb[l], writes=[b])
        ot = opool.next()
        for n in range(nt):
            p = pp.next()
            for k in range(8):
                o.mm(p, p[:, 0:3], w, w[:, k, n * 128:(n + 1) * 128], sc, sc[:, k, :], start=(k == 0), stop=(k == 7))
            o.ts("vector", ot, ot[:, n, :], p, p[:, 0:3], b[:, n:n + 1], None, ALU.add, extra_r=[b])
        evs.append(fw.dma("gpsimd", out[l].rearrange("(n p) g -> p n g", p=128), ot[:], reads=[ot]))
    return env.done(evs)


def build_p1(TL, TC=256, NCH=512, env=None, vsplit=False, mv=None):
    T = TL + TC
    env = env or Env()
    nc, fw, o = env.nc, env.fw, env.o
    xT = env.io("xT", [D, TL + 16], F32, "ExternalInput")
    hT = env.io("hT", [D, TC + 16], F32, "ExternalInput")
    pvec_d = env.io("pvec", [128, NPV], F32, "ExternalInput")
    win_d = env.io("w_in", [D, INW], F32, "ExternalInput")
    ropeC_d = env.io("ropeC", [128, TL], F32, "ExternalInput")
    ropeS_d = env.io("ropeS", [128, TL], F32, "ExternalInput")
    cm_d = env.io("cmats", [128, 4, 128], F32, "ExternalInput")
    plin_d = env.io("plin", [128, 2, 128], F32, "ExternalInput")
    invc_d = env.io("invc", [128, 2, 2, 16], F32, "ExternalInput")
    ypool_d = env.io("ypool", [256, T], BF16, "ExternalOutput")
    x0c_d = env.io("x0c", [256, T], BF16, "ExternalOutput")
    v2_d = env.io("v2", [256, T], F32, "ExternalOutput")
    q_d = env.io("qT", [512, T], BF16, "ExternalOutput")
    k_d = env.io("kT", [512, T], BF16, "ExternalOutput")
    v_d = env.io("v", [4, T, 128] if vsplit else [T, 512], BF16, "ExternalOutput")

    NW = NCH + 16
    pvec = fw.sb("pvec", [128, NPV], F32)
    fw.dma("sync", pvec[:], pvec_d, writes=[pvec])
    if mv is not None:
        mvb, l_ = mv
        for nm_, t0_, g_ in (("sh_l", 0, 0), ("sc_l", 8, 0), ("sh_c", 0, 1), ("sc_c", 8, 1)):
            c0_, _ = PV[nm_]
            o.cp("vector", pvec, pvec[:, c0_:c0_ + 8], mvb, mvb[:, l_, t0_:t0_ + 8, g_])
    def pv(name, i=0):
        c0, n = PV[name]
        return pvec[:, c0 + i:c0 + i + 1]
    cm32 = fw.sb("cm32", [128, 4, 128], F32)
    fw.dma("sync", cm32[:], cm_d, writes=[cm32])
    cmb = fw.sb("cmb", [128, 4, 128], BF16)
    o.cp("vector", cmb, cmb[:], cm32, cm32[:])
    pl32 = fw.sb("pl32", [128, 2, 128], F32)
    fw.dma("sync", pl32[:], plin_d, writes=[pl32])
    plb = fw.sb("plb", [128, 2, 128], BF16)
    o.cp("vector", plb, plb[:], pl32, pl32[:])
    invc = fw.sb("invc", [128, 2, 2, 16], F32)
    fw.dma("sync", invc[:], invc_d, writes=[invc])
    Amod = fw.sb("Amod", [128, 2, 8], F32)
    for si, nm in enumerate(["sc_l", "sc_c"]):
        c0, _ = PV[nm]
        g0, _ = PV["g1"]
        o.stt(Amod, Amod[:, si, :], pvec, pvec[:, c0:c0 + 8], 1.0, pvec, pvec[:, g0:g0 + 8], ALU.add, ALU.mult)
    wb = fw.sb("wb", [128, 8, INW], BF16)
    stg = Pool(fw, "wstg", [128, INW // 2], F32, 2)
    for k in range(8):
        for hf in range(2):
            s = stg.next()
            c0_ = hf * (INW // 2)
            fw.dma("sync" if hf == 0 else "gpsimd", s[:], win_d[k * 128:(k + 1) * 128, c0_:c0_ + INW // 2], writes=[s])
            o.cp("gpsimd" if hf == 0 else "vector", wb, wb[:, k, c0_:c0_ + INW // 2], s, s[:])

    xin = Pool(fw, "xin", [128, 8, NW], F32, 2)
    sqp = Pool(fw, "sq", [128, NW], BF16, 2)
    rstdp = Pool(fw, "rstd", [128, NW], F32, 2)
    up = Pool(fw, "u", [128, 8, NW], BF16, 2)
    tmpn = Pool(fw, "tmpn", [128, NW], F32, 2)
    zp = Pool(fw, "z", [128, NW], F32, 3)
    psA = Pool(fw, "psA", [128, 1024], F32, 2, space="ps")
    psB = Pool(fw, "psB", [128, 512], F32, 3, space="ps")
    sA = Pool(fw, "sA", [128, NW], F32, 2)
    sB = Pool(fw, "sB", [128, NW], F32, 2)
    dpool = Pool(fw, "dpl", [128, NCH], BF16, 2)
    ob16 = Pool(fw, "ob16", [128, NCH], BF16, 4)
    of32 = Pool(fw, "of32", [128, NCH], F32, 3)
    cvp = Pool(fw, "cv", [128, NCH], F32, 4)
    ropeCp = Pool(fw, "rC", [128, NCH], F32, 2)
    ropeSp = Pool(fw, "rS", [128, NCH], F32, 2)
    qf = Pool(fw, "qf", [128, NCH], F32, 2)
    qsq = Pool(fw, "qsq", [128, NCH], BF16, 2)
    qr = Pool(fw, "qr", [128, NCH], F32, 2)
    qn = Pool(fw, "qn", [128, NCH], BF16, 2)
    t1p = Pool(fw, "t1", [128, NCH], F32, 2)
    t2p = Pool(fw, "t2", [128, NCH], F32, 2)
    vout = Pool(fw, "vout", [128, 512], BF16, 2)
    out_evs = []
    stq = ["gpsimd"]

    def store(dst_ap, buf, ap):
        out_evs.append(fw.dma("gpsimd", dst_ap, ap, reads=[buf]))

    def prep(seg, src, c0, N):
        W = N + 16
        x = xin.next()
        for k in range(8):
            fw.dma("sync", x[:, k, 0:W], src[k * 128:(k + 1) * 128, c0:c0 + W], writes=[x])
        pst = psA.next()
        for k in range(8):
            sq = sqp.next()
            o.act(sq, sq[:, 0:W], x, x[:, k, 0:W], AF.Square)
            a = min(W, 512)
            o.mm(pst, pst[:, 0:a], cmb, cmb[:, 0, :], sq, sq[:, 0:a], start=(k == 0), stop=(k == 7))
            if W > 512:
                o.mm(pst, pst[:, 512:W], cmb, cmb[:, 0, :], sq, sq[:, 512:W], start=(k == 0), stop=(k == 7))
        rstd = rstdp.next()
        o.act(rstd, rstd[:, 0:W], pst, pst[:, 0:W], AF.Sqrt, bias=pv("eps"), scale=1.0, extra_r=[pvec])
        o.recip(rstd, rstd[:, 0:W], rstd, rstd[:, 0:W])
        u = up.next()
        shn = "sh_l" if seg == 0 else "sh_c"
        for k in range(8):
            t = tmpn.next()
            o.stt(t, t[:, 0:W], x, x[:, k, 0:W], Amod[:, seg, k:k + 1], rstd, rstd[:, 0:W], ALU.mult, ALU.mult,
                  extra_r=[Amod])
            o.act(u, u[:, k, 0:W], t, t[:, 0:W], AF.Identity, bias=pv(shn, k), scale=1.0, extra_r=[pvec])
        return u

    def chunk(u, seg, src, c0, N, first, last, col0, rope_c0):
        W = N + 16

        def proj(n, c_lo, c_hi):
            p = psA.next()
            w = c_hi - c_lo
            for k in range(8):
                a = min(w, 512)
                o.mm(p, p[:, 0:a], wb, wb[:, k, n * 128:(n + 1) * 128], u, u[:, k, c_lo:c_lo + a],
                     start=(k == 0), stop=(k == 7))
                if w > 512:
                    o.mm(p, p[:, 512:512 + w - 512], wb, wb[:, k, n * 128:(n + 1) * 128], u,
                         u[:, k, c_lo + 512:c_hi], start=(k == 0), stop=(k == 7))
            return p

        def evac_masked(n):
            p = proj(n, 0, W)
            z = zp.next()
            o.act(z, z[:, 0:W], p, p[:, 0:W], AF.Copy)
            if first:
                o.ts("vector", z, z[:, 0:8], z, z[:, 0:8], pv("mL") if seg == 0 else pv("zero"), None, ALU.mult,
                     extra_r=[pvec])
            if last:
                o.ts("vector", z, z[:, N + 8:W], z, z[:, N + 8:W], pv("mR") if seg == 0 else pv("zero"), None,
                     ALU.mult, extra_r=[pvec])
            return z

        for pt in range(2):
            z = evac_masked(pt)
            a = sA.next()
            b = sB.next()
            o.tt("vector", a, a[:, 1:W], z, z[:, 0:W - 1], z, z[:, 1:W], ALU.add)
            o.tt("vector", b, b[:, 2:W - 1], a, a[:, 1:W - 2], a, a[:, 3:W], ALU.add)
            if pt == 0:
                lo, hi, wl, wh = a, b, 2.0, 4.0
            else:
                a2 = sA.next()
                o.tt("vector", a2, a2[:, 4:W - 3], b, b[:, 2:W - 5], b, b[:, 6:W - 1], ALU.add)
                b2 = sB.next()
                o.tt("vector", b2, b2[:, 8:W - 7], a2, a2[:, 4:W - 11], a2, a2[:, 12:W - 3], ALU.add)
                lo, hi, wl, wh = a2, b2, 8.0, 16.0
            d = dpool.next()
            dd = of32.next()
            o.stt(dd, dd[0:64, 0:N], lo, lo[0:64, 8:N + 8], 1.0 / wl, z, z[0:64, 8:N + 8], ALU.mult, ALU.subtract)
            o.stt(dd, dd[64:128, 0:N], hi, hi[64:128, 8:N + 8], 1.0 / wh, z, z[64:128, 8:N + 8], ALU.mult,
                  ALU.subtract)
            for (flag, cs, ic0) in ((first, 0, 0), (last, N - 8, 8)):
                if not flag:
                    continue
                for (src_b, r0, r1) in ((lo, 0, 64), (hi, 64, 128)):
                    o.tt("vector", dd, dd[r0:r1, cs:cs + 8], src_b, src_b[r0:r1, cs + 8:cs + 16], invc,
                         invc[r0:r1, seg, pt, ic0:ic0 + 8], ALU.mult)
                    o.tt("vector", dd, dd[r0:r1, cs:cs + 8], dd, dd[r0:r1, cs:cs + 8], z, z[r0:r1, cs + 8:cs + 16],
                         ALU.subtract)
            o.cp("gpsimd", d, d[:, 0:N], dd, dd[:, 0:N])
            pp = psB.next()
            o.mm(pp, pp[:, 0:N], plb, plb[:, pt, :], d, d[:, 0:N])
            yb = ob16.next()
            o.act(yb, yb[:, 0:N], pp, pp[:, 0:N], AF.Identity, bias=pv("zero"), scale=pv("psc", pt), extra_r=[pvec])
            store(ypool_d[pt * 128:(pt + 1) * 128, col0:col0 + N], yb, yb[:, 0:N])

        conv = {}
        for ht in range(6):
            z = evac_masked(2 + ht)
            c = cvp.next()
            cw0, _ = PV["cw"]
            o.act(c, c[:, 0:N], z, z[:, 7:N + 7], AF.Identity, bias=pv("cb", ht), scale=pv("cw", ht * 3 + 0),
                  extra_r=[pvec])
            o.stt(c, c[:, 0:N], z, z[:, 8:N + 8], pv("cw", ht * 3 + 1), c, c[:, 0:N], ALU.mult, ALU.add,
                  extra_r=[pvec])
            o.stt(c, c[:, 0:N], z, z[:, 9:N + 9], pv("cw", ht * 3 + 2), c, c[:, 0:N], ALU.mult, ALU.add,
                  extra_r=[pvec])
            if ht < 2:
                xb = ob16.next()
                o.cp("gpsimd", xb, xb[:, 0:N], c, c[:, 0:N])
                store(x0c_d[ht * 128:(ht + 1) * 128, col0:col0 + N], xb, xb[:, 0:N])
            elif ht < 4:
                conv[ht] = c
            else:
                x1 = conv[ht - 2]
                vv = of32.next()
                o.tt("vector", vv, vv[:, 0:N], c, c[:, 0:N], x1, x1[:, 0:N], ALU.mult)
                store(v2_d[(ht - 4) * 128:(ht - 3) * 128, col0:col0 + N], vv, vv[:, 0:N])

        if seg == 0:
            rc = ropeCp.next()
            rs = ropeSp.next()
            fw.dma("sync", rc[:, 0:N], ropeC_d[:, rope_c0:rope_c0 + N], writes=[rc])
            fw.dma("sync", rs[:, 0:N], ropeS_d[:, rope_c0:rope_c0 + N], writes=[rs])
        for qt in range(8):
            p = proj(8 + qt, 8, N + 8)
            f = qf.next()
            o.act(f, f[:, 0:N], p, p[:, 0:N], AF.Copy)
            s = qsq.next()
            o.act(s, s[:, 0:N], p, p[:, 0:N], AF.Square)
            pss = psB.next()
            o.mm(pss, pss[:, 0:N], cmb, cmb[:, 1, :], s, s[:, 0:N])
            r = qr.next()
            o.act(r, r[:, 0:N], pss, pss[:, 0:N], AF.Sqrt, bias=pv("eps"), scale=1.0, extra_r=[pvec])
            o.recip(r, r[:, 0:N], r, r[:, 0:N])
            gname = "gq" if qt < 4 else "gk"
            dst = q_d if qt < 4 else k_d
            hh = qt % 4
            if seg == 0:
                n_ = qn.next()
                o.stt(n_, n_[:, 0:N], f, f[:, 0:N], pv(gname), r, r[:, 0:N], ALU.mult, ALU.mult, extra_r=[pvec])
                pp2 = psB.next()
                o.mm(pp2, pp2[:, 0:N], cmb, cmb[:, 2, :], n_, n_[:, 0:N])
                t1 = t1p.next()
                o.tt("gpsimd", t1, t1[:, 0:N], n_, n_[:, 0:N], rc, rc[:, 0:N], ALU.mult)
                t2 = t2p.next()
                o.tt("vector", t2, t2[:, 0:N], pp2, pp2[:, 0:N], rs, rs[:, 0:N], ALU.mult)
                qo = ob16.next()
                o.tt("gpsimd", qo, qo[:, 0:N], t1, t1[:, 0:N], t2, t2[:, 0:N], ALU.add)
            else:
                qo = ob16.next()
                o.stt(qo, qo[:, 0:N], f, f[:, 0:N], pv(gname), r, r[:, 0:N], ALU.mult, ALU.mult, extra_r=[pvec])
            store(dst[hh * 128:(hh + 1) * 128, col0:col0 + N], qo, qo[:, 0:N])

        for tb in range(N // 128):
            p = psB.next()
            for k in range(8):
                o.mm(p, p[:, :], u, u[:, k, 8 + tb * 128:8 + (tb + 1) * 128], wb, wb[:, k, 2048:2560],
                     start=(k == 0), stop=(k == 7))
            vb = vout.next()
            o.act(vb, vb[:, :], p, p[:, :], AF.Copy)
            if vsplit:
                for hh_ in range(4):
                    store(v_d[hh_, col0 + tb * 128:col0 + (tb + 1) * 128, :], vb, vb[:, hh_ * 128:(hh_ + 1) * 128])
            else:
                store(v_d[col0 + tb * 128:col0 + (tb + 1) * 128, :], vb, vb[:, :])

    nlc = TL // NCH
    work = [(0, xT, ci * NCH, NCH, ci == 0, ci == nlc - 1, ci * NCH, ci * NCH) for ci in range(nlc)]
    work.append((1, hT, 0, TC, True, True, TL, 0))
    u_next = prep(work[0][0], work[0][1], work[0][2], work[0][3])
    for wi, wk in enumerate(work):
        u_cur = u_next
        if wi + 1 < len(work):
            nx = work[wi + 1]
            u_next = prep(nx[0], nx[1], nx[2], nx[3])
        chunk(u_cur, *wk)
    return env.done(out_evs)


NPV2 = 5


def build_p2(L, C=256, with_ctx=True, NQ=512, env=None, nq=None, LOOK=2, NBUF=4):
    env = env or Env()
    nc, fw, o = env.nc, env.fw, env.o
    LQ = L + (C if with_ctx else 0)
    LK = C + L
    nkt = LK // 128
    if nq is None:
        q_d = env.io("qT", [128, LQ], BF16, "ExternalInput")
        k_d = env.io("kT", [128, LK], BF16, "ExternalInput")
        v_d = env.io("v", [LK, 128], BF16, "ExternalInput")
        out_d = env.io("attT", [128, LQ], BF16, "ExternalOutput")
        qsrc = lambda q0, N: q_d[:, q0:q0 + N]
        ksrcs = [(c0, min(LK, c0 + 2048), k_d[:, c0:min(LK, c0 + 2048)]) for c0 in range(0, LK, 2048)]
        vsrc_ = v_d.rearrange("(n p) d -> p n d", p=128)
        vsrcs = [(n0, min(nkt, n0 + 16), vsrc_[:, n0:min(nkt, n0 + 16), :]) for n0 in range(0, nkt, 16)]
        odst = lambda q0, N: [out_d[:, q0:q0 + N]]
    else:
        TLq = L // nq
        Tq = TLq + C
        q_r = env.io("q_recv", [nq, 128, Tq], BF16, "ExternalInput")
        k_r = env.io("k_recv", [nq, 128, Tq], BF16, "ExternalInput")
        v_r = env.io("v_recv", [nq, Tq, 128], BF16, "ExternalInput")
        a_s = env.io("att_send", [nq, 128, Tq], BF16, "ExternalOutput")
        def qsrc(q0, N):
            if q0 >= L:
                return q_r[0, :, TLq:TLq + N]
            return q_r[q0 // TLq, :, q0 % TLq:q0 % TLq + N]
        ksrcs = [(0, C, k_r[0, :, TLq:Tq])]
        vsrcs = [(0, C // 128, v_r[0, TLq:Tq, :].rearrange("(n p) d -> p n d", p=128))]
        for qd_ in range(nq):
            for c0 in range(0, TLq, 2048):
                c1 = min(TLq, c0 + 2048)
                ksrcs.append((C + qd_ * TLq + c0, C + qd_ * TLq + c1, k_r[qd_, :, c0:c1]))
                vsrcs.append(((C + qd_ * TLq + c0) // 128, (C + qd_ * TLq + c1) // 128,
                              v_r[qd_, c0:c1, :].rearrange("(n p) d -> p n d", p=128)))
        def odst(q0, N):
            if q0 >= L:
                return [a_s[qd_, :, TLq:TLq + N] for qd_ in range(nq)]
            return [a_s[q0 // TLq, :, q0 % TLq:q0 % TLq + N]]
    dl_d = env.io("dlam", [128, 4, 64], F32, "ExternalInput")
    pv_d = env.io("pvec", [128, NPV2], F32, "ExternalInput")
    id_d = env.io("ident", [128, 128], F32, "ExternalInput")
    pvec = fw.sb("pvec", [128, NPV2], F32)
    fw.dma("sync", pvec[:], pv_d, writes=[pvec])
    id32 = fw.sb("id32", [128, 128], F32)
    fw.dma("sync", id32[:], id_d, writes=[id32])
    idb = fw.sb("idb", [128, 128], BF16)
    o.cp("vector", idb, idb[:], id32, id32[:])
    dl = fw.sb("dl", [128, 4, 64], F32)
    fw.dma("sync", dl[:], dl_d, writes=[dl])
    pr = fw.sb("pr", [128, 2, 64], F32)
    o.tt("vector", pr, pr[:, 0, :], dl, dl[:, 0, :], dl, dl[:, 1, :], ALU.mult)
    o.tt("vector", pr, pr[:, 1, :], dl, dl[:, 2, :], dl, dl[:, 3, :], ALU.mult)
    sm = fw.sb("sm", [128, 2], F32)
    fw.op("vector", lambda e: e.reduce_sum(out=sm[:], in_=pr[:], axis=AX.X), [pr], [sm])
    ex = fw.sb("ex", [128, 2], F32)
    o.act(ex, ex[:], sm, sm[:], AF.Exp)
    neglam = fw.sb("neglam", [128, 1], F32)
    o.tt("vector", neglam, neglam[:], ex, ex[:, 1:2], ex, ex[:, 0:1], ALU.subtract)
    o.tt("vector", neglam, neglam[:], neglam, neglam[:], pvec, pvec[:, 1:2], ALU.subtract)
    gsc = fw.sb("gsc", [128, 1], F32)
    o.tt("vector", gsc, gsc[:], pvec, pvec[:, 2:3], pvec, pvec[:, 3:4], ALU.mult)

    kT = fw.sb("kT", [128, LK], BF16)
    for (c0, c1, src_) in ksrcs:
        fw.dma("sync", kT[:, c0:c1], src_, writes=[kT])
    vt = fw.sb("vt", [128, nkt, 129], BF16)
    o.memset("gpsimd", vt, vt[:, :, 128:129], 1.0)
    for (n0, n1, src_) in vsrcs:
        fw.dma("gpsimd", vt[:, n0:n1, 0:128], src_, writes=[vt])

    qp = Pool(fw, "q", [128, NQ], BF16, 2)
    psS = Pool(fw, "psS", [128, 512], F32, 4, space="ps")
    accb = [fw.ps("acc%d" % i, [128, 512], F32) for i in range(3)]
    trb = fw.ps("trb", [128, 512], BF16)
    Pp = Pool(fw, "P", [128, NQ], BF16, NBUF)
    small = Pool(fw, "small", [128, 8], F32, 4)
    o1p = Pool(fw, "o1", [128, 128], F32, 2)
    o2p = Pool(fw, "o2", [128, 128], F32, 2)
    junk = Pool(fw, "junk", [128, 128], F32, 2)
    onp = Pool(fw, "on", [128, 128], BF16, 2)
    outp = Pool(fw, "outp", [128, NQ], BF16, 2)
    out_evs = []

    def qchunk(q0, N, kts):
        q = qp.next()
        fw.dma("sync", q[:, 0:N], qsrc(q0, N), writes=[q])
        nqb = N // 128
        started = set()
        steps = [(ki, kt, m) for ki, kt in enumerate(kts) for m in range(2)]
        Ps = {}

        def emit_S(i):
            ki, kt, m = steps[i]
            ps = psS.next()
            o.mm(ps, ps[:, 0:N], kT, kT[64 * m:64 * m + 64, kt * 128:(kt + 1) * 128], q, q[64 * m:64 * m + 64, 0:N])
            P = Pp.next()
            o.act(P, P[:, 0:N], ps, ps[:, 0:N], AF.Exp, scale=0.125)
            Ps[i] = P

        def emit_PV(i):
            ki, kt, m = steps[i]
            P = Ps.pop(i)
            for qb in range(nqb):
                a = m * 4 + qb
                bank, slot = a // 3, a % 3
                ab = accb[bank]
                st = bank not in started
                started.add(bank)
                o.mm(ab, ab[:, slot * 129:(slot + 1) * 129], P, P[:, qb * 128:(qb + 1) * 128], vt, vt[:, kt, :],
                     start=st, stop=(ki == len(kts) - 1), skip_group_check=True)

        for i in range(min(LOOK, len(steps))):
            emit_S(i)
        for i in range(len(steps)):
            if i + LOOK < len(steps):
                emit_S(i + LOOK)
            emit_PV(i)
        for qb in range(nqb):
            a0, a1 = qb, 4 + qb
            A0 = accb[a0 // 3]; s0 = (a0 % 3) * 129
            A1 = accb[a1 // 3]; s1 = (a1 % 3) * 129
            sm_ = small.next()
            o.recip(sm_, sm_[:, 0:1], A0, A0[:, s0 + 128:s0 + 129])
            o.recip(sm_, sm_[:, 1:2], A1, A1[:, s1 + 128:s1 + 129])
            o.tt("vector", sm_, sm_[:, 2:3], sm_, sm_[:, 1:2], neglam, neglam[:], ALU.mult)
            o1 = o1p.next()
            o.ts("vector", o1, o1[:], A0, A0[:, s0:s0 + 128], sm_[:, 0:1], None, ALU.mult, extra_r=[sm_])
            o2 = o2p.next()
            o.stt(o2, o2[:], A1, A1[:, s1:s1 + 128], sm_[:, 2:3], o1, o1[:], ALU.mult, ALU.add, extra_r=[sm_])
            jk = junk.next()
            o.act(jk, jk[:], o2, o2[:], AF.Square, accum=(sm_, sm_[:, 3:4]))
            o.act(sm_, sm_[:, 4:5], sm_, sm_[:, 3:4], AF.Sqrt, bias=pvec[:, 0:1], scale=1.0 / 128.0, extra_r=[pvec])
            o.recip(sm_, sm_[:, 5:6], sm_, sm_[:, 4:5])
            on = onp.next()
            o.ts("vector", on, on[:], o2, o2[:], sm_[:, 5:6], None, ALU.mult, extra_r=[sm_])
            o.tr(trb, trb[:, qb * 128:(qb + 1) * 128], on, on[:], idb, idb[:])
        ot = outp.next()
        o.act(ot, ot[:, 0:N], trb, trb[:, 0:N], AF.Identity, bias=pvec[:, 4:5], scale=gsc[:, 0:1], extra_r=[pvec, gsc])
        for dst_ in odst(q0, N):
            out_evs.append(fw.dma("gpsimd", dst_, ot[:, 0:N], reads=[ot]))

    allk = list(range(nkt))
    for qc in range(L // NQ):
        qchunk(qc * NQ, NQ, allk)
    if with_ctx:
        qchunk(L, C, list(range(C // 128)))
    return env.done(out_evs)


PI = float(np.pi)


def lc_tables(L):
    M = L // 128
    N2 = 2 * M
    N = 2 * L
    kb = min(128, N2)
    nb2 = (N2 + 127) // 128
    f64 = np.float64
    n2 = np.arange(M, dtype=f64)[:, None]; k2 = np.arange(N2, dtype=f64)[None, :]
    ang = 2 * np.pi * n2 * k2 / N2
    F2 = np.concatenate([np.cos(ang), -np.sin(ang)], 1)
    n1 = np.arange(128, dtype=f64)[:, None]
    ang = 2 * np.pi * n1 * k2 / N
    Tr, Ti = np.cos(ang), -np.sin(ang)
    TA = np.concatenate([Tr, Tr], 1); TB = np.concatenate([Ti, Ti], 1)
    k1 = np.arange(128, dtype=f64)[None, :]
    ang = 2 * np.pi * n1 * k1 / 128
    Fr, Fi = np.cos(ang), -np.sin(ang)
    F1 = np.stack([Fr, Fi, -Fi], 1)
    Gr, Gi = np.cos(ang), np.sin(ang)
    G1 = np.stack([np.concatenate([Gr, Gi], 1), np.concatenate([-Gi, Gr], 1)], 1)
    T2 = np.zeros((kb, nb2, 2, 256)); G2 = np.zeros((kb, nb2, 2, M))
    for j in range(nb2):
        kk = (np.arange(kb, dtype=f64) + j * 128)[:, None]
        ang = 2 * np.pi * kk * np.arange(128, dtype=f64)[None, :] / N
        T2[:, j, 0] = np.concatenate([np.cos(ang), np.cos(ang)], 1)
        T2[:, j, 1] = np.concatenate([np.sin(ang), np.sin(ang)], 1)
        ang = 2 * np.pi * kk * np.arange(M, dtype=f64)[None, :] / N2
        G2[:, j, 0] = np.cos(ang) / N
        G2[:, j, 1] = -np.sin(ang) / N
    f = lambda a: np.ascontiguousarray(a.astype(np.float32))
    return {"F2": f(F2), "TA": f(TA), "TB": f(TB), "F1": f(F1), "G1": f(G1), "T2": f(T2), "G2": f(G2)}


def filt_tables(L, ch0, nch=32):
    f32 = np.float32
    t = np.linspace(0.0, 1.0, L, dtype=f32)[:, None]
    w = (f32(2.0 * np.pi / L) * np.arange(L, dtype=f32))[:, None]
    bands = np.linspace(1e-4, 15, 16, dtype=f32)[None, :]
    feat = np.concatenate([t, np.cos(bands * w), -np.sin(bands * w)], -1).astype(f32)
    max_decay = np.log(1.0 / 1e-2) / 0.3
    min_decay = np.log(1.0 / 1e-2) / 1.5
    deltas = np.linspace(min_decay, max_decay, 256, dtype=f32)
    dec = np.exp(-t * deltas[None, ch0:ch0 + nch]).astype(f32)
    dec2 = np.concatenate([dec, dec], 1).T
    return np.ascontiguousarray(feat.T), np.ascontiguousarray(dec2)


def build_p3(Ls=(16384, 256), nch=32, env=None, nq=None):
    env = env or Env()
    nc, fw, o = env.nc, env.fw, env.o
    R = 2 * nch
    w1_d = env.io("w1", [33, 64], F32, "ExternalInput")
    w2_d = env.io("w2", [64, 64], F32, "ExternalInput")
    w3_d = env.io("w3s", [64, R], F32, "ExternalInput")
    fpv_d = env.io("fpv", [64, 4], F32, "ExternalInput")
    sel_d = env.io("sel", [R, nch], F32, "ExternalInput")
    w1 = fw.sb("w1", [33, 64], F32); w2 = fw.sb("w2", [64, 64], F32); w3s = fw.sb("w3s", [64, R], F32)
    fpv = fw.sb("fpv", [64, 6], F32); sel = fw.sb("sel", [R, nch], F32)
    fw.dma("sync", w1[:], w1_d, writes=[w1]); fw.dma("sync", w2[:], w2_d, writes=[w2])
    fw.dma("sync", w3s[:], w3_d, writes=[w3s]); fw.dma("sync", fpv[:, 0:4], fpv_d, writes=[fpv])
    fw.dma("sync", sel[:], sel_d, writes=[sel])
    o.tt("vector", fpv, fpv[:, 4:5], fpv, fpv[:, 0:1], fpv, fpv[:, 1:2], ALU.mult)
    o.tt("vector", fpv, fpv[:, 5:6], fpv, fpv[:, 2:3], fpv, fpv[:, 3:4], ALU.mult)
    ones = fw.sb("ones", [128, 128], F32)
    o.memset("vector", ones, ones[:], 1.0)

    PSP = Pool(fw, "PSP", [128, 512], F32, 2, space="ps")
    PSX = Pool(fw, "PSX", [128, 512], F32, 2, space="ps")
    PSQ = Pool(fw, "PSQ", [128, 512], F32, 2, space="ps")
    PSY = Pool(fw, "PSY", [128, 512], F32, 2, space="ps")

    class _Alt:
        def __init__(self, a, b):
            self.p = (a, b); self.i = 0
        def next(self):
            self.i += 1
            return self.p[self.i % 2].next()
    PS = _Alt(PSP, PSX)
    featp = Pool(fw, "feat", [33, 512], F32, 2)
    decp = Pool(fw, "dec", [R, 512], F32, 2)
    prep = Pool(fw, "pre", [64, 512], F32, 2)
    mkp = Pool(fw, "mk", [64, 512], F32, 2)
    hp = Pool(fw, "hh", [64, 512], F32, 3)
    kp = Pool(fw, "kk", [R, 512], F32, 2)
    jkp = Pool(fw, "jk", [R, 512], F32, 2)
    xin = Pool(fw, "xin", [128, 128], F32, 6)
    sP = Pool(fw, "sP", [128, 512], F32, 4)
    sA = Pool(fw, "sAA", [128, 512], F32, 4)
    sB = Pool(fw, "sBB", [128, 512], F32, 4)
    sZ = Pool(fw, "sZ", [128, 512], F32, 4)
    sK = Pool(fw, "sK", [128, 2, 512], F32, 2)
    sXa = Pool(fw, "sXa", [128, 512], F32, 4)
    sXv = Pool(fw, "sXv", [128, 512], F32, 4)
    sQ = Pool(fw, "sQ", [128, 256], F32, 4)
    sQA = Pool(fw, "sQA", [128, 256], F32, 3)
    sQB = Pool(fw, "sQB", [128, 256], F32, 3)
    sZ2 = Pool(fw, "sZ2", [128, 2, 256], F32, 3)
    sY = Pool(fw, "sY", [128, 128], F32, 3)
    out_evs = []

    for L in Ls:
        sfx = "_%d" % L
        M = L // 128
        N2 = 2 * M
        W2 = 2 * N2
        kb = min(128, N2)
        nb2 = (N2 + 127) // 128
        d = {}
        for nm, shp in (("F2", [M, W2]), ("TA", [128, W2]), ("TB", [128, W2]), ("F1", [128, 3, 128]),
                        ("G1", [128, 2, 256]), ("T2", [kb, nb2, 2, 256]), ("G2", [kb, nb2, 2, M])):
            d[nm] = env.io(nm + sfx, shp, F32, "ExternalInput")
        feat_d = env.io("feat" + sfx, [33, L], F32, "ExternalInput")
        dec_d = env.io("dec" + sfx, [R, L], F32, "ExternalInput")
        if nq is None:
            v_d = env.io("vin" + sfx, [R, L], F32, "ExternalInput")
            y_d = env.io("yout" + sfx, [R, L], F32, "ExternalOutput")
        else:
            Lmain = Ls[0]
            TLq = Lmain // nq
            Tq = TLq + Ls[1]
            v2r = env.io("v2_recv", [8, nch, Tq], F32, "ExternalInput")
            cvs = env.io("conv_send", [8, nch, Tq], F32, "ExternalOutput")
        kf = fw.dram("kf" + sfx, [R, L], F32)
        tb = {}
        for nm in d:
            tb[nm] = fw.sb("t" + nm + sfx, list(d[nm].shape), F32)
            fw.dma("sync", tb[nm][:], d[nm], writes=[tb[nm]])
        CH = min(512, L)
        nchk = L // CH
        rs = fw.sb("rs" + sfx, [R, nchk], F32)

        def sin_layer(ps, fcol, bcol):
            pre = prep.next()
            o.ts("vector", pre, pre[:, 0:CH], ps, ps[0:64, 0:CH], fpv[:, fcol:fcol + 1], fpv[:, bcol:bcol + 1],
                 ALU.mult, ALU.add, extra_r=[fpv])
            mk = mkp.next()
            o.ts("gpsimd", mk, mk[:, 0:CH], pre, pre[:, 0:CH], PI, None, ALU.is_gt)
            o.stt(pre, pre[:, 0:CH], mk, mk[:, 0:CH], -2.0 * PI, pre, pre[:, 0:CH], ALU.mult, ALU.add)
            mk2 = mkp.next()
            o.ts("gpsimd", mk2, mk2[:, 0:CH], pre, pre[:, 0:CH], -PI, None, ALU.is_lt)
            o.stt(pre, pre[:, 0:CH], mk2, mk2[:, 0:CH], 2.0 * PI, pre, pre[:, 0:CH], ALU.mult, ALU.add)
            h = hp.next()
            o.act(h, h[:, 0:CH], pre, pre[:, 0:CH], AF.Sin)
            return h

        for ci in range(nchk):
            c0 = ci * CH
            ft = featp.next()
            fw.dma("sync", ft[:, 0:CH], feat_d[:, c0:c0 + CH], writes=[ft])
            dc = decp.next()
            fw.dma("sync", dc[:, 0:CH], dec_d[:, c0:c0 + CH], writes=[dc])
            ps = PS.next()
            o.mm(ps, ps[0:64, 0:CH], w1, w1[:, :], ft, ft[:, 0:CH])
            h1 = sin_layer(ps, 0, 4)
            ps = PS.next()
            o.mm(ps, ps[0:64, 0:CH], w2, w2[:, :], h1, h1[:, 0:CH])
            h2 = sin_layer(ps, 2, 5)
            ps = PS.next()
            o.mm(ps, ps[0:R, 0:CH], w3s, w3s[:, :], h2, h2[:, 0:CH])
            k = kp.next()
            o.tt("vector", k, k[:, 0:CH], ps, ps[0:R, 0:CH], dc, dc[:, 0:CH], ALU.mult)
            if ci == 0:
                o.memset("vector", k, k[nch:R, 0:1], 0.0)
            j = jkp.next()
            o.act(j, j[:, 0:CH], k, k[:, 0:CH], AF.Abs, accum=(rs, rs[:, ci:ci + 1]))
            fw.dma("gpsimd", kf[:, c0:c0 + CH], k[:, 0:CH], reads=[k], writes=[kf])
        rtot = fw.sb("rtot" + sfx, [R, 1], F32)
        fw.op("vector", lambda e, rtot=rtot, rs=rs: e.reduce_sum(out=rtot[:], in_=rs[:], axis=AX.X), [rs], [rtot])
        Rb = fw.sb("Rb" + sfx, [R, 128], F32)
        o.ts("vector", Rb, Rb[:], ones, ones[0:R, :], rtot[:, 0:1], None, ALU.mult, extra_r=[rtot])
        pss = PS.next()
        o.mm(pss, pss[:, 0:nch], Rb, Rb[:, :], sel, sel[:, :])
        Sinv = fw.sb("Sinv" + sfx, [128, 2, nch], F32)
        o.recip(Sinv, Sinv[:, 0, :], pss, pss[:, 0:nch])
        o.ts("vector", Sinv, Sinv[:, 1, :], Sinv, Sinv[:, 0, :], -1.0, None, ALU.mult)

        kfv = kf.t.ap().rearrange("r (a i) -> r a i", i=128)
        if nq is None:
            vv = v_d.rearrange("r (a i) -> r a i", i=128)
            yv = y_d.rearrange("r (a i) -> r a i", i=128)
            vin_parts = lambda r: [(0, M, vv[r])]
            yout_parts = lambda r: [(0, M, yv[r])]
        elif L == Ls[0]:
            mq = M // nq
            def vin_parts(r, v2r=v2r, mq=mq, TLq=TLq):
                b_, c_ = r // nch, r % nch
                return [(qd_ * mq, (qd_ + 1) * mq, v2r[b_ * nq + qd_, c_, 0:TLq].rearrange("(a i) -> a i", i=128))
                        for qd_ in range(nq)]
            def yout_parts(r, cvs=cvs, mq=mq, TLq=TLq):
                b_, c_ = r // nch, r % nch
                return [(qd_ * mq, (qd_ + 1) * mq, cvs[b_ * nq + qd_, c_, 0:TLq].rearrange("(a i) -> a i", i=128))
                        for qd_ in range(nq)]
        else:
            def vin_parts(r, v2r=v2r, TLq=TLq, Tq=Tq, M=M):
                b_, c_ = r // nch, r % nch
                return [(0, M, v2r[b_ * nq, c_, TLq:Tq].rearrange("(a i) -> a i", i=128))]
            def yout_parts(r, cvs=cvs, TLq=TLq, Tq=Tq, M=M):
                b_, c_ = r // nch, r % nch
                return [(0, M, cvs[b_ * nq + qd_, c_, TLq:Tq].rearrange("(a i) -> a i", i=128)) for qd_ in range(nq)]

        F1 = tb["F1"]; G1 = tb["G1"]; T2 = tb["T2"]; G2 = tb["G2"]

        def fwd_pair(srcs):
            Pl = []
            for (parts, bufs) in srcs:
                x = xin.next()
                for (p0_, p1_, ap_) in parts:
                    fw.dma("sync", x[p0_:p1_, :], ap_, reads=bufs, writes=[x])
                P = PSP.next()
                o.mm(P, P[:, 0:W2], x, x[0:M, :], tb["F2"], tb["F2"][:, :])
                Pl.append(P)
            Zl = []
            for P in Pl:
                Ps = sP.next()
                o.act(Ps, Ps[:, 0:W2], P, P[:, 0:W2], AF.Copy)
                A = sA.next(); B = sB.next()
                o.tt("vector", A, A[:, 0:W2], Ps, Ps[:, 0:W2], tb["TA"], tb["TA"][:, :], ALU.mult)
                o.tt("vector", B, B[:, 0:W2], Ps, Ps[:, 0:W2], tb["TB"], tb["TB"][:, :], ALU.mult)
                Z = sZ.next()
                o.tt("vector", Z, Z[:, 0:N2], A, A[:, 0:N2], B, B[:, N2:W2], ALU.subtract)
                o.tt("vector", Z, Z[:, N2:W2], B, B[:, 0:N2], A, A[:, N2:W2], ALU.add)
                Zl.append(Z)
            Xl = []
            for Z in Zl:
                X = PSX.next()
                o.mm(X, X[:, 0:N2], F1, F1[:, 0, :], Z, Z[:, 0:N2], start=True, stop=False)
                o.mm(X, X[:, 0:N2], F1, F1[:, 2, :], Z, Z[:, N2:W2], start=False, stop=True)
                o.mm(X, X[:, N2:W2], F1, F1[:, 1, :], Z, Z[:, 0:N2], start=True, stop=False, skip_group_check=True)
                o.mm(X, X[:, N2:W2], F1, F1[:, 0, :], Z, Z[:, N2:W2], start=False, stop=True, skip_group_check=True)
                Xl.append(X)
            return Xl

        def filt_fwd(c):
            Xa, Xb = fwd_pair([([(0, M, kfv[c])], [kf]), ([(0, M, kfv[nch + c])], [kf])])
            Xas = sXa.next(); Xbs = sXa.next()
            o.act(Xas, Xas[:, 0:W2], Xa, Xa[:, 0:W2], AF.Identity, bias=0.0, scale=Sinv[:, 0, c:c + 1], extra_r=[Sinv])
            o.act(Xbs, Xbs[:, 0:W2], Xb, Xb[:, 0:W2], AF.Identity, bias=0.0, scale=Sinv[:, 0, c:c + 1], extra_r=[Sinv])
            return Xas, Xbs

        nxt = filt_fwd(0)
        for c in range(nch):
            Xas, Xbs = nxt
            Xv = fwd_pair([(vin_parts(b * nch + c), []) for b in range(2)])
            Xsb = []
            for b in range(2):
                Xs = sXv.next()
                o.act(Xs, Xs[:, 0:W2], Xv[b], Xv[b][:, 0:W2], AF.Copy)
                Xsb.append((b * nch + c, Xs))
            K = sK.next()
            o.tt("vector", K, K[:, 0, 0:N2], Xas, Xas[:, 0:N2], Xbs, Xbs[:, 0:N2], ALU.add)
            o.tt("vector", K, K[:, 1, 0:N2], Xas, Xas[:, N2:W2], Xbs, Xbs[:, N2:W2], ALU.subtract)
            o.cp("vector", K, K[:, 0, N2:W2], K, K[:, 0, 0:N2])
            o.cp("vector", K, K[:, 1, N2:W2], K, K[:, 1, 0:N2])
            Ysb = []
            for (r, Xs) in Xsb:
                A = sA.next(); B = sB.next()
                o.tt("vector", A, A[:, 0:W2], Xs, Xs[:, 0:W2], K, K[:, 0, 0:W2], ALU.mult)
                o.tt("vector", B, B[:, 0:W2], Xs, Xs[:, 0:W2], K, K[:, 1, 0:W2], ALU.mult)
                Y = sZ.next()
                o.tt("vector", Y, Y[:, 0:N2], A, A[:, 0:N2], B, B[:, N2:W2], ALU.subtract)
                o.tt("vector", Y, Y[:, N2:W2], B, B[:, 0:N2], A, A[:, N2:W2], ALU.add)
                Ysb.append((r, Y))
            if c + 1 < nch:
                nxt = filt_fwd(c + 1)
            Zsb = []
            for (r, Y) in Ysb:
                Z2 = sZ2.next()
                for j in range(nb2):
                    Q = PSQ.next()
                    o.mm(Q, Q[0:kb, 0:256], Y, Y[:, j * 128:j * 128 + kb], G1, G1[:, 0, :], start=True, stop=False)
                    o.mm(Q, Q[0:kb, 0:256], Y, Y[:, N2 + j * 128:N2 + j * 128 + kb], G1, G1[:, 1, :], start=False,
                         stop=True)
                    Qs = sQ.next()
                    o.act(Qs, Qs[0:kb, :], Q, Q[0:kb, 0:256], AF.Copy)
                    QA = sQA.next(); QB = sQB.next()
                    o.tt("vector", QA, QA[0:kb, :], Qs, Qs[0:kb, :], T2, T2[:, j, 0, :], ALU.mult)
                    o.tt("vector", QB, QB[0:kb, :], Qs, Qs[0:kb, :], T2, T2[:, j, 1, :], ALU.mult)
                    o.tt("vector", Z2, Z2[0:kb, j, 0:128], QA, QA[0:kb, 0:128], QB, QB[0:kb, 128:256], ALU.subtract)
                    o.tt("vector", Z2, Z2[0:kb, j, 128:256], QB, QB[0:kb, 0:128], QA, QA[0:kb, 128:256], ALU.add)
                Zsb.append((r, Z2))
            for (r, Z2) in Zsb:
                yp = PSY.next()
                for j in range(nb2):
                    o.mm(yp, yp[0:M, 0:128], G2, G2[:, j, 0, :], Z2, Z2[0:kb, j, 0:128], start=(j == 0), stop=False)
                    o.mm(yp, yp[0:M, 0:128], G2, G2[:, j, 1, :], Z2, Z2[0:kb, j, 128:256], start=False,
                         stop=(j == nb2 - 1))
                ys = sY.next()
                o.act(ys, ys[0:M, :], yp, yp[0:M, 0:128], AF.Copy)
                for (p0_, p1_, ap_) in yout_parts(r):
                    if p1_ - p0_ == M and p0_ == 0:
                        out_evs.append(fw.dma("gpsimd", ap_, ys[0:M, :], reads=[ys]))
                    else:
                        out_evs.append(fw.dma("gpsimd", ap_, ys[p0_:p1_, :], reads=[ys]))
    return env.done(out_evs)


D = 1024
PV4 = {}
_c4 = 0
def _add4(name, n):
    global _c4
    PV4[name] = (_c4, n)
    _c4 += n
_add4("hyb", 2)
_add4("g2_l", 8); _add4("g2_c", 8)
_add4("n2g", 8)
_add4("sh_l", 8); _add4("sc_l", 8); _add4("sh_c", 8); _add4("sc_c", 8)
_add4("g5_l", 8); _add4("g5_c", 8)
_add4("eps", 1); _add4("zero", 1)
NPV4 = _c4


def build_p4(TL, TC=256, moe=False, NCH=512, env=None, halo=False, mv=None):
    T = TL + TC
    env = env or Env()
    nc, fw, o = env.nc, env.fw, env.o
    if not halo:
        xT = env.io("xT", [D, T], F32, "ExternalInput")
        xsrc = lambda seg, k, c0, N: xT[k * 128:(k + 1) * 128, c0:c0 + N]
    else:
        XH = env.io("XH", [D, TL + 16], F32, "ExternalInput")
        HH = env.io("HH", [D, TC + 16], F32, "ExternalInput")
        xsrc = lambda seg, k, c0, N: (XH[k * 128:(k + 1) * 128, 8 + c0:8 + c0 + N] if seg == 0 else
                                      HH[k * 128:(k + 1) * 128, 8 + c0 - TL:8 + c0 - TL + N])
    ypool_d = env.io("ypool", [256, T], BF16, "ExternalInput")
    x0c_d = env.io("x0c", [256, T], BF16, "ExternalInput")
    v2_d = env.io("v2", [256, T], F32, "ExternalInput")
    conv_d = env.io("conv", [256, T], F32, "ExternalInput")
    att_d = env.io("attT", [512, T], BF16, "ExternalInput")
    wout_d = env.io("w_out", [D, D], F32, "ExternalInput")
    pvec_d = env.io("pvec", [128, NPV4], F32, "ExternalInput")
    ones_d = env.io("onesD", [128, 128], F32, "ExternalInput")
    x1_d = env.io("x1T", [D, T], F32, "ExternalOutput")
    u2_d = env.io("u2T", [D, T], BF16, "ExternalOutput")
    if moe:
        rw_d = env.io("rw", [128, 8, 8], F32, "ExternalInput")
        id_d = env.io("ident", [128, 128], F32, "ExternalInput")
        comb_d = env.io("combT", [8, T], F32, "ExternalOutput")
    pvec = fw.sb("pvec", [128, NPV4], F32)
    fw.dma("sync", pvec[:], pvec_d, writes=[pvec])
    if mv is not None:
        mvb, l_ = mv
        for nm_, t0_, g_ in (("g2_l", 16, 0), ("g2_c", 16, 1), ("sh_l", 24, 0), ("sc_l", 32, 0), ("sh_c", 24, 1),
                             ("sc_c", 32, 1), ("g5_l", 40, 0), ("g5_c", 40, 1)):
            c0_, _ = PV4[nm_]
            o.cp("vector", pvec, pvec[:, c0_:c0_ + 8], mvb, mvb[:, l_, t0_:t0_ + 8, g_])
    def pv(name, i=0):
        c0, n = PV4[name]
        return pvec[:, c0 + i:c0 + i + 1]
    on32 = fw.sb("on32", [128, 128], F32)
    fw.dma("sync", on32[:], ones_d, writes=[on32])
    onb = fw.sb("onb", [128, 128], BF16)
    o.cp("vector", onb, onb[:], on32, on32[:])
    Amod = fw.sb("Amod", [128, 2, 8], F32)
    for si, nm in enumerate(["sc_l", "sc_c"]):
        c0, _ = PV4[nm]
        g0, _ = PV4["n2g"]
        o.stt(Amod, Amod[:, si, :], pvec, pvec[:, c0:c0 + 8], 1.0, pvec, pvec[:, g0:g0 + 8], ALU.add, ALU.mult)
    wb = fw.sb("wb", [128, 8, D], BF16)
    stg = Pool(fw, "wstg", [128, D], F32, 2)
    for k in range(8):
        s = stg.next()
        fw.dma("sync" if k % 2 == 0 else "gpsimd", s[:], wout_d[k * 128:(k + 1) * 128, :], writes=[s])
        o.cp("gpsimd" if k % 2 == 0 else "vector", wb, wb[:, k, :], s, s[:])
    if moe:
        rw = fw.sb("rw", [128, 8, 8], F32)
        fw.dma("sync", rw[:], rw_d, writes=[rw])
        ident = fw.sb("ident", [128, 128], F32)
        fw.dma("sync", ident[:], id_d, writes=[ident])

    xin = Pool(fw, "xin", [128, 8, NCH], F32, 2)
    yin = Pool(fw, "yin", [128, 8, NCH], BF16, 2)
    x0p = Pool(fw, "x0", [128, 2, NCH], BF16, 2)
    v2p = Pool(fw, "v2", [128, 2, NCH], F32, 2)
    cvp = Pool(fw, "cv", [128, 2, NCH], F32, 2)
    tmp = Pool(fw, "tmp", [128, NCH], F32, 3)
    x1p = Pool(fw, "x1", [128, 8, NCH], F32, 2)
    sqp = Pool(fw, "sq", [128, NCH], BF16, 2)
    rstdp = Pool(fw, "rstd", [128, NCH], F32, 2)
    u2f = Pool(fw, "u2f", [128, 8, NCH], F32, 1 if moe else 1)
    u2b = Pool(fw, "u2b", [128, 8, NCH], BF16, 2)
    PS = Pool(fw, "PS", [128, 512], F32, 6, space="ps")
    sm = Pool(fw, "sm", [128, 64], F32, 4)
    cbo = Pool(fw, "cbo", [8, NCH], F32, 2)
    out_evs = []

    def chunk(seg, c0, N):
        x = xin.next()
        for k in range(8):
            fw.dma("sync", x[:, k, 0:N], xsrc(seg, k, c0, N), writes=[x])
        y = yin.next()
        for k in range(2):
            fw.dma("sync", y[:, k, 0:N], ypool_d[k * 128:(k + 1) * 128, c0:c0 + N], writes=[y])
        for k in range(4):
            fw.dma("sync", y[:, 4 + k, 0:N], att_d[k * 128:(k + 1) * 128, c0:c0 + N], writes=[y])
        x0 = x0p.next(); v2 = v2p.next(); cv = cvp.next()
        for k in range(2):
            fw.dma("gpsimd", x0[:, k, 0:N], x0c_d[k * 128:(k + 1) * 128, c0:c0 + N], writes=[x0])
            fw.dma("gpsimd", v2[:, k, 0:N], v2_d[k * 128:(k + 1) * 128, c0:c0 + N], writes=[v2])
            fw.dma("gpsimd", cv[:, k, 0:N], conv_d[k * 128:(k + 1) * 128, c0:c0 + N], writes=[cv])
        for k in range(2):
            t = tmp.next()
            o.stt(t, t[:, 0:N], v2, v2[:, k, 0:N], pv("hyb", k), cv, cv[:, k, 0:N], ALU.mult, ALU.add, extra_r=[pvec])
            o.tt("vector", y, y[:, 2 + k, 0:N], t, t[:, 0:N], x0, x0[:, k, 0:N], ALU.mult)
        x1 = x1p.next()
        gn = "g2_l" if seg == 0 else "g2_c"
        for n in range(8):
            p = PS.next()
            for k in range(8):
                o.mm(p, p[:, 0:N], wb, wb[:, k, n * 128:(n + 1) * 128], y, y[:, k, 0:N], start=(k == 0), stop=(k == 7))
            o.stt(x1, x1[:, n, 0:N], p, p[:, 0:N], pv(gn, n), x, x[:, n, 0:N], ALU.mult, ALU.add, extra_r=[pvec])
            out_evs.append(fw.dma("gpsimd", x1_d[n * 128:(n + 1) * 128, c0:c0 + N], x1[:, n, 0:N], reads=[x1]))
        pst = PS.next()
        for k in range(8):
            sq = sqp.next()
            o.act(sq, sq[:, 0:N], x1, x1[:, k, 0:N], AF.Square)
            o.mm(pst, pst[:, 0:N], onb, onb[:, :], sq, sq[:, 0:N], start=(k == 0), stop=(k == 7))
        rstd = rstdp.next()
        o.act(rstd, rstd[:, 0:N], pst, pst[:, 0:N], AF.Sqrt, bias=pv("eps"), scale=1.0, extra_r=[pvec])
        o.recip(rstd, rstd[:, 0:N], rstd, rstd[:, 0:N])
        uf = u2f.next(); ub = u2b.next()
        shn = "sh_l" if seg == 0 else "sh_c"
        for k in range(8):
            t = tmp.next()
            o.stt(t, t[:, 0:N], x1, x1[:, k, 0:N], Amod[:, seg, k:k + 1], rstd, rstd[:, 0:N], ALU.mult, ALU.mult,
                  extra_r=[Amod])
            if moe:
                o.act(uf, uf[:, k, 0:N], t, t[:, 0:N], AF.Identity, bias=pv(shn, k), scale=1.0, extra_r=[pvec])
                o.cp("gpsimd", ub, ub[:, k, 0:N], uf, uf[:, k, 0:N])
            else:
                o.act(ub, ub[:, k, 0:N], t, t[:, 0:N], AF.Identity, bias=pv(shn, k), scale=1.0, extra_r=[pvec])
            out_evs.append(fw.dma("gpsimd", u2_d[k * 128:(k + 1) * 128, c0:c0 + N], ub[:, k, 0:N], reads=[ub]))
        if moe:
            cps = PS.next()
            for tb in range(N // 128):
                p = PS.next()
                for k in range(8):
                    o.mm(p, p[:, 0:8], uf, uf[:, k, tb * 128:(tb + 1) * 128], rw, rw[:, k, :], start=(k == 0),
                         stop=(k == 7))
                s = sm.next()
                o.cp("vector", s, s[:, 0:8], p, p[:, 0:8])
                fw.op("vector", lambda e, s=s: e.reduce_max(out=s[:, 8:9], in_=s[:, 0:8], axis=AX.X), [s], [s])
                o.ts("vector", s, s[:, 9:10], s, s[:, 8:9], -1.0, None, ALU.mult)
                o.ts("vector", s, s[:, 10:18], s, s[:, 0:8], s[:, 8:9], None, ALU.is_equal)
                o.stt(s, s[:, 18:26], s, s[:, 10:18], -1e30, s, s[:, 0:8], ALU.mult, ALU.add)
                fw.op("vector", lambda e, s=s: e.reduce_max(out=s[:, 26:27], in_=s[:, 18:26], axis=AX.X), [s], [s])
                o.ts("vector", s, s[:, 27:35], s, s[:, 0:8], s[:, 26:27], None, ALU.is_ge)
                o.act(s, s[:, 35:43], s, s[:, 0:8], AF.Exp, bias=s[:, 9:10], scale=1.0)
                o.tt("vector", s, s[:, 35:43], s, s[:, 35:43], s, s[:, 27:35], ALU.mult)
                fw.op("vector", lambda e, s=s: e.reduce_sum(out=s[:, 43:44], in_=s[:, 35:43], axis=AX.X), [s], [s])
                o.recip(s, s[:, 44:45], s, s[:, 43:44])
                o.ts("vector", s, s[:, 45:53], s, s[:, 35:43], s[:, 44:45], None, ALU.mult)
                o.tr(cps, cps[0:8, tb * 128:(tb + 1) * 128], s, s[:, 45:53], ident, ident[:, :])
            cb = cbo.next()
            o.act(cb, cb[:, 0:N], cps, cps[0:8, 0:N], AF.Copy)
            out_evs.append(fw.dma("gpsimd", comb_d[:, c0:c0 + N], cb[:, 0:N], reads=[cb]))

    for ci in range(TL // NCH):
        chunk(0, ci * NCH, NCH)
    chunk(1, TL, TC)
    return env.done(out_evs)


def build_p5(TL, TC=256, moe=False, NCH=512, E=None, F=None, SCN=3, env=None, halo=False, last=False, mv=None, wl=None):
    T = TL + TC
    if E is None:
        E = 8 if moe else 1
    if F is None:
        F = 3584 if moe else 2816
    env = env or Env()
    nc, fw, o = env.nc, env.fw, env.o
    x1_d = env.io("x1T", [D, T], F32, "ExternalInput")
    u2_d = env.io("u2T", [D, T], BF16, "ExternalInput")
    wg_d = env.io("wg", [E, D, F], F32, "ExternalInput")
    wu_d = env.io("wu", [E, D, F], F32, "ExternalInput")
    wd_d = env.io("wd", [E, F, D], F32, "ExternalInput")
    pvec_d = env.io("pvec", [128, NPV4], F32, "ExternalInput")
    if not halo:
        x2_d = env.io("x2T", [D, T], F32, "ExternalOutput")
        x2dst = lambda seg, n, c0, N: [x2_d[n * 128:(n + 1) * 128, c0:c0 + N]]
    else:
        XH = env.io("XH", [D, TL + 16], F32, "ExternalOutput")
        HH = env.io("HH", [D, TC + 16], F32, "ExternalOutput")
        if last:
            OUT = env.io("outT", [D, TL], F32, "ExternalOutput")
        def x2dst(seg, n, c0, N):
            if seg == 1:
                return [HH[n * 128:(n + 1) * 128, 8 + c0 - TL:8 + c0 - TL + N]]
            if last:
                return [OUT[n * 128:(n + 1) * 128, c0:c0 + N]]
            return [XH[n * 128:(n + 1) * 128, 8 + c0:8 + c0 + N]]
    if moe:
        comb_d = env.io("combT", [8, T], F32, "ExternalInput")
        sel_d = env.io("selE", [8, 8, 128], F32, "ExternalInput")
    pvec = fw.sb("pvec", [128, NPV4], F32)
    fw.dma("sync", pvec[:], pvec_d, writes=[pvec])
    if mv is not None:
        mvb, l_ = mv
        for nm_, t0_, g_ in (("g2_l", 16, 0), ("g2_c", 16, 1), ("sh_l", 24, 0), ("sc_l", 32, 0), ("sh_c", 24, 1),
                             ("sc_c", 32, 1), ("g5_l", 40, 0), ("g5_c", 40, 1)):
            c0_, _ = PV4[nm_]
            o.cp("vector", pvec, pvec[:, c0_:c0_ + 8], mvb, mvb[:, l_, t0_:t0_ + 8, g_])
    def pv(name, i=0):
        c0, n = PV4[name]
        return pvec[:, c0 + i:c0 + i + 1]
    if moe:
        selE = fw.sb("selE", [8, 8, 128], F32)
        fw.dma("sync", selE[:], sel_d, writes=[selE])
    chunks = [(0, ci * NCH, NCH) for ci in range(TL // NCH)] + [(1, TL, TC)]
    scs = []
    per = (len(chunks) + SCN - 1) // SCN
    for i in range(0, len(chunks), per):
        scs.append(chunks[i:i + per])
    maxw = max(sum(c[2] for c in sc) for sc in scs)
    maxc = max(len(sc) for sc in scs)
    yacc = fw.sb("yacc", [128, 8, maxw], F32)
    u2 = fw.sb("u2", [128, 8, maxw], BF16)
    GF = 512
    groups = []
    for e in range(E):
        for f0 in range(0, F, GF):
            groups.append((e, f0, min(GF, F - f0)))
    wgp = Pool(fw, "wgb", [128, 8, GF], BF16, 2)
    wup = Pool(fw, "wub", [128, 8, GF], BF16, 2)
    wdp = Pool(fw, "wdb", [128, GF // 128, D], BF16, 2)
    stg = Pool(fw, "stg", [128, 2048], F32, 3)
    PSg = Pool(fw, "PSg", [128, 512], F32, 2, space="ps")
    PSu = Pool(fw, "PSu", [128, 512], F32, 2, space="ps")
    PSy = Pool(fw, "PSy", [128, 512], F32, 2, space="ps")
    PSc = Pool(fw, "PSc", [128, 512], F32, 1, space="ps")
    sgp = Pool(fw, "sg", [128, NCH], F32, 2)
    hp = Pool(fw, "h", [128, GF // 128, NCH], BF16, 2)
    cbp = Pool(fw, "cb", [128, maxc, NCH], F32, 2)
    cin = Pool(fw, "cin", [8, maxw], F32, 1)
    x1p = Pool(fw, "x1", [128, NCH], F32, 3)
    x2p = Pool(fw, "x2", [128, NCH], F32, 3)
    out_evs = []
    cast_engs = ["gpsimd", "vector", "gpsimd", "scalar"]
    cast_i = [0]

    def cast(dst_b, dst_ap, src_b, src_ap):
        e = cast_engs[cast_i[0] % len(cast_engs)]
        cast_i[0] += 1
        if e == "scalar":
            o.act(dst_b, dst_ap, src_b, src_ap, AF.Copy)
        else:
            o.cp(e, dst_b, dst_ap, src_b, src_ap)

    dq = ["sync", "gpsimd"]
    dqi = [0]
    def ldq():
        dqi[0] += 1
        return dq[dqi[0] % 2]

    def load_group(e, f0, fw_):
        wg = wgp.next(); wu = wup.next(); wd = wdp.next()
        for (dst, src) in ((wg, wg_d), (wu, wu_d)):
            for k2 in range(0, 8, 4):
                s = stg.next()
                for kk in range(4):
                    k = k2 + kk
                    fw.dma("sync", s[:, kk * 512:kk * 512 + fw_], src[e, k * 128:(k + 1) * 128, f0:f0 + fw_], writes=[s])
                for kk in range(4):
                    cast(dst, dst[:, k2 + kk, 0:fw_], s, s[:, kk * 512:kk * 512 + fw_])
        nf = fw_ // 128
        for f2 in range(0, nf, 2):
            s = stg.next()
            for ff in range(min(2, nf - f2)):
                f = f2 + ff
                fw.dma("sync", s[:, ff * 1024:(ff + 1) * 1024], wd_d[e, f0 + f * 128:f0 + (f + 1) * 128, :], writes=[s])
            for ff in range(min(2, nf - f2)):
                cast(wd, wd[:, f2 + ff, :], s, s[:, ff * 1024:(ff + 1) * 1024])
        return wg, wu, wd

    for sc in scs:
        off = 0
        offs = []
        for (seg, c0, N) in sc:
            offs.append(off)
            for k in range(8):
                fw.dma("gpsimd", u2[:, k, off:off + N], u2_d[k * 128:(k + 1) * 128, c0:c0 + N], writes=[u2])
            off += N
        if moe:
            ci_ = cin.next()
            off2 = 0
            for (seg, c0, N) in sc:
                fw.dma("gpsimd", ci_[:, off2:off2 + N], comb_d[:, c0:c0 + N], writes=[ci_])
                off2 += N
        cur_e = -1
        cb = None
        for gi, (e, f0, fw_) in enumerate(groups):
            wg, wu, wd = load_group(e, f0, fw_)
            nf = fw_ // 128
            if moe and e != cur_e:
                cur_e = e
                cb = cbp.next()
                for ci2, (seg, c0, N) in enumerate(sc):
                    p = PSc.next()
                    o.mm(p, p[:, 0:N], selE, selE[:, e, :], ci_, ci_[:, offs[ci2]:offs[ci2] + N])
                    o.act(cb, cb[:, ci2, 0:N], p, p[:, 0:N], AF.Copy)
            for ci2, (seg, c0, N) in enumerate(sc):
                of = offs[ci2]
                h = hp.next()
                for f in range(nf):
                    pg = PSg.next(); pu = PSu.next()
                    for k in range(8):
                        o.mm(pg, pg[:, 0:N], wg, wg[:, k, f * 128:(f + 1) * 128], u2, u2[:, k, of:of + N],
                             start=(k == 0), stop=(k == 7))
                    for k in range(8):
                        o.mm(pu, pu[:, 0:N], wu, wu[:, k, f * 128:(f + 1) * 128], u2, u2[:, k, of:of + N],
                             start=(k == 0), stop=(k == 7))
                    sg = sgp.next()
                    o.act(sg, sg[:, 0:N], pg, pg[:, 0:N], AF.Silu)
                    if moe:
                        sg2 = sgp.next()
                        o.tt("vector", sg2, sg2[:, 0:N], sg, sg[:, 0:N], pu, pu[:, 0:N], ALU.mult)
                        o.tt("gpsimd", h, h[:, f, 0:N], sg2, sg2[:, 0:N], cb, cb[:, ci2, 0:N], ALU.mult)
                    else:
                        o.tt("vector", h, h[:, f, 0:N], sg, sg[:, 0:N], pu, pu[:, 0:N], ALU.mult)
                for n in range(8):
                    py = PSy.next()
                    for f in range(nf):
                        o.mm(py, py[:, 0:N], wd, wd[:, f, n * 128:(n + 1) * 128], h, h[:, f, 0:N], start=(f == 0),
                             stop=(f == nf - 1))
                    if gi == 0:
                        o.act(yacc, yacc[:, n, of:of + N], py, py[:, 0:N], AF.Copy)
                    else:
                        o.tt("vector", yacc, yacc[:, n, of:of + N], yacc, yacc[:, n, of:of + N], py, py[:, 0:N], ALU.add)
        for ci2, (seg, c0, N) in enumerate(sc):
            of = offs[ci2]
            gn = "g5_l" if seg == 0 else "g5_c"
            for n in range(8):
                x1 = x1p.next()
                fw.dma("sync", x1[:, 0:N], x1_d[n * 128:(n + 1) * 128, c0:c0 + N], writes=[x1])
                x2 = x2p.next()
                o.stt(x2, x2[:, 0:N], yacc, yacc[:, n, of:of + N], pv(gn, n), x1, x1[:, 0:N], ALU.mult, ALU.add,
                      extra_r=[pvec])
                for dst_ in x2dst(seg, n, c0, N):
                    out_evs.append(fw.dma("gpsimd", dst_, x2[:, 0:N], reads=[x2]))
    return env.done(out_evs)


POOL_WINDOWS = (2, 4, 8, 16)

def mod_inputs(inputs, nl=4):
    cT = np.stack([inputs["c"][0], inputs["c"][1], inputs["c_ctx"]], axis=1).astype(np.float32)
    maps = []
    for j in range(8):
        maps.append({"cT": cT, "mw": np.ascontiguousarray(inputs["mod_w"][:nl, :, j * 768:(j + 1) * 768]),
                     "mb": np.ascontiguousarray(inputs["mod_b"][:nl, j * 768:(j + 1) * 768].reshape(nl, 6, 128).transpose(0, 2, 1))})
    return maps

def mod_gather(results):
    return np.concatenate([r["out"] for r in results], axis=1)

def cols(v):
    return np.ascontiguousarray(v.reshape(-1, 128).T)

def rope_tables(L, grid_w=64):
    rows = L // grid_w
    row = np.repeat(np.arange(rows, dtype=np.float32), grid_w)
    col = np.tile(np.arange(grid_w, dtype=np.float32), rows)
    inv = np.power(np.float32(10000.0), -np.arange(16, dtype=np.float32) / np.float32(16)).astype(np.float32)
    C = np.zeros((128, L), np.float32); S = np.zeros((128, L), np.float32)
    for p in range(128):
        dh = p % 64
        axis = dh // 32; ab = (dh % 32) // 16; f = dh % 16
        pos = row if axis == 0 else col
        ang = (pos * inv[f]).astype(np.float32)
        C[p] = np.cos(ang); S[p] = np.sin(ang) * (-1.0 if ab == 0 else 1.0)
    return C, S

def const_mats():
    cm = np.zeros((128, 4, 128), np.float32)
    cm[:, 0, :] = 1.0 / 1024.0
    for p in range(128):
        for k in range(128):
            if p // 64 == k // 64:
                cm[k, 1, p] = 1.0 / 64.0
    for p in range(128):
        dh = p % 64; ab = (dh % 32) // 16
        partner = p + 16 if ab == 0 else p - 16
        cm[partner, 2, p] = 1.0
    cm[:, 3, :] = np.eye(128, dtype=np.float32)
    return cm

def invc_table(qd, nq, L, Lc=256):
    t = np.zeros((128, 2, 2, 16), np.float32)
    for p in range(128):
        for pt in range(2):
            g = 2 * pt + (1 if p >= 64 else 0)
            w = POOL_WINDOWS[g]
            for e in range(16):
                for seg, Ls, realL, realR in ((0, L, qd == 0, qd == nq - 1), (1, Lc, True, True)):
                    if e < 8:
                        tt = e
                        cnt = min(tt + w // 2, Ls) - max(tt - w // 2, 0) if realL else w
                    else:
                        tt = Ls - 16 + e
                        cnt = min(tt + w // 2, Ls) - max(tt - w // 2, 0) if realR else w
                    t[p, seg, pt, e] = 1.0 / cnt
    return t

def p1_inputs(inputs, l, mvec, x, h, ropeC, ropeS, cm):
    B, L, D = x.shape
    nq = 8 // B
    TL = L // nq
    maps = []
    pl = inputs["pool_lin"][l]
    plin = np.zeros((128, 2, 128), np.float32)
    for pt in range(2):
        plin[0:64, pt, 0:64] = pl[2 * pt]; plin[64:128, pt, 64:128] = pl[2 * pt + 1]
    for j in range(8):
        b, qd = j // nq, j % nq
        xp = np.zeros((TL + 16, D), np.float32)
        lo, hi = qd * TL - 8, qd * TL + TL + 8
        slo, shi = max(lo, 0), min(hi, L)
        xp[slo - lo:shi - lo] = x[b, slo:shi]
        hp = np.zeros((h.shape[1] + 16, D), np.float32)
        hp[8:8 + h.shape[1]] = h[b]
        pv = np.zeros((128, NPV), np.float32)
        def put(name, arr):
            c0, n = PV[name]; pv[:, c0:c0 + n] = arr.reshape(128, n)
        m = mvec[l]
        put("g1", cols(inputs["norm1_g"][l]))
        put("sh_l", cols(m[0:1024, b])); put("sc_l", cols(m[1024:2048, b]))
        put("sh_c", cols(m[0:1024, 2])); put("sc_c", cols(m[1024:2048, 2]))
        sw = inputs["hy_short_w"][l]
        cw = np.zeros((128, 18), np.float32)
        for t in range(6):
            for tap in range(3):
                cw[:, t * 3 + tap] = sw[tap, t * 128:(t + 1) * 128]
        put("cw", cw); put("cb", cols(inputs["hy_short_b"][l]))
        put("gq", np.tile(inputs["qk_norm_g"][l, 0], 2)[:, None]); put("gk", np.tile(inputs["qk_norm_g"][l, 1], 2)[:, None])
        put("psc", cols(inputs["pool_scale"][l]))
        put("mL", np.full((128, 1), 0.0 if qd == 0 else 1.0, np.float32))
        put("mR", np.full((128, 1), 0.0 if qd == nq - 1 else 1.0, np.float32))
        put("eps", np.full((128, 1), 1e-6, np.float32))
        maps.append({"xT": np.ascontiguousarray(xp.T), "hT": np.ascontiguousarray(hp.T), "pvec": pv,
                     "w_in": np.ascontiguousarray(inputs["w_in"][l]),
                     "ropeC": np.ascontiguousarray(ropeC[:, qd * TL:(qd + 1) * TL]),
                     "ropeS": np.ascontiguousarray(ropeS[:, qd * TL:(qd + 1) * TL]),
                     "cmats": cm, "plin": plin, "invc": invc_table(qd, nq, L, h.shape[1])})
    return maps


def pvec4(inputs, l, mvec, b):
    pv = np.zeros((128, NPV4), np.float32)
    def put(name, arr):
        c0, n = PV4[name]; pv[:, c0:c0 + n] = arr.reshape(128, n)
    m = mvec[l]
    put("hyb", cols(inputs["hy_bias"][l]))
    put("g2_l", cols(m[2048:3072, b])); put("g2_c", cols(m[2048:3072, 2]))
    put("n2g", cols(inputs["norm2_g"][l]))
    put("sh_l", cols(m[3072:4096, b])); put("sc_l", cols(m[4096:5120, b]))
    put("sh_c", cols(m[3072:4096, 2])); put("sc_c", cols(m[4096:5120, 2]))
    put("g5_l", cols(m[5120:6144, b])); put("g5_c", cols(m[5120:6144, 2]))
    put("eps", np.full((128, 1), 1e-6, np.float32))
    return pv

ONESD = np.full((128, 128), 1.0 / 1024.0, np.float32)
IDENT = np.eye(128, dtype=np.float32)
SELE = np.zeros((8, 8, 128), np.float32)
for _e in range(8):
    SELE[_e, _e, :] = 1.0


SEL32 = np.ascontiguousarray(np.concatenate([np.eye(32), np.eye(32)], 0).astype(np.float32))


def pvec1(inputs, l, mvec, b, qd, nq):
    pv = np.zeros((128, NPV), np.float32)
    def put(name, arr):
        c0, n = PV[name]; pv[:, c0:c0 + n] = arr.reshape(128, n)
    m = mvec[l]
    put("g1", cols(inputs["norm1_g"][l]))
    put("sh_l", cols(m[0:1024, b])); put("sc_l", cols(m[1024:2048, b]))
    put("sh_c", cols(m[0:1024, 2])); put("sc_c", cols(m[1024:2048, 2]))
    sw = inputs["hy_short_w"][l]
    cw = np.zeros((128, 18), np.float32)
    for t in range(6):
        for tap in range(3):
            cw[:, t * 3 + tap] = sw[tap, t * 128:(t + 1) * 128]
    put("cw", cw); put("cb", cols(inputs["hy_short_b"][l]))
    put("gq", np.tile(inputs["qk_norm_g"][l, 0], 2)[:, None]); put("gk", np.tile(inputs["qk_norm_g"][l, 1], 2)[:, None])
    put("psc", cols(inputs["pool_scale"][l]))
    put("mL", np.full((128, 1), 0.0 if qd == 0 else 1.0, np.float32))
    put("mR", np.full((128, 1), 0.0 if qd == nq - 1 else 1.0, np.float32))
    put("eps", np.full((128, 1), 1e-6, np.float32))
    return pv

_PROGS = {}


def _prog(key, builder):
    if key not in _PROGS:
        _PROGS[key] = builder()
    return _PROGS[key]


def _run(nc, maps):
    import time as _t
    t0 = _t.time()
    res = run_bass_kernel_spmd(nc, maps, core_ids=list(range(8)))
    if _VERBOSE:
        print("[kernel] launch done in %.1fs" % (_t.time() - t0), flush=True)
    return res.results


_VERBOSE = False


_BF = ml_dtypes.bfloat16


def kernel(**inputs):
    inp = {k: np.asarray(v) for k, v in inputs.items()}
    x0 = inp["x"]
    B, L, Dm = x0.shape
    C = inp["ctx"].shape[1]
    nq = 8 // B
    TL = L // nq
    T = TL + C
    NL = inp["mod_w"].shape[0]
    mvec = mod_gather(_run(_prog(("p0", NL), lambda: build_p0(768, NL)), mod_inputs(inp, NL)))
    ropeC, ropeS = rope_tables(L)
    cm = const_mats()
    tabs = {LL: lc_tables(LL) for LL in (L, C)}
    ftabs = {(LL, j): filt_tables(LL, 32 * j) for LL in (L, C) for j in range(8)}
    xcur = []
    for j in range(8):
        b, qd = j // nq, j % nq
        xcur.append(np.ascontiguousarray(np.concatenate([x0[b, qd * TL:(qd + 1) * TL], inp["ctx"][b]], 0).T))
    z8 = np.zeros((Dm, 8), np.float32)
    for l in range(NL):
        moe = (l % 2 == 1)
        jl = l // 2
        lam_init = 0.8 - 0.6 * math.exp(-0.3 * l)
        maps = []
        pl = inp["pool_lin"][l]
        plin = np.zeros((128, 2, 128), np.float32)
        for pt in range(2):
            plin[0:64, pt, 0:64] = pl[2 * pt]
            plin[64:128, pt, 64:128] = pl[2 * pt + 1]
        for j in range(8):
            b, qd = j // nq, j % nq
            left = xcur[j - 1][:, TL - 8:TL] if qd > 0 else z8
            right = xcur[j + 1][:, 0:8] if qd < nq - 1 else z8
            xT = np.ascontiguousarray(np.concatenate([left, xcur[j][:, :TL], right], 1))
            hT = np.ascontiguousarray(np.concatenate([z8, xcur[j][:, TL:], z8], 1))
            maps.append({"xT": xT, "hT": hT, "pvec": pvec1(inp, l, mvec, b, qd, nq),
                         "w_in": np.ascontiguousarray(inp["w_in"][l]),
                         "ropeC": np.ascontiguousarray(ropeC[:, qd * TL:(qd + 1) * TL]),
                         "ropeS": np.ascontiguousarray(ropeS[:, qd * TL:(qd + 1) * TL]),
                         "cmats": cm, "plin": plin, "invc": invc_table(qd, nq, L, C)})
        r1 = _run(_prog(("p1", TL, C), lambda: build_p1(TL, C)), maps)
        r1 = [{k: np.asarray(v) for k, v in r.items()} for r in r1]
        maps = []
        pv2 = np.zeros((128, NPV2), np.float32)
        pv2[:, 0] = 1e-6
        pv2[:, 1] = lam_init
        pv2[:, 2] = inp["subln_g"][l]
        pv2[:, 3] = 1.0 - lam_init
        dlam = np.ascontiguousarray(np.broadcast_to(inp["diff_lambda"][l][None], (128, 4, 64))).astype(np.float32)
        for j in range(8):
            b, hd = j // 4, j % 4
            cores = [b * nq + qd for qd in range(nq)]
            rs_ = slice(hd * 128, (hd + 1) * 128)
            qT = np.concatenate([r1[c]["qT"][rs_, :TL] for c in cores] + [r1[cores[0]]["qT"][rs_, TL:]], 1)
            kT = np.concatenate([r1[cores[0]]["kT"][rs_, TL:]] + [r1[c]["kT"][rs_, :TL] for c in cores], 1)
            v = np.concatenate([r1[cores[0]]["v"][TL:, rs_]] + [r1[c]["v"][:TL, rs_] for c in cores], 0)
            maps.append({"qT": np.ascontiguousarray(qT), "kT": np.ascontiguousarray(kT), "v": np.ascontiguousarray(v),
                         "dlam": dlam, "pvec": pv2, "ident": IDENT})
        r2 = _run(_prog(("p2", L, C), lambda: build_p2(L, C, True)), maps)
        r2 = [np.asarray(r["attT"]) for r in r2]
        maps = []
        for j in range(8):
            ch0 = 32 * j
            w3 = inp["hy_f_w3"][l]
            m = {"w1": np.ascontiguousarray(inp["hy_f_w1"][l]), "w2": np.ascontiguousarray(inp["hy_f_w2"][l]),
                 "w3s": np.ascontiguousarray(np.concatenate([w3[:, ch0:ch0 + 32], w3[:, 256 + ch0:256 + ch0 + 32]], 1)),
                 "fpv": np.ascontiguousarray(np.stack([inp["hy_f_freq1"][l], inp["hy_f_b1"][l], inp["hy_f_freq2"][l],
                                                       inp["hy_f_b2"][l]], 1)),
                 "sel": SEL32}
            for LL in (L, C):
                sfx = "_%d" % LL
                for k_, v_ in tabs[LL].items():
                    m[k_ + sfx] = v_
                m["feat" + sfx], m["dec" + sfx] = ftabs[(LL, j)]
                rows = []
                for b in range(B):
                    if LL == L:
                        rows.append(np.concatenate([r1[b * nq + qd]["v2"][ch0:ch0 + 32, :TL] for qd in range(nq)], 1))
                    else:
                        rows.append(r1[b * nq]["v2"][ch0:ch0 + 32, TL:])
                m["vin" + sfx] = np.ascontiguousarray(np.concatenate(rows, 0))
            maps.append(m)
        r3 = _run(_prog(("p3", L, C), lambda: build_p3((L, C))), maps)
        r3 = [{k: np.asarray(v) for k, v in r.items()} for r in r3]
        maps = []
        for j in range(8):
            b, qd = j // nq, j % nq
            conv = np.zeros((256, T), np.float32)
            att = np.zeros((512, T), _BF)
            for jj in range(8):
                conv[32 * jj:32 * jj + 32, :TL] = r3[jj]["yout_%d" % L][b * 32:(b + 1) * 32, qd * TL:(qd + 1) * TL]
                conv[32 * jj:32 * jj + 32, TL:] = r3[jj]["yout_%d" % C][b * 32:(b + 1) * 32, :]
            for hd in range(4):
                a = r2[b * 4 + hd]
                att[hd * 128:(hd + 1) * 128, :TL] = a[:, qd * TL:(qd + 1) * TL]
                att[hd * 128:(hd + 1) * 128, TL:] = a[:, L:L + C]
            mp = {"xT": xcur[j], "ypool": r1[j]["ypool"], "x0c": r1[j]["x0c"], "v2": r1[j]["v2"], "conv": conv,
                  "attT": att, "w_out": np.ascontiguousarray(inp["w_out"][l]), "pvec": pvec4(inp, l, mvec, b),
                  "onesD": ONESD}
            if moe:
                mp["rw"] = np.ascontiguousarray(inp["router_w"][jl].reshape(8, 128, 8).transpose(1, 0, 2))
                mp["ident"] = IDENT
            maps.append(mp)
        r4 = _run(_prog(("p4", TL, C, moe), lambda: build_p4(TL, C, moe)), maps)
        r4 = [{k: np.asarray(v) for k, v in r.items()} for r in r4]
        del r1, r2, r3
        maps = []
        if moe:
            wg = np.ascontiguousarray(inp["moe_w_gate"][jl]); wu = np.ascontiguousarray(inp["moe_w_up"][jl])
            wd = np.ascontiguousarray(inp["moe_w_down"][jl])
        else:
            wg = np.ascontiguousarray(inp["ffn_w_gate"][jl][None]); wu = np.ascontiguousarray(inp["ffn_w_up"][jl][None])
            wd = np.ascontiguousarray(inp["ffn_w_down"][jl][None])
        for j in range(8):
            b = j // nq
            mp = {"x1T": r4[j]["x1T"], "u2T": r4[j]["u2T"], "pvec": pvec4(inp, l, mvec, b), "wg": wg, "wu": wu, "wd": wd}
            if moe:
                mp["combT"] = r4[j]["combT"]
                mp["selE"] = SELE
            maps.append(mp)
        r5 = _run(_prog(("p5", TL, C, moe), lambda: build_p5(TL, C, moe)), maps)
        xcur = [np.asarray(r["x2T"]) for r in r5]
        del r4, r5
    out = np.zeros((B, L, Dm), np.float32)
    for j in range(8):
        b, qd = j // nq, j % nq
        out[b, qd * TL:(qd + 1) * TL] = xcur[j][:, :TL].T
    return out
```
